# Optimizing a Trainium2 kernel written in Bass

```python
import jax, jax.numpy as jnp
from jax import lax
import numpy as np

D_MODEL = 1024
BATCH = 8
SEQ = 2048
DEPTH = 4

CHUNK = 64
EPS = 1e-6
M_HEADS = 4
M_V = D_MODEL // 8
M_QK = M_V // 2
M_CONV = 4
G_HEADS = 4
G_V = D_MODEL // 16
G_QK = G_V // 2
G_RANK = 16
G_NORMALIZER = 16.0
R_HEADS = 4
R_V = D_MODEL // 16
R_QK = R_V // 2
ROPE_BASE = 10000.0
MAX_OFFSET = 4096
D_FF = 4 * D_MODEL

M_QK_W = M_HEADS * M_QK
M_V_W = M_HEADS * M_V
G_QK_W = G_HEADS * G_QK
G_V_W = G_HEADS * G_V
R_QK_W = R_HEADS * R_QK
R_V_W = R_HEADS * R_V
MIX = M_V_W + G_V_W + R_V_W
IN_SIZES = (M_QK_W, M_QK_W, M_V_W, M_HEADS, M_HEADS, M_V_W,
            G_QK_W, G_QK_W, G_V_W, G_RANK, G_V_W,
            R_QK_W, R_QK_W, R_V_W, R_V_W)
IN_COLS = sum(IN_SIZES)

kernel_name = 'hybrid_mlstm_gla_retention_sandwich'


def rmsnorm(x, g):
    xf = x.astype(jnp.float32)
    y = xf * lax.rsqrt(jnp.mean(xf * xf, axis=-1, keepdims=True) + EPS)
    return (y * g.astype(jnp.float32)).astype(x.dtype)


def head_rmsnorm(y, g, heads):
    B, S, C = y.shape
    yh = y.reshape(B, S, heads, C // heads)
    yh = yh * lax.rsqrt(jnp.mean(yh * yh, axis=-1, keepdims=True) + EPS)
    return yh.reshape(B, S, C) * g.astype(jnp.float32)


def causal_depthwise_conv(x, w):
    K, C = w.shape
    return lax.conv_general_dilated(x, w[:, None, :], window_strides=(1,), padding=[(K - 1, 0)],
                                    dimension_numbers=('NWC', 'WIO', 'NWC'), feature_group_count=C)


def to_chunks(x, heads):
    B, S, C = x.shape
    return x.reshape(B, S // CHUNK, CHUNK, heads, C // heads).transpose(0, 3, 1, 2, 4)


def from_chunks(y):
    B, H, N, L, d = y.shape
    return y.transpose(0, 2, 3, 1, 4).reshape(B, N * L, H * d)


def apply_rotary(x, cos, sin, heads):
    B, S, C = x.shape
    xh = x.reshape(B, S, heads, C // heads)
    x1, x2 = jnp.split(xh, 2, axis=-1)
    out = jnp.concatenate([x1 * cos - x2 * sin, x1 * sin + x2 * cos], axis=-1)
    return out.reshape(B, S, C)


def chunk_state_scan(decay, update):
    def step(state, inp):
        a, u = inp
        return a * state + u, state
    _, prev = lax.scan(step, jnp.zeros_like(update[:, :, 0]),
                       (jnp.moveaxis(decay, 2, 0), jnp.moveaxis(update, 2, 0)))
    return jnp.moveaxis(prev, 0, 2)


def chunked_gated_linear_attention(q, k, v, log_decay):
    L = q.shape[3]
    causal = jnp.tril(jnp.ones((L, L), dtype=bool))
    b = jnp.cumsum(log_decay, axis=3)
    b_last = b[:, :, :, -1:, :]
    rel = b[:, :, :, :, None, :] - b[:, :, :, None, :, :]
    decay = jnp.exp(jnp.where(causal[:, :, None], rel, -jnp.inf))
    if log_decay.shape[-1] == 1:
        scores = jnp.einsum('bhnid,bhnjd->bhnij', q, k) * decay[..., 0]
    else:
        scores = jnp.einsum('bhnid,bhnjd,bhnijd->bhnij', q, k, decay)
    intra = jnp.einsum('bhnij,bhnje->bhnie', scores, v)
    update = jnp.einsum('bhnjd,bhnje->bhnde', k * jnp.exp(b_last - b), v)
    prev = chunk_state_scan(jnp.exp(b_last[:, :, :, 0, :])[..., None], update)
    inter = jnp.einsum('bhnid,bhnde->bhnie', q * jnp.exp(b), prev)
    return intra + inter


def mlstm_chunked(q, k, v, i_pre, f_pre):
    L = q.shape[3]
    causal = jnp.tril(jnp.ones((L, L), dtype=bool))
    b = jnp.cumsum(jax.nn.log_sigmoid(f_pre), axis=3)
    b_last = b[..., -1]
    tail = b_last[..., None] - b + i_pre
    a = jnp.max(tail, axis=-1)
    wt = jnp.exp(tail - a[..., None])
    upd_c = jnp.einsum('bhnj,bhnjd,bhnje->bhnde', wt, k, v)
    upd_n = jnp.einsum('bhnj,bhnjd->bhnd', wt, k)

    def step(carry, inp):
        c, n, m = carry
        bl, an, uc, un = inp
        m_new = jnp.maximum(bl + m, an)
        s_old = jnp.exp(bl + m - m_new)
        s_new = jnp.exp(an - m_new)
        c_new = s_old[..., None, None] * c + s_new[..., None, None] * uc
        n_new = s_old[..., None] * n + s_new[..., None] * un
        return (c_new, n_new, m_new), (c, n, m)

    init = (jnp.zeros_like(upd_c[:, :, 0]), jnp.zeros_like(upd_n[:, :, 0]), jnp.zeros_like(a[:, :, 0]))
    _, (prev_c, prev_n, prev_m) = lax.scan(
        step, init, (jnp.moveaxis(b_last, 2, 0), jnp.moveaxis(a, 2, 0),
                     jnp.moveaxis(upd_c, 2, 0), jnp.moveaxis(upd_n, 2, 0)))
    prev_c = jnp.moveaxis(prev_c, 0, 2)
    prev_n = jnp.moveaxis(prev_n, 0, 2)
    prev_m = jnp.moveaxis(prev_m, 0, 2)

    d_log = b[..., :, None] - b[..., None, :] + i_pre[..., None, :]
    d_log = jnp.where(causal, d_log, -jnp.inf)
    inter_log = b + prev_m[..., None]
    m_i = jnp.maximum(jnp.max(d_log, axis=-1), inter_log)
    p = jnp.exp(d_log - m_i[..., None]) * jnp.einsum('bhnid,bhnjd->bhnij', q, k)
    inter_scale = jnp.exp(inter_log - m_i)
    num = (jnp.einsum('bhnij,bhnje->bhnie', p, v)
           + inter_scale[..., None] * jnp.einsum('bhnid,bhnde->bhnie', q, prev_c))
    den = jnp.sum(p, axis=-1) + inter_scale * jnp.einsum('bhnid,bhnd->bhni', q, prev_n)
    return num / jnp.maximum(jnp.abs(den), jnp.exp(-m_i))[..., None]


def hybrid_mixer(u, cos, sin, w_in, conv_w, i_bias, f_bias, m_norm, g_w_up, g_bias, g_norm, r_norm, w_out):
    f32 = jnp.float32
    proj = jnp.einsum('bsd,dc->bsc', u, w_in).astype(f32)
    (m_q, m_k, m_v, m_i, m_f, m_o, g_q, g_k, g_v, g_a, g_g,
     r_q, r_k, r_v, r_g) = jnp.split(proj, [int(c) for c in np.cumsum(IN_SIZES)[:-1]], axis=-1)

    qk = jax.nn.silu(causal_depthwise_conv(jnp.concatenate([m_q, m_k], axis=-1), conv_w.astype(f32)))
    mq, mk = jnp.split(qk, 2, axis=-1)
    h_m = mlstm_chunked(to_chunks(mq, M_HEADS), to_chunks(mk * (M_QK ** -0.5), M_HEADS),
                        to_chunks(m_v, M_HEADS),
                        to_chunks(m_i + i_bias.astype(f32), M_HEADS)[..., 0],
                        to_chunks(m_f + f_bias.astype(f32), M_HEADS)[..., 0])
    y_m = head_rmsnorm(from_chunks(h_m), m_norm, M_HEADS) * jax.nn.sigmoid(m_o)

    log_alpha = jax.nn.log_sigmoid(g_a @ g_w_up.astype(f32) + g_bias.astype(f32)) / G_NORMALIZER
    h_g = chunked_gated_linear_attention(to_chunks(g_q * (G_QK ** -0.5), G_HEADS), to_chunks(g_k, G_HEADS),
                                         to_chunks(g_v, G_HEADS), to_chunks(log_alpha, G_HEADS))
    y_g = head_rmsnorm(from_chunks(h_g), g_norm, G_HEADS) * jax.nn.silu(g_g)

    rq = to_chunks(apply_rotary(r_q, cos, sin, R_HEADS), R_HEADS)
    rk = to_chunks(apply_rotary(r_k, cos, sin, R_HEADS) * (R_QK ** -0.5), R_HEADS)
    log_gamma = jnp.log1p(-jnp.exp2(-5.0 - jnp.arange(R_HEADS, dtype=f32)))
    B, H, N, L, _ = rq.shape
    log_decay = jnp.broadcast_to(log_gamma[None, :, None, None, None], (B, H, N, L, 1))
    h_r = chunked_gated_linear_attention(rq, rk, to_chunks(r_v, R_HEADS), log_decay)
    y_r = head_rmsnorm(from_chunks(h_r), r_norm, R_HEADS) * jax.nn.silu(r_g)

    y = jnp.concatenate([y_m, y_g, y_r], axis=-1).astype(u.dtype)
    return jnp.einsum('bsc,cd->bsd', y, w_out)


def squared_relu_mlp(u, w1, w2):
    hdn = jnp.square(jax.nn.relu(jnp.einsum('bsd,df->bsf', u, w1)))
    return jnp.einsum('bsf,fd->bsd', hdn, w2)


def setup_inputs(seed: int = 0) -> dict:
    key = jax.random.key(seed)
    ks = jax.random.split(key, 20)
    f32 = jnp.float32

    def nrm(k, shape, scale):
        return jax.random.normal(k, shape, f32) * scale

    def gain(k, width):
        return 1.0 + nrm(k, (DEPTH, width), 0.02)

    x = nrm(ks[0], (BATCH, SEQ, D_MODEL), 1.0)
    offsets = jax.random.randint(ks[1], (BATCH, 1), 0, MAX_OFFSET, dtype=jnp.int32)
    positions = (offsets + jnp.arange(SEQ, dtype=jnp.int32)[None, :]).astype(jnp.int32)
    return {
        'x': x,
        'positions': positions,
        'norm_pre_mix': gain(ks[2], D_MODEL),
        'norm_post_mix': gain(ks[3], D_MODEL),
        'norm_pre_ffn': gain(ks[4], D_MODEL),
        'norm_post_ffn': gain(ks[5], D_MODEL),
        'w_in': nrm(ks[6], (DEPTH, D_MODEL, IN_COLS), D_MODEL ** -0.5),
        'mlstm_conv_w': nrm(ks[7], (DEPTH, M_CONV, 2 * M_QK_W), M_CONV ** -0.5),
        'mlstm_i_bias': nrm(ks[8], (DEPTH, M_HEADS), 0.1),
        'mlstm_f_bias': jnp.linspace(3.0, 6.0, M_HEADS, dtype=f32)[None, :] + nrm(ks[9], (DEPTH, M_HEADS), 0.1),
        'mlstm_norm': gain(ks[10], M_V_W),
        'gla_w_up': nrm(ks[11], (DEPTH, G_RANK, G_QK_W), G_RANK ** -0.5),
        'gla_gate_bias': nrm(ks[12], (DEPTH, G_QK_W), 0.1),
        'gla_norm': gain(ks[13], G_V_W),
        'ret_norm': gain(ks[14], R_V_W),
        'w_out': nrm(ks[15], (DEPTH, MIX, D_MODEL), MIX ** -0.5),
        'w_ff1': nrm(ks[16], (DEPTH, D_MODEL, D_FF), D_MODEL ** -0.5),
        'w_ff2': nrm(ks[17], (DEPTH, D_FF, D_MODEL), D_FF ** -0.5),
    }


def reference(x, positions, norm_pre_mix, norm_post_mix, norm_pre_ffn, norm_post_ffn, w_in,
              mlstm_conv_w, mlstm_i_bias, mlstm_f_bias, mlstm_norm, gla_w_up, gla_gate_bias,
              gla_norm, ret_norm, w_out, w_ff1, w_ff2):
    inv_freq = ROPE_BASE ** (-jnp.arange(0, R_QK, 2, dtype=jnp.float32) / R_QK)
    ang = positions.astype(jnp.float32)[..., None] * inv_freq
    cos = jnp.cos(ang)[:, :, None, :]
    sin = jnp.sin(ang)[:, :, None, :]

    h = x
    for l in range(DEPTH):
        mixed = hybrid_mixer(rmsnorm(h, norm_pre_mix[l]), cos, sin, w_in[l], mlstm_conv_w[l],
                             mlstm_i_bias[l], mlstm_f_bias[l], mlstm_norm[l], gla_w_up[l],
                             gla_gate_bias[l], gla_norm[l], ret_norm[l], w_out[l])
        h = h + rmsnorm(mixed, norm_post_mix[l])
        ff = squared_relu_mlp(rmsnorm(h, norm_pre_ffn[l]), w_ff1[l], w_ff2[l])
        h = h + rmsnorm(ff, norm_post_ffn[l])
    return h
```

```python
import numpy as np
from contextlib import ExitStack
import concourse.bass as bass
import concourse.mybir as mybir
from concourse.bass_utils import run_bass_kernel_spmd

F32 = mybir.dt.float32
BF16 = mybir.dt.bfloat16
I32 = mybir.dt.int32
AF = mybir.ActivationFunctionType
ALU = mybir.AluOpType
AX = mybir.AxisListType

ENGS = ["tensor", "vector", "scalar", "gpsimd", "sync"]
N_DMA_SEMS = 8

D = 1024
T = 2048
DEPTH = 4
NPART = 2
TH = T // NPART
NT = TH // 128
NBLK = TH // 512
KC = D // 128
INC = 3096
DFF = 4096
NG = 8
EPS = 1e-6
C_MQ, C_MK, C_MV, C_MIF, C_MO = 0, 256, 512, 1024, 1032
C_GQ, C_GK, C_GV, C_GA, C_GG = 1544, 1672, 1800, 2056, 2072
C_RQ, C_RK, C_RV, C_RG = 2328, 2456, 2584, 2840
K_ID, K_TRI, K_ONES, K_HM, K_BM, K_RE, K_EL, K_IF, K_END = 0, 128, 256, 384, 388, 644, 900, 901, 920


class Buf:
    __slots__ = ("name", "last_w", "readers", "excl")

    def __init__(self, name="", excl=False):
        self.name = name
        self.last_w = None
        self.readers = []
        self.excl = excl


class Op:
    __slots__ = ("eng", "fn", "deps", "signal", "ordinal", "dma", "dma_sem", "dma_val", "dma_prev")

    def __init__(self, eng, fn, dma):
        self.eng = eng
        self.fn = fn
        self.deps = set()
        self.signal = False
        self.ordinal = None
        self.dma = dma
        self.dma_sem = None
        self.dma_val = None
        self.dma_prev = 0


class Sched:
    def __init__(self, same_engine_sync=True):
        self.ops = {e: [] for e in ENGS}
        self.same_engine_sync = same_engine_sync
        self.n_dma = {e: 0 for e in ENGS}

    def add(self, eng, fn, reads=(), writes=(), dma=False):
        op = Op(eng, fn, dma)
        deps = set()
        for b in reads:
            if b.last_w is not None:
                deps.add(b.last_w)
            if b.excl:
                for r in b.readers:
                    if r.eng != eng:
                        deps.add(r)
        for b in writes:
            if b.last_w is not None:
                deps.add(b.last_w)
            deps.update(b.readers)
        for b in reads:
            b.readers.append(op)
        for b in writes:
            b.last_w = op
            b.readers = []
        deps.discard(op)
        pruned = set()
        for d in deps:
            if d.eng == eng and not d.dma:
                if eng == "tensor" or not self.same_engine_sync:
                    continue
            pruned.add(d)
        op.deps = pruned
        for d in pruned:
            d.signal = True
        if dma:
            n = self.n_dma[eng]
            self.n_dma[eng] = n + 1
            op.dma_sem = n % N_DMA_SEMS
            op.dma_val = 16 * (n // N_DMA_SEMS + 1)
            op.dma_prev = 16 * (n // N_DMA_SEMS)
        self.ops[eng].append(op)
        return op

    def emit(self, nc, stack):
        for e in ENGS:
            c = 0
            for op in self.ops[e]:
                if not op.dma and op.signal:
                    c += 1
                    op.ordinal = c
        esem = {e: stack.enter_context(nc.semaphore("es_" + e)) for e in ENGS}
        dsem = {e: [stack.enter_context(nc.semaphore("ds_%s_%d" % (e, i))) for i in range(N_DMA_SEMS)]
                for e in ENGS if self.n_dma[e] > 0}
        block = stack.enter_context(nc.Block())
        nwaits = [0]

        def run(ename, eng):
            known = {}
            for op in self.ops[ename]:
                waits = {}
                for d in op.deps:
                    if d.dma:
                        s = dsem[d.eng][d.dma_sem]
                        v = d.dma_val
                    else:
                        s = esem[d.eng]
                        v = d.ordinal
                    key = id(s)
                    if key not in waits or waits[key][1] < v:
                        waits[key] = (s, v)
                if op.dma and op.dma_prev > 0:
                    s = dsem[ename][op.dma_sem]
                    key = id(s)
                    if key not in waits or waits[key][1] < op.dma_prev:
                        waits[key] = (s, op.dma_prev)
                for key, (s, v) in waits.items():
                    if known.get(key, 0) >= v:
                        continue
                    known[key] = v
                    eng.wait_ge(s, v)
                    nwaits[0] += 1
                ins = op.fn(eng)
                if ins is None:
                    continue
                if op.dma:
                    ins.then_inc(dsem[ename][op.dma_sem], 16)
                elif op.signal:
                    ins.then_inc(esem[ename], 1)

        @block.sync
        def _(e):
            run("sync", e)

        @block.tensor
        def _(e):
            run("tensor", e)

        @block.vector
        def _(e):
            run("vector", e)

        @block.scalar
        def _(e):
            run("scalar", e)

        @block.gpsimd
        def _(e):
            run("gpsimd", e)
        self.nwaits = nwaits[0]


def _dsize(dt):
    return 2 if dt == BF16 else 4


class Arena:
    def __init__(self, big, lo, hi):
        self.big = big
        self.lo = lo
        self.cur = lo
        self.hi = hi

    def alloc(self, free_shape, dtype=F32, parts=128):
        n = 1
        for s in free_shape:
            n *= s
        nbytes = n * _dsize(dtype)
        nbytes_r = (nbytes + 31) // 32 * 32
        off = self.cur
        self.cur += nbytes_r
        assert self.cur <= self.hi, "arena overflow %d > %d" % (self.cur, self.hi)
        assert nbytes % 4 == 0
        v = self.big[0:parts, off // 4: off // 4 + nbytes // 4]
        if dtype != F32:
            v = v.bitcast(dtype)
        if len(free_shape) == 2:
            v = v.rearrange("p (a b) -> p a b", a=free_shape[0])
        elif len(free_shape) == 3:
            v = v.rearrange("p (a b c) -> p a b c", a=free_shape[0], b=free_shape[1])
        return v


def bc_mid(ap2, n):
    P, F = ap2.shape
    return ap2.rearrange("p (o f) -> p o f", o=1).broadcast_to([P, n, F])


def bc_last(ap2, n):
    P, H = ap2.shape
    return ap2.rearrange("p (h o) -> p h o", o=1).broadcast_to([P, H, n])


def build_program(n_layers=DEPTH, run_parts=NPART, stage=None):
    nc = bass.Bass("TRN2", target_bir_lowering=False)
    L = n_layers

    def din(name, shape, dt=F32):
        return nc.dram_tensor(name, shape, dt, kind="ExternalInput").ap()

    x_d = din("x", [T, D])
    pos_d = din("pos", [128, T // 128], I32)
    gains_d = din("gains", [L * 4, D])
    w_in_d = din("w_in", [L, D, INC])
    w_out_d = din("w_out", [L, D, D])
    w_ff1_d = din("w_ff1", [L, D, DFF])
    w_ff2_d = din("w_ff2", [L, DFF, D])
    convT_d = din("convT", [512, L * 4])
    ifb_d = din("ifb", [1, L * 8])
    hn_d = din("hn", [L, D])
    wup_d = din("wup", [L, 16, 128])
    gb_d = din("gb", [1, L * 128])
    cf_d = din("cf", [128, K_END])
    out_d = nc.dram_tensor("out", [T, D], F32, kind="ExternalOutput").ap()

    S = Sched()

    class StopBuild(Exception):
        pass

    def ck(n):
        if stage is not None and stage == n:
            raise StopBuild()

    def op(eng, method, reads, writes, *args, **kw):
        return S.add(eng, lambda e: getattr(e, method)(*args, **kw), reads, writes)

    def dma(eng, out, in_, reads, writes):
        return S.add(eng, lambda e: e.dma_start(out=out, in_=in_), reads, writes, dma=True)

    st = ExitStack()
    with st:
        total = nc.sbuf_bytes_remaining
        total = (total // 128) * 128 - 128
        big_h = nc.alloc_sbuf_tensor("big", [128, total // 4], F32)
        A = Arena(big_h, 0, total)

        psum = [nc.alloc_psum_tensor("ps%d" % i, [128, 512], F32) for i in range(8)]
        psB = [Buf("ps%d" % i, excl=True) for i in range(8)]
        bank_ctr = [0]

        def bank():
            i = bank_ctr[0] % 8
            bank_ctr[0] += 1
            return psum[i][:], psB[i]

        h = A.alloc([NT, D])
        hB = [Buf("h%d" % i) for i in range(NT)]
        winb = A.alloc([KC, INC], BF16)
        winB = [Buf("win%d" % i) for i in range(4)]
        woutb = A.alloc([KC, D], BF16)
        woutB = Buf("wout")
        ring = [A.alloc([8 * 512], BF16) for _ in range(2)]
        ringB = [Buf("ringA"), Buf("ringB")]
        w1v = ring[0].rearrange("p (c f) -> p c f", c=8)
        w2v = ring[1].rearrange("p (c d) -> p c d", c=4)
        cf = A.alloc([K_END])
        cfB = Buf("cf")
        identb = A.alloc([128], BF16)
        cosT = A.alloc([T // 128, 16])
        sinT = A.alloc([T // 128, 16])
        rotB = Buf("rot")
        cw = A.alloc([4, L * 4])
        ifb = A.alloc([L * 8])
        gbb = A.alloc([128]); gbbB = Buf("gbb")
        wup = A.alloc([L, 128], parts=16)
        smallB = Buf("small")
        Cst = [A.alloc([2, 129]) for _ in range(L)]
        Sg = [A.alloc([256]) for _ in range(L)]
        Sr = [A.alloc([256]) for _ in range(L)]
        ctail = [A.alloc([4, 3]) for _ in range(L)]
        stB = [Buf("state%d" % l) for l in range(L)]
        C_bf = A.alloc([2, 129], BF16)
        Sg_bf = A.alloc([256], BF16)
        Sr_bf = A.alloc([256], BF16)
        v_ext = A.alloc([4, 129], BF16)
        vB = Buf("v_ext")
        gbc = A.alloc([D])
        gbcB = Buf("gbc")
        gbc2 = A.alloc([D])
        gbc2B = Buf("gbc2")
        sm = A.alloc([64])
        smB = Buf("sm")
        ss = sm[:, 0:1]
        ss2 = sm[:, 1:3]
        rstd = sm[:, 3:4]
        ph_lo = A.cur
        ph_hi = total

        M = Arena(big_h, ph_lo, ph_hi)
        hnbc = M.alloc([D]); hnB = Buf("hn")
        uTb = M.alloc([KC, 512], BF16); uTB = Buf("uTb")
        ytile = M.alloc([D], BF16); yB = Buf("ytile")
        xq = M.alloc([4, 515]); xqB = Buf("xq")
        eq = xq[:, :, 0:512]
        yq = M.alloc([4, 512]); yqB = Buf("yq")
        qkb = M.alloc([4, 512], BF16); qkB = Buf("qkb")
        gqk = M.alloc([2, 512], BF16); gqkB = Buf("gqk")
        gaT = M.alloc([512], parts=16); gaB = Buf("gaT")
        ifp = M.alloc([8]); lf = M.alloc([4]); t4 = M.alloc([4]); aj = M.alloc([4]); wj = M.alloc([4])
        den = M.alloc([4]); ssm = M.alloc([4]); fac = M.alloc([4]); mB = Buf("msmall")
        og = M.alloc([512]); ogB = Buf("og")
        t512 = [M.alloc([512]) for _ in range(3)]; t512B = [Buf("t512_%d" % i) for i in range(3)]
        gate = M.alloc([512]); gateB = Buf("gate")
        xg = t512[2]; xgB = t512B[2]
        R = t512[2].rearrange("p (h i) -> p h i", h=4); RB = t512B[2]
        vgr = M.alloc([512], BF16); vgrB = Buf("vgr")
        Xr = M.alloc([8, 32]); XrB = Buf("Xr")
        Xo = t512[1][:, 0:256].rearrange("p (a b) -> p a b", a=8); XoB = t512B[1]
        rt = [t512[0][:, 0:128].rearrange("p (a b) -> p a b", a=8), t512[0][:, 128:256].rearrange("p (a b) -> p a b", a=8)]; rtB = t512B[0]
        qkr = M.alloc([256], BF16); qkrB = Buf("qkr")
        DT = M.alloc([4, 128]); DTB = Buf("DT")
        E = M.alloc([4, 128]); EB = Buf("E")
        PT = M.alloc([4, 128], BF16); PTB = Buf("PT")
        qp = M.alloc([2, 128], BF16); qpB = Buf("qp")
        kp = M.alloc([256], BF16); kpB = Buf("kp")
        yT = M.alloc([KC, 128], BF16); yTB = Buf("yT")
        la = t512[2][:, 0:128]; laB = t512B[2]
        Ep = t512[2][:, 128:256]; En = t512[2][:, 256:384]; EpB = t512B[2]
        qt = M.alloc([128], BF16); qtB = Buf("qt")
        kt = M.alloc([128], BF16); ktB = Buf("kt")
        ktok = M.alloc([128], BF16); ktokB = Buf("ktok")
        Qbd = M.alloc([4, 128], BF16); QbdB = Buf("Qbd")
        rs4 = M.alloc([4]); rs4B = Buf("rs4")
        mixer_bufs = [hnB, uTB, yB, xqB, yqB, qkB, gqkB, gaB, mB, ogB, gateB, vgrB, XrB, qkrB, DTB, EB, PTB,
                      qpB, kpB, yTB, qtB, ktB, ktokB, QbdB, rs4B] + t512B

        FA = Arena(big_h, ph_lo, ph_hi)
        acc = FA.alloc([NT, D]); accB = [Buf("acc%d" % i) for i in range(NT)]
        uTh = FA.alloc([KC, TH], BF16); uThB = Buf("uTh")
        hdn = FA.alloc([4, TH], BF16); hdnB = Buf("hdn")
        rl = FA.alloc([512]); rlB = Buf("rl")
        utokf = FA.alloc([D], BF16); utokfB = Buf("utok_f")
        tf = FA.alloc([512]); tfB = Buf("tf")
        ffn_bufs = accB + [uThB, hdnB, rlB, utokfB, tfB]
        print("SBUF: persistent %d, mixer %d, ffn %d, phase avail %d" % (ph_lo, M.cur - ph_lo, FA.cur - ph_lo, ph_hi - ph_lo))

        def fence():
            op("gpsimd", "memset", [], mixer_bufs + ffn_bufs, sm[:, 8:9], 0.0)

        dma("sync", cf, cf_d, [], [cfB])
        ident = cf[:, K_ID:K_ID + 128]
        tri = cf[:, K_TRI:K_TRI + 128]
        ones = cf[:, K_ONES:K_ONES + 128]
        hmask = cf[:, K_HM:K_HM + 4]
        bmask = cf[:, K_BM:K_BM + 256]
        retE = cf[:, K_RE:K_RE + 256]
        elast_r = cf[:, K_EL:K_EL + 1]
        invf = cf[:, K_IF:K_IF + 16]
        hm2 = cf[:, K_IF + 16:K_IF + 18]
        op("vector", "tensor_copy", [cfB], [cfB], out=identb, in_=ident)
        dma("sync", cw, convT_d.rearrange("(m p) k -> p m k", p=128), [], [smallB])
        dma("sync", ifb, ifb_d.partition_broadcast(128), [], [smallB])
        dma("sync", wup, wup_d.rearrange("l r c -> r l c"), [], [smallB])

        RA = Arena(big_h, ph_lo, ph_hi)
        NTT = T // 128
        posi = RA.alloc([NTT], I32)
        posf = RA.alloc([NTT])
        ang = RA.alloc([NTT, 16])
        tq = RA.alloc([NTT, 16])
        tqi = RA.alloc([NTT, 16], I32)
        tB = Buf("rot_tmp")
        dma("sync", posi, pos_d, [], [tB])
        op("vector", "tensor_copy", [tB], [tB], out=posf, in_=posi)
        op("vector", "tensor_tensor", [tB, cfB], [tB], out=ang, in0=bc_last(posf, 16), in1=bc_mid(invf, NTT), op=ALU.mult)
        TWO_PI = float(2 * np.pi)
        PI = float(np.pi)

        def make_trig(dst, shift):
            op("vector", "tensor_scalar", [tB], [tB], out=tq, in0=ang, scalar1=shift, scalar2=1.0 / TWO_PI, op0=ALU.add, op1=ALU.mult)
            op("vector", "tensor_copy", [tB], [tB], out=tqi, in_=tq)
            op("vector", "tensor_copy", [tB], [tB], out=tq, in_=tqi)
            op("vector", "scalar_tensor_tensor", [tB], [tB], out=tq, in0=tq, scalar=-TWO_PI, in1=ang, op0=ALU.mult, op1=ALU.add)
            op("vector", "tensor_scalar", [tB], [tB], out=tq, in0=tq, scalar1=shift, scalar2=None, op0=ALU.add)
            op("vector", "tensor_scalar", [tB], [rotB], out=dst, in0=tq, scalar1=PI, scalar2=-TWO_PI, op0=ALU.is_gt, op1=ALU.mult)
            op("vector", "tensor_tensor", [tB, rotB], [tB], out=tq, in0=tq, in1=dst, op=ALU.add)
            op("vector", "tensor_scalar", [tB], [rotB], out=dst, in0=tq, scalar1=-PI, scalar2=TWO_PI, op0=ALU.is_lt, op1=ALU.mult)
            op("vector", "tensor_tensor", [tB, rotB], [tB], out=tq, in0=tq, in1=dst, op=ALU.add)
            op("vector", "tensor_scalar", [tB], [tB], out=tq, in0=tq, scalar1=PI, scalar2=-PI, op0=ALU.min, op1=ALU.max)
            op("scalar", "activation", [tB], [rotB], out=dst, in_=tq, func=AF.Sin)

        make_trig(sinT, 0.0)
        make_trig(cosT, float(np.pi / 2))
        op("gpsimd", "memset", [], [tB] + mixer_bufs + ffn_bufs, sm[:, 8:9], 0.0)

        for l in range(L):
            op("gpsimd", "memset", [], [stB[l]], Cst[l], 0.0)
            op("gpsimd", "memset", [], [stB[l]], Sg[l], 0.0)
            op("gpsimd", "memset", [], [stB[l]], Sr[l], 0.0)
            op("gpsimd", "memset", [], [stB[l]], ctail[l], 0.0)
        op("gpsimd", "memset", [], [vB], v_ext, 1.0)

        def load_mixer_weights(l):
            src = w_in_d[l].rearrange("(c p) n -> p c n", p=128)
            for q in range(4):
                dma("gpsimd", winb[:, 2 * q:2 * q + 2, :], src[:, 2 * q:2 * q + 2, :], [], [winB[q]])
            dma("gpsimd", woutb, w_out_d[l].rearrange("(c p) n -> p c n", p=128), [], [woutB])

        def load_w1(l, g):
            dma("gpsimd", w1v, w_ff1_d[l][:, g * 512:(g + 1) * 512].rearrange("(c p) f -> p c f", p=128), [], [ringB[0]])

        def load_w2(l, g):
            dma("gpsimd", w2v, w_ff2_d[l][g * 512:(g + 1) * 512, :].rearrange("(c p) d -> p c d", p=128), [], [ringB[1]])

        def load_gain(row):
            if row % 2 == 0:
                dma("sync", gbc, gains_d[row:row + 1, :].partition_broadcast(128), [], [gbcB])
            else:
                dma("sync", gbc2, gains_d[row:row + 1, :].partition_broadcast(128), [], [gbc2B])

        def rstd_from(ss_ap, n, out_ap, buf):
            op("scalar", "activation", [buf], [buf], out=out_ap, in_=ss_ap, func=AF.Ln, scale=1.0 / n, bias=EPS)
            op("scalar", "activation", [buf], [buf], out=out_ap, in_=out_ap, func=AF.Exp, scale=-0.5)

        def norm_to_T(src_ap, srcB, utok, utokB, dstT, dstB):
            op("gpsimd", "memset", [], [smB], ss, 0.0)
            op("scalar", "activation", [srcB, smB], [utokB, smB], out=utok, in_=src_ap, func=AF.Square, accum_out=ss)
            rstd_from(ss, D, rstd, smB)
            op("vector", "scalar_tensor_tensor", [srcB, smB, gbcB], [utokB], out=utok, in0=src_ap, scalar=rstd, in1=gbc, op0=ALU.mult, op1=ALU.mult)
            pb, pB = bank()
            pbb = pb.bitcast(BF16)
            for k in range(KC):
                op("tensor", "transpose", [utokB, cfB], [pB], out=pbb[:, k * 128:(k + 1) * 128], in_=utok[:, k * 128:(k + 1) * 128], identity=identb)
            op("scalar", "activation", [pB], [dstB], out=dstT, in_=pbb.rearrange("p (k t) -> p k t", k=KC), func=AF.Copy)

        def post_norm_residual(srcs, srcBs, tmp, tmpB, tt):
            op("gpsimd", "memset", [], [smB], ss2, 0.0)
            for dh in range(2):
                op("scalar", "activation", [srcBs[dh], smB], [tmpB, smB], out=tmp.bitcast(BF16)[:, 0:512], in_=srcs[dh], func=AF.Square, accum_out=ss2[:, dh:dh + 1])
            op("vector", "tensor_tensor", [smB], [smB], out=ss, in0=ss2[:, 0:1], in1=ss2[:, 1:2], op=ALU.add)
            rstd_from(ss, D, rstd, smB)
            for dh in range(2):
                op("vector", "scalar_tensor_tensor", [srcBs[dh], smB, gbc2B], [tmpB], out=tmp, in0=srcs[dh], scalar=rstd, in1=gbc2[:, dh * 512:(dh + 1) * 512], op0=ALU.mult, op1=ALU.mult)
                hs = h[:, tt, dh * 512:(dh + 1) * 512]
                op("gpsimd", "tensor_tensor", [tmpB, hB[tt]], [hB[tt]], out=hs, in0=hs, in1=tmp, op=ALU.add)

        def core(l, c0, v_ap, ktok_ap, ktok_B, Sst, Sbf, elast_ap, elB, gate_ap, ycols):
            sB = stB[l]
            op("gpsimd", "tensor_tensor", [qtB, cfB], [QbdB], out=Qbd, in0=bc_mid(qt, 4), in1=bc_last(hmask, 128), op=ALU.mult)
            Sc, ScB = bank()
            op("tensor", "matmul", [ktB, QbdB], [ScB], Sc, lhsT=kt, rhs=Qbd.rearrange("p h i -> p (h i)"), start=True, stop=True)
            op("vector", "tensor_tensor", [ScB, cfB], [PTB], out=PT, in0=Sc.rearrange("p (h i) -> p h i", h=4), in1=bc_mid(tri, 4), op=ALU.mult)
            op("gpsimd", "tensor_copy", [sB], [sB], out=Sbf, in_=Sst)
            ob, oB = bank()
            for hh in range(4):
                op("tensor", "matmul", [PTB, vgrB], [oB], ob[:, hh * 64:(hh + 1) * 64], lhsT=PT[:, hh, :], rhs=v_ap[:, hh * 64:(hh + 1) * 64], start=(hh == 0), stop=False, skip_group_check=True)
            op("tensor", "matmul", [qtB, sB], [oB], ob[:, 0:256], lhsT=qt, rhs=Sbf, start=False, stop=True, skip_group_check=True)
            o3 = ob[:, 0:256].rearrange("p (h e) -> p h e", h=4)
            tmp = t512[0][:, 0:256]
            tmp3 = tmp.rearrange("p (h e) -> p h e", h=4)
            op("scalar", "activation", [oB], [t512B[0]], out=tmp, in_=ob[:, 0:256], func=AF.Square)
            op("vector", "tensor_reduce", [t512B[0]], [rs4B], out=rs4, in_=tmp3, axis=AX.X, op=ALU.add)
            rstd_from(rs4, 64, rs4, rs4B)
            op("vector", "tensor_tensor", [oB, rs4B], [t512B[0]], out=tmp3, in0=o3, in1=bc_last(rs4, 64), op=ALU.mult)
            op("gpsimd", "tensor_tensor", [t512B[0], gateB], [yB], out=ytile[:, ycols:ycols + 256], in0=tmp, in1=gate_ap, op=ALU.mult)
            ub, uB = bank()
            op("tensor", "matmul", [ktok_B, vgrB], [uB], ub[:, 0:256], lhsT=ktok_ap, rhs=v_ap, start=True, stop=True)
            tU = t512[1][:, 0:256]
            op("vector", "scalar_tensor_tensor", [uB, elB, cfB], [t512B[1]], out=tU, in0=ub[:, 0:256], scalar=elast_ap, in1=bmask, op0=ALU.mult, op1=ALU.mult)
            op("vector", "scalar_tensor_tensor", [sB, elB, t512B[1]], [sB], out=Sst, in0=Sst, scalar=elast_ap, in1=tU, op0=ALU.mult, op1=ALU.add)

        load_mixer_weights(0)
        load_w1(0, 0)
        load_w2(0, 0)
        for part in range(run_parts):
            t0 = part * TH
            xsrc = x_d[t0:t0 + TH, :].rearrange("(t p) d -> p t d", p=128)
            for q in range(NT):
                dma("sync", h[:, q, :], xsrc[:, q, :], [], [hB[q]])

            for l in range(L if stage != 0 else 0):
              try:
                sB = stB[l]
                load_gain(l * 4 + 0)
                load_gain(l * 4 + 1)
                dma("sync", gbb, gb_d[:, l * 128:(l + 1) * 128].partition_broadcast(128), [], [gbbB])
                dma("sync", hnbc, hn_d[l:l + 1, :].partition_broadcast(128), [], [hnB])
                for blk in range(NBLK):
                    for ti in range(4):
                        tt = blk * 4 + ti
                        norm_to_T(h[:, tt, :], hB[tt], ytile, yB, uTb[:, :, ti * 128:(ti + 1) * 128], uTB)
                    op("gpsimd", "tensor_copy", [sB], [xqB], out=xq[:, :, 0:3], in_=ctail[l])
                    bcols = [C_MQ, C_MQ + 128, C_MK, C_MK + 128, C_GQ, C_GK, C_GA]
                    bM = [128, 128, 128, 128, 128, 128, 16]
                    for i in range(7):
                        pb, pB = bank()
                        for k in range(KC):
                            op("tensor", "matmul", [winB[k // 2], uTB], [pB], pb[0:bM[i], :], lhsT=winb[:, k, bcols[i]:bcols[i] + bM[i]], rhs=uTb[:, k, :],
                               start=(k == 0), stop=(k == KC - 1))
                        if i < 4:
                            op("scalar", "activation", [pB], [xqB], out=xq[:, i, 3:515], in_=pb, func=AF.Copy)
                        elif i == 4:
                            op("scalar", "activation", [pB], [gqkB], out=gqk[:, 0, :], in_=pb, func=AF.Copy, scale=float(32 ** -0.5))
                        elif i == 5:
                            op("scalar", "activation", [pB], [gqkB], out=gqk[:, 1, :], in_=pb, func=AF.Copy)
                        else:
                            op("scalar", "activation", [pB], [gaB], out=gaT, in_=pb[0:16, :], func=AF.Copy)
                    for i in range(4):
                        op("gpsimd", "tensor_scalar", [xqB, smallB], [yqB], out=yq[:, i, :], in0=xq[:, i, 3:515], scalar1=cw[:, i, l * 4 + 3:l * 4 + 4], scalar2=None, op0=ALU.mult)
                        for s in (2, 1, 0):
                            op("vector", "scalar_tensor_tensor", [xqB, smallB, yqB], [yqB], out=yq[:, i, :], in0=xq[:, i, s:s + 512], scalar=cw[:, i, l * 4 + s:l * 4 + s + 1],
                               in1=yq[:, i, :], op0=ALU.mult, op1=ALU.add)
                    op("gpsimd", "tensor_copy", [xqB], [sB], out=ctail[l], in_=xq[:, :, 512:515])
                    op("scalar", "activation", [yqB, sB], [xqB], out=eq, in_=yq, func=AF.Exp, scale=-1.0)
                    op("gpsimd", "tensor_scalar", [xqB], [xqB], out=eq, in0=eq, scalar1=1.0, scalar2=None, op0=ALU.add)
                    op("vector", "reciprocal", [xqB], [xqB], out=eq, in_=eq)
                    op("vector", "tensor_tensor", [yqB, xqB], [qkB], out=qkb[:, 0:2, :], in0=yq[:, 0:2, :], in1=eq[:, 0:2, :], op=ALU.mult)
                    op("vector", "scalar_tensor_tensor", [yqB, xqB], [qkB], out=qkb[:, 2:4, :], in0=yq[:, 2:4, :], scalar=0.125, in1=eq[:, 2:4, :], op0=ALU.mult, op1=ALU.mult)

                    ck(1)
                    for ti in range(4):
                        tt = blk * 4 + ti
                        gt = part * NT + tt
                        c0 = ti * 128

                        def amm(c_lo, n):
                            pb, pB = bank()
                            for k in range(KC):
                                op("tensor", "matmul", [winB[k // 2], uTB], [pB], pb[:, 0:n], lhsT=uTb[:, k, c0:c0 + 128], rhs=winb[:, k, c_lo:c_lo + n],
                                   start=(k == 0), stop=(k == KC - 1))
                            return pb, pB
                        pb, pB = amm(C_MV, 512)
                        op("scalar", "activation", [pB], [vB], out=v_ext[:, :, 0:128], in_=pb.rearrange("p (h e) -> p h e", h=4), func=AF.Copy)
                        pb, pB = amm(C_MIF, 8)
                        op("vector", "tensor_tensor", [pB, smallB], [mB], out=ifp, in0=pb[:, 0:8], in1=ifb[:, l * 8:(l + 1) * 8], op=ALU.add)
                        pb, pB = amm(C_MO, 512)
                        op("scalar", "activation", [pB], [t512B[0]], out=t512[0], in_=pb, func=AF.Exp, scale=-1.0)
                        op("gpsimd", "tensor_scalar", [t512B[0]], [t512B[0]], out=t512[0], in0=t512[0], scalar1=1.0, scalar2=None, op0=ALU.add)
                        op("vector", "reciprocal", [t512B[0]], [t512B[0]], out=t512[0], in_=t512[0])
                        op("gpsimd", "tensor_tensor", [t512B[0], hnB], [ogB], out=og, in0=t512[0], in1=hnbc[:, 0:512], op=ALU.mult)
                        pb, pB = amm(C_GV, 256)
                        op("scalar", "activation", [pB], [vgrB], out=vgr[:, 0:256], in_=pb[:, 0:256], func=AF.Copy)
                        pb, pB = amm(C_GG, 512)
                        op("scalar", "activation", [pB], [xgB], out=xg[:, 0:256], in_=pb[:, 0:256], func=AF.Copy)
                        op("scalar", "activation", [pB], [t512B[1]], out=t512[1][:, 0:256], in_=pb[:, 0:256], func=AF.Exp, scale=-1.0)
                        op("scalar", "activation", [pB], [XrB], out=Xr.rearrange("p a b -> p (a b)"), in_=pb[:, 256:512], func=AF.Copy)
                        pb, pB = amm(C_RV, 512)
                        op("scalar", "activation", [pB], [vgrB], out=vgr[:, 256:512], in_=pb[:, 0:256], func=AF.Copy)
                        op("scalar", "activation", [pB], [xgB], out=xg[:, 256:512], in_=pb[:, 256:512], func=AF.Copy)
                        op("scalar", "activation", [pB], [t512B[1]], out=t512[1][:, 256:512], in_=pb[:, 256:512], func=AF.Exp, scale=-1.0)
                        op("gpsimd", "tensor_scalar", [t512B[1]], [t512B[1]], out=t512[1], in0=t512[1], scalar1=1.0, scalar2=None, op0=ALU.add)
                        op("vector", "reciprocal", [t512B[1]], [t512B[1]], out=t512[1], in_=t512[1])
                        op("gpsimd", "tensor_tensor", [xgB, t512B[1]], [gateB], out=gate, in0=xg, in1=t512[1], op=ALU.mult)
                        op("gpsimd", "tensor_tensor", [gateB, hnB], [gateB], out=gate, in0=gate, in1=hnbc[:, 512:1024], op=ALU.mult)

                        ck(2)
                        op("scalar", "activation", [mB], [mB], out=t4, in_=ifp[:, 4:8], func=AF.Exp, scale=-1.0)
                        op("scalar", "activation", [mB], [mB], out=t4, in_=t4, func=AF.Ln, bias=1.0)
                        op("gpsimd", "tensor_scalar", [mB], [mB], out=lf, in0=t4, scalar1=-1.0, scalar2=None, op0=ALU.mult)
                        op("gpsimd", "tensor_tensor", [mB, cfB], [RB], out=R, in0=bc_mid(tri, 4), in1=bc_last(lf, 128), op=ALU.mult)
                        Bb, BbB = bank()
                        op("tensor", "matmul", [RB, cfB], [BbB], Bb, lhsT=ones, rhs=R.rearrange("p h i -> p (h i)"), start=True, stop=True)
                        bj, bjB = bank()
                        op("tensor", "matmul", [mB, cfB], [bjB], bj[:, 0:4], lhsT=tri, rhs=lf, start=True, stop=True)
                        op("vector", "scalar_tensor_tensor", [bjB, mB], [mB], out=aj, in0=bj[:, 0:4], scalar=-1.0, in1=ifp[:, 0:4], op0=ALU.mult, op1=ALU.add)
                        ck(22)
                        Bb3 = Bb.rearrange("p (h i) -> p h i", h=4)
                        for hh in range(4):
                            op("scalar", "activation", [BbB, mB], [DTB], out=DT[:, hh, :], in_=Bb3[:, hh, :], func=AF.Exp, bias=aj[:, hh:hh + 1])
                        op("scalar", "activation", [BbB], [EB], out=E.rearrange("p h i -> p (h i)"), in_=Bb, func=AF.Exp)
                        op("vector", "tensor_tensor", [BbB, mB], [mB], out=wj, in0=Bb3[:, :, 127], in1=aj, op=ALU.add)
                        op("scalar", "activation", [mB], [mB], out=wj, in_=wj, func=AF.Exp)
                        ck(23)
                        Sc, ScB = bank()
                        for mt in range(2):
                            op("gpsimd", "tensor_tensor", [qkB, cfB], [QbdB], out=Qbd[:, 2 * mt:2 * mt + 2, :], in0=bc_mid(qkb[:, mt, c0:c0 + 128], 2),
                               in1=bc_last(hm2, 128), op=ALU.mult)
                        for mt in range(2):
                            op("tensor", "matmul", [qkB, QbdB], [ScB], Sc[:, mt * 256:(mt + 1) * 256], lhsT=qkb[:, 2 + mt, c0:c0 + 128],
                               rhs=Qbd[:, 2 * mt:2 * mt + 2, :].rearrange("p h i -> p (h i)"), start=True, stop=True)
                        tS = t512[2]
                        op("vector", "tensor_tensor", [ScB, cfB], [t512B[2]], out=tS.rearrange("p (h i) -> p h i", h=4), in0=Sc.rearrange("p (h i) -> p h i", h=4),
                           in1=bc_mid(tri, 4), op=ALU.mult)
                        op("gpsimd", "tensor_tensor", [t512B[2], DTB], [PTB], out=PT.rearrange("p h i -> p (h i)"), in0=tS, in1=DT.rearrange("p h i -> p (h i)"), op=ALU.mult)
                        ck(231)
                        for mt in range(2):
                            for hh in range(2):
                                r0 = hh * 64
                                op("gpsimd", "tensor_tensor", [qkB, EB], [qpB], out=qp[r0:r0 + 64, mt, :], in0=qkb[r0:r0 + 64, mt, c0:c0 + 128], in1=E[r0:r0 + 64, 2 * mt + hh, :], op=ALU.mult)
                        ck(24)
                        op("gpsimd", "tensor_copy", [sB], [sB], out=C_bf, in_=Cst[l])
                        O = [bank(), bank()]
                        for hh in range(4):
                            ob, oB = O[hh // 2]
                            r0 = (hh % 2) * 64
                            osl = ob[:, (hh % 2) * 129:(hh % 2) * 129 + 129]
                            op("tensor", "matmul", [PTB, vB], [oB], osl, lhsT=PT[:, hh, :], rhs=v_ext[:, hh, :], start=(hh % 2 == 0), stop=False, skip_group_check=True)
                            op("tensor", "matmul", [qpB, sB], [oB], osl, lhsT=qp[r0:r0 + 64, hh // 2, :], rhs=C_bf[r0:r0 + 64, hh // 2, :], start=False, stop=True, skip_group_check=True)
                        ck(25)
                        for b2 in range(2):
                            ob, oB = O[b2]
                            op("vector", "tensor_copy", [oB], [mB], out=den[:, 2 * b2:2 * b2 + 2], in_=ob[:, 0:258].rearrange("p (h c) -> p h c", h=2)[:, :, 128])
                        op("vector", "tensor_scalar", [mB], [mB], out=t4, in0=den, scalar1=-1.0, scalar2=None, op0=ALU.mult)
                        op("vector", "tensor_tensor", [mB], [mB], out=den, in0=den, in1=t4, op=ALU.max)
                        op("vector", "tensor_scalar", [mB], [mB], out=den, in0=den, scalar1=1.0, scalar2=None, op0=ALU.max)
                        op("vector", "reciprocal", [mB], [mB], out=den, in_=den)
                        op("gpsimd", "memset", [], [mB], ssm, 0.0)
                        for hh in range(4):
                            ob, oB = O[hh // 2]
                            op("scalar", "activation", [oB, mB], [t512B[0], mB], out=t512[0].bitcast(BF16)[:, 0:128], in_=ob[:, (hh % 2) * 129:(hh % 2) * 129 + 128], func=AF.Square,
                               accum_out=ssm[:, hh:hh + 1])
                        op("vector", "tensor_tensor", [mB], [mB], out=fac, in0=den, in1=den, op=ALU.mult)
                        op("vector", "tensor_tensor", [mB], [mB], out=fac, in0=fac, in1=ssm, op=ALU.mult)
                        rstd_from(fac, 128, fac, mB)
                        op("vector", "tensor_tensor", [mB], [mB], out=fac, in0=fac, in1=den, op=ALU.mult)
                        for hh in range(4):
                            ob, oB = O[hh // 2]
                            op("vector", "scalar_tensor_tensor", [oB, mB, ogB], [yB], out=ytile[:, hh * 128:(hh + 1) * 128], in0=ob[:, (hh % 2) * 129:(hh % 2) * 129 + 128],
                               scalar=fac[:, hh:hh + 1], in1=og[:, hh * 128:(hh + 1) * 128], op0=ALU.mult, op1=ALU.mult)
                        ck(26)
                        Kp, KpB = bank()
                        Kpb = Kp.bitcast(BF16)
                        for mt in range(2):
                            op("tensor", "transpose", [qkB, cfB], [KpB], out=Kpb[:, mt * 128:(mt + 1) * 128], in_=qkb[:, 2 + mt, c0:c0 + 128], identity=identb)
                        op("vector", "tensor_tensor", [KpB, mB], [kpB], out=kp.rearrange("p (h d) -> p h d", h=4), in0=Kpb[:, 0:256].rearrange("p (h d) -> p h d", h=4),
                           in1=bc_last(wj, 64), op=ALU.mult)
                        U = [bank(), bank()]
                        for mt in range(2):
                            ub, uB = U[mt]
                            for hh in range(2):
                                op("tensor", "matmul", [kpB, vB], [uB], ub[:, hh * 129:hh * 129 + 129], lhsT=kp[:, mt * 128:(mt + 1) * 128], rhs=v_ext[:, 2 * mt + hh, :], start=True, stop=True)
                            for hh in range(2):
                                r0 = hh * 64
                                cs_ = Cst[l][r0:r0 + 64, mt, :]
                                op("vector", "scalar_tensor_tensor", [uB, EB, sB], [sB], out=cs_, in0=cs_, scalar=E[r0:r0 + 64, 2 * mt + hh, 127:128],
                                   in1=ub[r0:r0 + 64, hh * 129:hh * 129 + 129], op0=ALU.mult, op1=ALU.add)

                        ck(3)
                        Lp, LpB = bank()
                        op("tensor", "matmul", [gaB, smallB], [LpB], Lp[:, 0:128], lhsT=gaT[:, c0:c0 + 128], rhs=wup[:, l, :], start=True, stop=True)
                        op("vector", "tensor_tensor", [LpB, gbbB], [laB], out=la, in0=Lp[:, 0:128], in1=gbb, op=ALU.add)
                        op("scalar", "activation", [laB], [laB], out=la, in_=la, func=AF.Exp, scale=-1.0)
                        op("scalar", "activation", [laB], [laB], out=la, in_=la, func=AF.Ln, bias=1.0)
                        op("gpsimd", "tensor_scalar", [laB], [laB], out=la, in0=la, scalar1=-1.0 / 16.0, scalar2=None, op0=ALU.mult)
                        BT, BTB = bank()
                        op("tensor", "matmul", [laB, cfB], [BTB], BT[:, 0:128], lhsT=la, rhs=tri, start=True, stop=True)
                        op("scalar", "activation", [BTB], [EpB], out=Ep, in_=BT[:, 0:128], func=AF.Exp)
                        op("scalar", "activation", [BTB], [EpB], out=En, in_=BT[:, 0:128], func=AF.Exp, scale=-1.0)
                        op("gpsimd", "tensor_tensor", [gqkB, EpB], [qtB], out=qt, in0=gqk[:, 0, c0:c0 + 128], in1=Ep, op=ALU.mult)
                        op("gpsimd", "tensor_tensor", [gqkB, EpB], [ktB], out=kt, in0=gqk[:, 1, c0:c0 + 128], in1=En, op=ALU.mult)
                        Tp, TpB = bank()
                        Tpb = Tp.bitcast(BF16)
                        op("tensor", "transpose", [ktB, cfB], [TpB], out=Tpb[:, 0:128], in_=kt, identity=identb)
                        op("scalar", "activation", [TpB], [ktokB], out=ktok, in_=Tpb[:, 0:128], func=AF.Copy)
                        core(l, c0, vgr[:, 0:256], ktok, ktokB, Sg[l], Sg_bf, Ep[:, 127:128], EpB, gate[:, 0:256], 512)

                        ck(4)
                        cs = bc_mid(cosT[:, gt, :], 8)
                        sn = bc_mid(sinT[:, gt, :], 8)
                        X1 = Xr[:, :, 0:16]
                        X2 = Xr[:, :, 16:32]
                        op("gpsimd", "tensor_tensor", [XrB, rotB], [rtB], out=rt[0], in0=X1, in1=cs, op=ALU.mult)
                        op("gpsimd", "tensor_tensor", [XrB, rotB], [rtB], out=rt[1], in0=X2, in1=sn, op=ALU.mult)
                        op("gpsimd", "tensor_tensor", [rtB], [XoB], out=Xo[:, :, 0:16], in0=rt[0], in1=rt[1], op=ALU.subtract)
                        op("gpsimd", "tensor_tensor", [XrB, rotB], [rtB], out=rt[0], in0=X1, in1=sn, op=ALU.mult)
                        op("gpsimd", "tensor_tensor", [XrB, rotB], [rtB], out=rt[1], in0=X2, in1=cs, op=ALU.mult)
                        op("gpsimd", "tensor_tensor", [rtB], [XoB], out=Xo[:, :, 16:32], in0=rt[0], in1=rt[1], op=ALU.add)
                        op("gpsimd", "tensor_tensor", [XoB, cfB], [qkrB], out=qkr, in0=Xo.rearrange("p a b -> p (a b)"), in1=retE, op=ALU.mult)
                        Tp2, Tp2B = bank()
                        Tp2b = Tp2.bitcast(BF16)
                        op("tensor", "transpose", [qkrB, cfB], [Tp2B], out=Tp2b[:, 0:128], in_=qkr[:, 0:128], identity=identb)
                        op("tensor", "transpose", [qkrB, cfB], [Tp2B], out=Tp2b[:, 128:256], in_=qkr[:, 128:256], identity=identb)
                        op("scalar", "activation", [Tp2B], [qtB], out=qt, in_=Tp2b[:, 0:128], func=AF.Copy)
                        op("scalar", "activation", [Tp2B], [ktB], out=kt, in_=Tp2b[:, 128:256], func=AF.Copy)
                        core(l, c0, vgr[:, 256:512], qkr[:, 128:256], qkrB, Sr[l], Sr_bf, elast_r, cfB, gate[:, 256:512], 768)

                        ck(5)
                        pb, pB = bank()
                        pbb = pb.bitcast(BF16)
                        for k in range(KC):
                            op("tensor", "transpose", [yB, cfB], [pB], out=pbb[:, k * 128:(k + 1) * 128], in_=ytile[:, k * 128:(k + 1) * 128], identity=identb)
                        op("scalar", "activation", [pB], [yTB], out=yT, in_=pbb.rearrange("p (k t) -> p k t", k=KC), func=AF.Copy)
                        W = [bank(), bank()]
                        for dh in range(2):
                            wb, wB = W[dh]
                            for k in range(KC):
                                op("tensor", "matmul", [yTB, woutB], [wB], wb, lhsT=yT[:, k, :], rhs=woutb[:, k, dh * 512:(dh + 1) * 512], start=(k == 0), stop=(k == KC - 1))
                        post_norm_residual([W[0][0], W[1][0]], [W[0][1], W[1][1]], t512[1], t512B[1], tt)

                ck(6)
                fence()
                load_gain(l * 4 + 2)
                for tt in range(NT):
                    norm_to_T(h[:, tt, :], hB[tt], utokf, utokfB, uTh[:, :, tt * 128:(tt + 1) * 128], uThB)
                load_gain(l * 4 + 3)
                ck(7)
                nxt = None
                if l + 1 < L:
                    nxt = l + 1
                elif part + 1 < run_parts:
                    nxt = 0
                for g in range(NG):
                    for tb in range(TH // 512):
                        for fc in range(4):
                            pb, pB = bank()
                            for k in range(KC):
                                op("tensor", "matmul", [ringB[0], uThB], [pB], pb, lhsT=w1v[:, k, fc * 128:(fc + 1) * 128], rhs=uTh[:, k, tb * 512:(tb + 1) * 512],
                                   start=(k == 0), stop=(k == KC - 1))
                            op("scalar", "activation", [pB], [rlB], out=rl, in_=pb, func=AF.Relu)
                            op("gpsimd", "tensor_tensor", [rlB], [hdnB], out=hdn[:, fc, tb * 512:(tb + 1) * 512], in0=rl, in1=rl, op=ALU.mult)
                    if g + 1 < NG:
                        load_w1(l, g + 1)
                    elif nxt is not None:
                        load_w1(nxt, 0)
                    for tt in range(NT):
                        for dh in range(2):
                            pb, pB = bank()
                            for fc in range(4):
                                op("tensor", "matmul", [hdnB, ringB[1]], [pB], pb, lhsT=hdn[:, fc, tt * 128:(tt + 1) * 128], rhs=w2v[:, fc, dh * 512:(dh + 1) * 512],
                                   start=(fc == 0), stop=(fc == 3))
                            a_ = acc[:, tt, dh * 512:(dh + 1) * 512]
                            if g == 0:
                                op("vector", "tensor_copy", [pB], [accB[tt]], out=a_, in_=pb)
                            else:
                                op("vector", "tensor_tensor", [pB, accB[tt]], [accB[tt]], out=a_, in0=pb, in1=a_, op=ALU.add)
                    if g + 1 < NG:
                        load_w2(l, g + 1)
                    elif nxt is not None:
                        load_w2(nxt, 0)
                    if g == 3 and nxt is not None:
                        load_mixer_weights(nxt)
                for tt in range(NT):
                    post_norm_residual([acc[:, tt, 0:512], acc[:, tt, 512:1024]], [accB[tt], accB[tt]], tf, tfB, tt)
                fence()
              except StopBuild:
                break

            osrc = out_d[t0:t0 + TH, :].rearrange("(t p) d -> p t d", p=128)
            outB = Buf("out")
            for q in range(NT):
                dma("sync", osrc[:, q, :], h[:, q, :], [hB[q]], [outB])
            S.add("sync", lambda e: None, [outB], [])

        S.emit(nc, st)
    return nc, S


def make_consts():
    cf = np.zeros((128, K_END), np.float32)
    p = np.arange(128)
    cf[:, K_ID:K_ID + 128] = np.eye(128, dtype=np.float32)
    tri = (p[:, None] <= p[None, :]).astype(np.float32)
    cf[:, K_TRI:K_TRI + 128] = tri
    cf[:, K_ONES:K_ONES + 128] = 1.0
    cf[:, K_HM:K_HM + 4] = (p[:, None] // 32 == np.arange(4)[None, :]).astype(np.float32)
    cf[:, K_BM:K_BM + 256] = (p[:, None] // 32 == (np.arange(256)[None, :] // 64)).astype(np.float32)
    lg = np.log1p(-np.exp2(-5.0 - np.arange(4, dtype=np.float64)))
    hd = np.arange(128) // 32
    tok = np.arange(128, dtype=np.float64)
    epos = np.exp((tok[:, None] + 1.0) * lg[hd][None, :])
    eneg = np.exp(-(tok[:, None] + 1.0) * lg[hd][None, :]) * (32.0 ** -0.5)
    cf[:, K_RE:K_RE + 128] = epos
    cf[:, K_RE + 128:K_RE + 256] = eneg
    cf[:, K_EL] = np.exp(128.0 * lg[hd])
    invf = (np.float32(10000.0) ** (-np.arange(0, 32, 2, dtype=np.float32) / np.float32(32))).astype(np.float32)
    cf[:, K_IF:K_IF + 16] = invf[None, :]
    cf[:, K_IF + 16:K_IF + 18] = (p[:, None] // 64 == np.arange(2)[None, :]).astype(np.float32)
    return cf


def prepare_inputs(inputs, n_layers=DEPTH):
    L = n_layers
    f32 = np.float32
    g = lambda k: np.asarray(inputs[k])
    gains = np.stack([g("norm_pre_mix")[:L], g("norm_post_mix")[:L], g("norm_pre_ffn")[:L], g("norm_post_ffn")[:L]], axis=1).reshape(L * 4, D).astype(f32)
    convT = np.ascontiguousarray(np.transpose(g("mlstm_conv_w")[:L], (2, 0, 1)).reshape(512, L * 4)).astype(f32)
    ifb = np.concatenate([g("mlstm_i_bias")[:L], g("mlstm_f_bias")[:L]], axis=1).reshape(1, L * 8).astype(f32)
    hn = np.concatenate([g("mlstm_norm")[:L], g("gla_norm")[:L], g("ret_norm")[:L]], axis=1).astype(f32)
    shared = {
        "gains": np.ascontiguousarray(gains),
        "w_in": np.ascontiguousarray(g("w_in")[:L], dtype=f32),
        "w_out": np.ascontiguousarray(g("w_out")[:L], dtype=f32),
        "w_ff1": np.ascontiguousarray(g("w_ff1")[:L], dtype=f32),
        "w_ff2": np.ascontiguousarray(g("w_ff2")[:L], dtype=f32),
        "convT": convT,
        "ifb": np.ascontiguousarray(ifb),
        "hn": np.ascontiguousarray(hn),
        "wup": np.ascontiguousarray(g("gla_w_up")[:L], dtype=f32),
        "gb": np.ascontiguousarray(g("gla_gate_bias")[:L].reshape(1, L * 128), dtype=f32),
        "cf": make_consts(),
    }
    x = g("x")
    pos = g("positions")
    maps = []
    for b in range(x.shape[0]):
        m = dict(shared)
        m["x"] = np.ascontiguousarray(x[b], dtype=f32)
        m["pos"] = np.ascontiguousarray(pos[b].reshape(T // 128, 128).T).astype(np.int32)
        maps.append(m)
    return maps


_CACHE = {}


def kernel(**inputs):
    if "nc" not in _CACHE:
        _CACHE["nc"] = build_program()[0]
    nc = _CACHE["nc"]
    maps = prepare_inputs(inputs)
    res = run_bass_kernel_spmd(nc, maps, core_ids=list(range(len(maps))))
    out = np.stack([np.asarray(r["out"]) for r in res.results], axis=0)
    return out.astype(np.float32)
```

```python
import numpy as np
from contextlib import ExitStack
import concourse.bass as bass
import concourse.mybir as mybir
from concourse.bass_utils import run_bass_kernel_spmd

F32 = mybir.dt.float32
BF16 = mybir.dt.bfloat16
I32 = mybir.dt.int32
AF = mybir.ActivationFunctionType
ALU = mybir.AluOpType
AX = mybir.AxisListType

ENGS = ["tensor", "vector", "scalar", "gpsimd", "sync"]
N_DMA_SEMS = 8

D = 1024
T = 2048
DEPTH = 4
NPART = 2
TH = T // NPART
NT = TH // 128
NBLK = TH // 512
KC = D // 128
INC = 3096
DFF = 4096
NG = 8
EPS = 1e-6
C_MQ, C_MK, C_MV, C_MIF, C_MO = 0, 256, 512, 1024, 1032
C_GQ, C_GK, C_GV, C_GA, C_GG = 1544, 1672, 1800, 2056, 2072
C_RQ, C_RK, C_RV, C_RG = 2328, 2456, 2584, 2840
K_ID, K_TRI, K_ONES, K_HM, K_BM, K_RE, K_EL, K_IF, K_NTRI, K_T16, K_END = 0, 128, 256, 384, 388, 644, 900, 901, 920, 1048, 1176


class Buf:
    __slots__ = ("name", "last_w", "readers", "excl")

    def __init__(self, name="", excl=False):
        self.name = name
        self.last_w = None
        self.readers = []
        self.excl = excl


class Op:
    __slots__ = ("eng", "fn", "deps", "signal", "ordinal", "dma", "dma_sem", "dma_val", "dma_prev")

    def __init__(self, eng, fn, dma):
        self.eng = eng
        self.fn = fn
        self.deps = set()
        self.signal = False
        self.ordinal = None
        self.dma = dma
        self.dma_sem = None
        self.dma_val = None
        self.dma_prev = 0


class Sched:
    def __init__(self, same_engine_sync=True):
        self.ops = {e: [] for e in ENGS}
        self.same_engine_sync = same_engine_sync
        self.n_dma = {e: 0 for e in ENGS}

    def add(self, eng, fn, reads=(), writes=(), dma=False):
        op = Op(eng, fn, dma)
        deps = set()
        for b in reads:
            if b.last_w is not None:
                deps.add(b.last_w)
            if b.excl:
                for r in b.readers:
                    if r.eng != eng:
                        deps.add(r)
        for b in writes:
            if b.last_w is not None:
                deps.add(b.last_w)
            deps.update(b.readers)
        for b in reads:
            b.readers.append(op)
        for b in writes:
            b.last_w = op
            b.readers = []
        deps.discard(op)
        pruned = set()
        for d in deps:
            if d.eng == eng and not d.dma:
                if eng == "tensor" or not self.same_engine_sync:
                    continue
            pruned.add(d)
        op.deps = pruned
        for d in pruned:
            d.signal = True
        if dma:
            n = self.n_dma[eng]
            self.n_dma[eng] = n + 1
            op.dma_sem = n % N_DMA_SEMS
            op.dma_val = 16 * (n // N_DMA_SEMS + 1)
            op.dma_prev = 16 * (n // N_DMA_SEMS)
        self.ops[eng].append(op)
        return op

    def emit(self, nc, stack):
        for e in ENGS:
            c = 0
            for op in self.ops[e]:
                if not op.dma and op.signal:
                    c += 1
                    op.ordinal = c
        esem = {e: stack.enter_context(nc.semaphore("es_" + e)) for e in ENGS}
        dsem = {e: [stack.enter_context(nc.semaphore("ds_%s_%d" % (e, i))) for i in range(N_DMA_SEMS)]
                for e in ENGS if self.n_dma[e] > 0}
        block = stack.enter_context(nc.Block())
        nwaits = [0]

        def run(ename, eng):
            known = {}
            for op in self.ops[ename]:
                waits = {}
                for d in op.deps:
                    if d.dma:
                        s = dsem[d.eng][d.dma_sem]
                        v = d.dma_val
                    else:
                        s = esem[d.eng]
                        v = d.ordinal
                    key = id(s)
                    if key not in waits or waits[key][1] < v:
                        waits[key] = (s, v)
                if op.dma and op.dma_prev > 0:
                    s = dsem[ename][op.dma_sem]
                    key = id(s)
                    if key not in waits or waits[key][1] < op.dma_prev:
                        waits[key] = (s, op.dma_prev)
                for key, (s, v) in waits.items():
                    if known.get(key, 0) >= v:
                        continue
                    known[key] = v
                    eng.wait_ge(s, v)
                    nwaits[0] += 1
                ins = op.fn(eng)
                if ins is None:
                    continue
                if op.dma:
                    ins.then_inc(dsem[ename][op.dma_sem], 16)
                elif op.signal:
                    ins.then_inc(esem[ename], 1)

        @block.sync
        def _(e):
            run("sync", e)

        @block.tensor
        def _(e):
            run("tensor", e)

        @block.vector
        def _(e):
            run("vector", e)

        @block.scalar
        def _(e):
            run("scalar", e)

        @block.gpsimd
        def _(e):
            run("gpsimd", e)
        self.nwaits = nwaits[0]


def _dsize(dt):
    return 2 if dt == BF16 else 4


class Arena:
    def __init__(self, big, lo, hi):
        self.big = big
        self.lo = lo
        self.cur = lo
        self.hi = hi

    def alloc(self, free_shape, dtype=F32, parts=128):
        n = 1
        for s in free_shape:
            n *= s
        nbytes = n * _dsize(dtype)
        nbytes_r = (nbytes + 31) // 32 * 32
        off = self.cur
        self.cur += nbytes_r
        assert self.cur <= self.hi, "arena overflow %d > %d" % (self.cur, self.hi)
        assert nbytes % 4 == 0
        v = self.big[0:parts, off // 4: off // 4 + nbytes // 4]
        if dtype != F32:
            v = v.bitcast(dtype)
        if len(free_shape) == 2:
            v = v.rearrange("p (a b) -> p a b", a=free_shape[0])
        elif len(free_shape) == 3:
            v = v.rearrange("p (a b c) -> p a b c", a=free_shape[0], b=free_shape[1])
        return v


def bc_mid(ap2, n):
    P, F = ap2.shape
    return ap2.rearrange("p (o f) -> p o f", o=1).broadcast_to([P, n, F])


def bc_last(ap2, n):
    P, H = ap2.shape
    return ap2.rearrange("p (h o) -> p h o", o=1).broadcast_to([P, H, n])


def build_program(n_layers=DEPTH, run_parts=NPART, stage=None):
    nc = bass.Bass("TRN2", target_bir_lowering=False)
    L = n_layers

    def din(name, shape, dt=F32):
        return nc.dram_tensor(name, shape, dt, kind="ExternalInput").ap()

    x_d = din("x", [T, D])
    pos_d = din("pos", [128, T // 128], I32)
    gains_d = din("gains", [L * 4, D])
    w_in_d = din("w_in", [L, D, INC])
    w_out_d = din("w_out", [L, D, D])
    w_ff1_d = din("w_ff1", [L, D, DFF])
    w_ff2_d = din("w_ff2", [L, DFF, D])
    convT_d = din("convT", [512, L * 4])
    ifb_d = din("ifb", [1, L * 8])
    hn_d = din("hn", [L, D])
    wup_d = din("wup", [L, 16, 128])
    gb_d = din("gb", [1, L * 128])
    cf_d = din("cf", [128, K_END])
    out_d = nc.dram_tensor("out", [T, D], F32, kind="ExternalOutput").ap()

    S = Sched()

    class StopBuild(Exception):
        pass

    def ck(n):
        if stage is not None and stage == n:
            raise StopBuild()

    def op(eng, method, reads, writes, *args, **kw):
        return S.add(eng, lambda e: getattr(e, method)(*args, **kw), reads, writes)

    def dma(eng, out, in_, reads, writes):
        return S.add(eng, lambda e: e.dma_start(out=out, in_=in_), reads, writes, dma=True)

    st = ExitStack()
    with st:
        total = nc.sbuf_bytes_remaining
        total = (total // 128) * 128 - 128
        big_h = nc.alloc_sbuf_tensor("big", [128, total // 4], F32)
        A = Arena(big_h, 0, total)

        psum = [nc.alloc_psum_tensor("ps%d" % i, [128, 512], F32) for i in range(8)]
        psB = [Buf("ps%d" % i, excl=True) for i in range(8)]
        bank_ctr = [0]

        def bank():
            i = bank_ctr[0] % 8
            bank_ctr[0] += 1
            return psum[i][:], psB[i]

        h = A.alloc([NT, D])
        hB = [Buf("h%d" % i) for i in range(NT)]
        winb = A.alloc([KC, INC], BF16)
        winB = [Buf("win%d" % i) for i in range(4)]
        woutb = A.alloc([KC, D], BF16)
        woutB = Buf("wout")
        ring = [A.alloc([8 * 512], BF16) for _ in range(2)]
        ringB = [Buf("ringA"), Buf("ringB")]
        w1v = ring[0].rearrange("p (c f) -> p c f", c=8)
        w2v = ring[1].rearrange("p (c d) -> p c d", c=4)
        cf = A.alloc([K_END])
        cfB = Buf("cf")
        identb = A.alloc([128], BF16)
        cosT = A.alloc([T // 128, 16])
        sinT = A.alloc([T // 128, 16])
        rotB = Buf("rot")
        cw = A.alloc([4, L * 4])
        ifb = A.alloc([L * 8])
        gbb = A.alloc([128]); gbbB = Buf("gbb")
        wup = A.alloc([L, 128], parts=16)
        smallB = Buf("small")
        Cst = [A.alloc([2, 129]) for _ in range(L)]
        Sg = [A.alloc([256]) for _ in range(L)]
        Sr = [A.alloc([256]) for _ in range(L)]
        ctail = [A.alloc([4, 3]) for _ in range(L)]
        stB = [Buf("state%d" % l) for l in range(L)]
        C_bf = A.alloc([2, 129], BF16)
        Sg_bf = A.alloc([256], BF16)
        Sr_bf = A.alloc([256], BF16)
        v_ext = A.alloc([4, 129], BF16)
        vB = Buf("v_ext")
        gbc = A.alloc([D])
        gbcB = Buf("gbc")
        gbc2 = A.alloc([D])
        gbc2B = Buf("gbc2")
        sm = A.alloc([64])
        smB = Buf("sm")
        ss = sm[:, 0:1]
        ss2 = sm[:, 1:3]
        rstd = sm[:, 3:4]
        ph_lo = A.cur
        ph_hi = total

        M = Arena(big_h, ph_lo, ph_hi)
        hnbc = M.alloc([D]); hnB = Buf("hn")
        uTb = M.alloc([KC, 512], BF16); uTB = Buf("uTb")
        ytile = M.alloc([D], BF16); yB = Buf("ytile")
        xq = M.alloc([4, 515]); xqB = Buf("xq")
        eq = xq[:, :, 0:512]
        yq = M.alloc([4, 512]); yqB = Buf("yq")
        qkb = M.alloc([4, 512], BF16); qkB = Buf("qkb")
        gqk = M.alloc([2, 512], BF16); gqkB = Buf("gqk")
        gaT = M.alloc([512], parts=16); gaB = Buf("gaT")
        ifp = M.alloc([8]); lf = M.alloc([4]); t4 = M.alloc([4]); aj = M.alloc([4]); wj = M.alloc([4])
        den = M.alloc([4]); ssm = M.alloc([4]); fac = M.alloc([4]); mB = Buf("msmall")
        og = M.alloc([512]); ogB = Buf("og")
        t512 = [M.alloc([512]) for _ in range(3)]; t512B = [Buf("t512_%d" % i) for i in range(3)]
        gate = M.alloc([512]); gateB = Buf("gate")
        xg = t512[2]; xgB = t512B[2]
        R = t512[2].rearrange("p (h i) -> p h i", h=4); RB = t512B[2]
        vgr = M.alloc([512], BF16); vgrB = Buf("vgr")
        Xr = M.alloc([8, 32]); XrB = Buf("Xr")
        Xo = M.alloc([8, 32]); XoB = Buf("Xo")
        rt0 = M.alloc([8, 16]); rtB = Buf("rt")
        qkr = M.alloc([256], BF16); qkrB = Buf("qkr")
        DT = M.alloc([4, 128]); DTB = Buf("DT")
        E = M.alloc([4, 128]); EB = Buf("E")
        PT = M.alloc([4, 128], BF16); PTB = Buf("PT")
        qp = M.alloc([2, 128], BF16); qpB = Buf("qp")
        kp = M.alloc([256], BF16); kpB = Buf("kp")
        yT = M.alloc([KC, 128], BF16); yTB = Buf("yT")
        la = t512[2][:, 0:128]; laB = t512B[2]
        Ep = t512[2][:, 128:256]; En = t512[2][:, 256:384]; EpB = t512B[2]
        qt = M.alloc([128], BF16); qtB = Buf("qt")
        kt = M.alloc([128], BF16); ktB = Buf("kt")
        ktok = M.alloc([128], BF16); ktokB = Buf("ktok")
        Qbd = M.alloc([4, 128], BF16); QbdB = Buf("Qbd")
        rs4 = M.alloc([4]); rs4B = Buf("rs4")
        mixer_bufs = [hnB, uTB, yB, xqB, yqB, qkB, gqkB, gaB, mB, ogB, gateB, vgrB, XrB, XoB, rtB, qkrB, DTB, EB, PTB,
                      qpB, kpB, yTB, qtB, ktB, ktokB, QbdB, rs4B] + t512B

        FA = Arena(big_h, ph_lo, ph_hi)
        acc = FA.alloc([NT, D]); accB = [Buf("acc%d" % i) for i in range(NT)]
        uTh = FA.alloc([KC, TH], BF16); uThB = Buf("uTh")
        hdn = FA.alloc([4, TH], BF16); hdnB = Buf("hdn")
        rl = FA.alloc([512]); rlB = Buf("rl")
        utokf = FA.alloc([D], BF16); utokfB = Buf("utok_f")
        tf = FA.alloc([512]); tfB = Buf("tf")
        ffn_bufs = accB + [uThB, hdnB, rlB, utokfB, tfB]
        print("SBUF: persistent %d, mixer %d, ffn %d, phase avail %d" % (ph_lo, M.cur - ph_lo, FA.cur - ph_lo, ph_hi - ph_lo))

        def fence():
            op("gpsimd", "memset", [], mixer_bufs + ffn_bufs, sm[:, 8:9], 0.0)

        dma("sync", cf, cf_d, [], [cfB])
        ident = cf[:, K_ID:K_ID + 128]
        tri = cf[:, K_TRI:K_TRI + 128]
        ones = cf[:, K_ONES:K_ONES + 128]
        hmask = cf[:, K_HM:K_HM + 4]
        bmask = cf[:, K_BM:K_BM + 256]
        retE = cf[:, K_RE:K_RE + 256]
        elast_r = cf[:, K_EL:K_EL + 1]
        invf = cf[:, K_IF:K_IF + 16]
        hm2 = cf[:, K_IF + 16:K_IF + 18]
        ntri = cf[:, K_NTRI:K_NTRI + 128]
        tri16 = cf[:, K_T16:K_T16 + 128]
        op("vector", "tensor_copy", [cfB], [cfB], out=identb, in_=ident)
        dma("sync", cw, convT_d.rearrange("(m p) k -> p m k", p=128), [], [smallB])
        dma("sync", ifb, ifb_d.partition_broadcast(128), [], [smallB])
        dma("sync", wup, wup_d.rearrange("l r c -> r l c"), [], [smallB])

        RA = Arena(big_h, ph_lo, ph_hi)
        NTT = T // 128
        posi = RA.alloc([NTT], I32)
        posf = RA.alloc([NTT])
        ang = RA.alloc([NTT, 16])
        tq = RA.alloc([NTT, 16])
        tqi = RA.alloc([NTT, 16], I32)
        tB = Buf("rot_tmp")
        dma("sync", posi, pos_d, [], [tB])
        op("vector", "tensor_copy", [tB], [tB], out=posf, in_=posi)
        op("vector", "tensor_tensor", [tB, cfB], [tB], out=ang, in0=bc_last(posf, 16), in1=bc_mid(invf, NTT), op=ALU.mult)
        TWO_PI = float(2 * np.pi)
        PI = float(np.pi)

        def make_trig(dst, shift):
            op("vector", "tensor_scalar", [tB], [tB], out=tq, in0=ang, scalar1=shift, scalar2=1.0 / TWO_PI, op0=ALU.add, op1=ALU.mult)
            op("vector", "tensor_copy", [tB], [tB], out=tqi, in_=tq)
            op("vector", "tensor_copy", [tB], [tB], out=tq, in_=tqi)
            op("vector", "scalar_tensor_tensor", [tB], [tB], out=tq, in0=tq, scalar=-TWO_PI, in1=ang, op0=ALU.mult, op1=ALU.add)
            op("vector", "tensor_scalar", [tB], [tB], out=tq, in0=tq, scalar1=shift, scalar2=None, op0=ALU.add)
            op("vector", "tensor_scalar", [tB], [rotB], out=dst, in0=tq, scalar1=PI, scalar2=-TWO_PI, op0=ALU.is_gt, op1=ALU.mult)
            op("vector", "tensor_tensor", [tB, rotB], [tB], out=tq, in0=tq, in1=dst, op=ALU.add)
            op("vector", "tensor_scalar", [tB], [rotB], out=dst, in0=tq, scalar1=-PI, scalar2=TWO_PI, op0=ALU.is_lt, op1=ALU.mult)
            op("vector", "tensor_tensor", [tB, rotB], [tB], out=tq, in0=tq, in1=dst, op=ALU.add)
            op("vector", "tensor_scalar", [tB], [tB], out=tq, in0=tq, scalar1=PI, scalar2=-PI, op0=ALU.min, op1=ALU.max)
            op("scalar", "activation", [tB], [rotB], out=dst, in_=tq, func=AF.Sin)

        make_trig(sinT, 0.0)
        make_trig(cosT, float(np.pi / 2))
        op("gpsimd", "memset", [], [tB] + mixer_bufs + ffn_bufs, sm[:, 8:9], 0.0)

        for l in range(L):
            op("gpsimd", "memset", [], [stB[l]], Cst[l], 0.0)
            op("gpsimd", "memset", [], [stB[l]], Sg[l], 0.0)
            op("gpsimd", "memset", [], [stB[l]], Sr[l], 0.0)
            op("gpsimd", "memset", [], [stB[l]], ctail[l], 0.0)
        op("gpsimd", "memset", [], [vB], v_ext, 1.0)

        def load_mixer_weights(l):
            src = w_in_d[l].rearrange("(c p) n -> p c n", p=128)
            for q in range(4):
                dma("gpsimd", winb[:, 2 * q:2 * q + 2, :], src[:, 2 * q:2 * q + 2, :], [], [winB[q]])
            dma("gpsimd", woutb, w_out_d[l].rearrange("(c p) n -> p c n", p=128), [], [woutB])

        def load_w1(l, g):
            dma("gpsimd", w1v, w_ff1_d[l][:, g * 512:(g + 1) * 512].rearrange("(c p) f -> p c f", p=128), [], [ringB[0]])

        def load_w2(l, g):
            dma("gpsimd", w2v, w_ff2_d[l][g * 512:(g + 1) * 512, :].rearrange("(c p) d -> p c d", p=128), [], [ringB[1]])

        def load_gain(row):
            if row % 2 == 0:
                dma("sync", gbc, gains_d[row:row + 1, :].partition_broadcast(128), [], [gbcB])
            else:
                dma("sync", gbc2, gains_d[row:row + 1, :].partition_broadcast(128), [], [gbc2B])

        def rstd_from(ss_ap, n, out_ap, buf):
            op("scalar", "activation", [buf], [buf], out=out_ap, in_=ss_ap, func=AF.Ln, scale=1.0 / n, bias=EPS)
            op("scalar", "activation", [buf], [buf], out=out_ap, in_=out_ap, func=AF.Exp, scale=-0.5)

        def norm_to_T(src_ap, srcB, utok, utokB, dstT, dstB):
            op("gpsimd", "memset", [], [smB], ss, 0.0)
            op("scalar", "activation", [srcB, smB], [utokB, smB], out=utok, in_=src_ap, func=AF.Square, accum_out=ss)
            rstd_from(ss, D, rstd, smB)
            op("vector", "scalar_tensor_tensor", [srcB, smB, gbcB], [utokB], out=utok, in0=src_ap, scalar=rstd, in1=gbc, op0=ALU.mult, op1=ALU.mult)
            pb, pB = bank()
            pbb = pb.bitcast(BF16)
            for k in range(KC):
                op("tensor", "transpose", [utokB, cfB], [pB], out=pbb[:, k * 128:(k + 1) * 128], in_=utok[:, k * 128:(k + 1) * 128], identity=identb)
            op("scalar", "activation", [pB], [dstB], out=dstT, in_=pbb.rearrange("p (k t) -> p k t", k=KC), func=AF.Copy)

        def post_norm_residual(srcs, srcBs, tmp, tmpB, tt):
            op("gpsimd", "memset", [], [smB], ss2, 0.0)
            for dh in range(2):
                op("scalar", "activation", [srcBs[dh], smB], [tmpB, smB], out=tmp.bitcast(BF16)[:, 0:512], in_=srcs[dh], func=AF.Square, accum_out=ss2[:, dh:dh + 1])
            op("vector", "tensor_tensor", [smB], [smB], out=ss, in0=ss2[:, 0:1], in1=ss2[:, 1:2], op=ALU.add)
            rstd_from(ss, D, rstd, smB)
            for dh in range(2):
                op("vector", "scalar_tensor_tensor", [srcBs[dh], smB, gbc2B], [tmpB], out=tmp, in0=srcs[dh], scalar=rstd, in1=gbc2[:, dh * 512:(dh + 1) * 512], op0=ALU.mult, op1=ALU.mult)
                hs = h[:, tt, dh * 512:(dh + 1) * 512]
                op("vector", "tensor_tensor", [tmpB, hB[tt]], [hB[tt]], out=hs, in0=hs, in1=tmp, op=ALU.add)

        def core(l, c0, v_ap, ktok_ap, ktok_B, Sst, Sbf, elast_ap, elB, gate_ap, ycols):
            sB = stB[l]
            op("gpsimd", "tensor_tensor", [qtB, cfB], [QbdB], out=Qbd, in0=bc_mid(qt, 4), in1=bc_last(hmask, 128), op=ALU.mult)
            Sc, ScB = bank()
            op("tensor", "matmul", [ktB, QbdB], [ScB], Sc, lhsT=kt, rhs=Qbd.rearrange("p h i -> p (h i)"), start=True, stop=True)
            op("vector", "tensor_tensor", [ScB, cfB], [PTB], out=PT, in0=Sc.rearrange("p (h i) -> p h i", h=4), in1=bc_mid(tri, 4), op=ALU.mult)
            ob, oB = bank()
            for hh in range(4):
                op("tensor", "matmul", [PTB, vgrB], [oB], ob[:, hh * 64:(hh + 1) * 64], lhsT=PT[:, hh, :], rhs=v_ap[:, hh * 64:(hh + 1) * 64], start=(hh == 0), stop=False, skip_group_check=True)
            op("tensor", "matmul", [qtB, sB], [oB], ob[:, 0:256], lhsT=qt, rhs=Sbf, start=False, stop=True, skip_group_check=True)
            o3 = ob[:, 0:256].rearrange("p (h e) -> p h e", h=4)
            tmp = t512[0][:, 0:256]
            tmp3 = tmp.rearrange("p (h e) -> p h e", h=4)
            op("scalar", "activation", [oB], [t512B[0]], out=tmp, in_=ob[:, 0:256], func=AF.Square)
            op("vector", "tensor_reduce", [t512B[0]], [rs4B], out=rs4, in_=tmp3, axis=AX.X, op=ALU.add)
            rstd_from(rs4, 64, rs4, rs4B)
            op("vector", "tensor_tensor", [oB, rs4B], [t512B[0]], out=tmp3, in0=o3, in1=bc_last(rs4, 64), op=ALU.mult)
            op("gpsimd", "tensor_tensor", [t512B[0], gateB], [yB], out=ytile[:, ycols:ycols + 256], in0=tmp, in1=gate_ap, op=ALU.mult)
            ub, uB = bank()
            op("tensor", "matmul", [ktok_B, vgrB], [uB], ub[:, 0:256], lhsT=ktok_ap, rhs=v_ap, start=True, stop=True)
            tU = t512[1][:, 0:256]
            op("vector", "scalar_tensor_tensor", [uB, elB, cfB], [t512B[1]], out=tU, in0=ub[:, 0:256], scalar=elast_ap, in1=bmask, op0=ALU.mult, op1=ALU.mult)
            op("vector", "scalar_tensor_tensor", [sB, elB, t512B[1]], [sB], out=Sst, in0=Sst, scalar=elast_ap, in1=tU, op0=ALU.mult, op1=ALU.add)
            op("gpsimd", "tensor_copy", [sB], [sB], out=Sbf, in_=Sst)

        load_mixer_weights(0)
        load_w1(0, 0)
        load_w2(0, 0)
        for part in range(run_parts):
            t0 = part * TH
            xsrc = x_d[t0:t0 + TH, :].rearrange("(t p) d -> p t d", p=128)
            for q in range(NT):
                dma("sync", h[:, q, :], xsrc[:, q, :], [], [hB[q]])

            for l in range(L if stage != 0 else 0):
              try:
                sB = stB[l]
                load_gain(l * 4 + 0)
                load_gain(l * 4 + 1)
                op("gpsimd", "tensor_copy", [sB], [sB], out=C_bf, in_=Cst[l])
                op("gpsimd", "tensor_copy", [sB], [sB], out=Sg_bf, in_=Sg[l])
                op("gpsimd", "tensor_copy", [sB], [sB], out=Sr_bf, in_=Sr[l])
                dma("sync", gbb, gb_d[:, l * 128:(l + 1) * 128].partition_broadcast(128), [], [gbbB])
                dma("sync", hnbc, hn_d[l:l + 1, :].partition_broadcast(128), [], [hnB])
                for blk in range(NBLK):
                    for ti in range(4):
                        tt = blk * 4 + ti
                        norm_to_T(h[:, tt, :], hB[tt], ytile, yB, uTb[:, :, ti * 128:(ti + 1) * 128], uTB)
                    op("gpsimd", "tensor_copy", [sB], [xqB], out=xq[:, :, 0:3], in_=ctail[l])
                    bcols = [C_MQ, C_MQ + 128, C_MK, C_MK + 128, C_GQ, C_GK, C_GA]
                    bM = [128, 128, 128, 128, 128, 128, 16]
                    for i in range(7):
                        pb, pB = bank()
                        for k in range(KC):
                            op("tensor", "matmul", [winB[k // 2], uTB], [pB], pb[0:bM[i], :], lhsT=winb[:, k, bcols[i]:bcols[i] + bM[i]], rhs=uTb[:, k, :],
                               start=(k == 0), stop=(k == KC - 1))
                        if i < 4:
                            op("scalar", "activation", [pB], [xqB], out=xq[:, i, 3:515], in_=pb, func=AF.Copy)
                        elif i == 4:
                            op("scalar", "activation", [pB], [gqkB], out=gqk[:, 0, :], in_=pb, func=AF.Copy, scale=float(32 ** -0.5))
                        elif i == 5:
                            op("scalar", "activation", [pB], [gqkB], out=gqk[:, 1, :], in_=pb, func=AF.Copy)
                        else:
                            op("scalar", "activation", [pB], [gaB], out=gaT, in_=pb[0:16, :], func=AF.Copy)
                    for i in range(4):
                        op("scalar", "activation", [xqB, smallB], [yqB], out=yq[:, i, :], in_=xq[:, i, 3:515], func=AF.Copy, scale=cw[:, i, l * 4 + 3:l * 4 + 4])
                        for s in (2, 1, 0):
                            op("vector", "scalar_tensor_tensor", [xqB, smallB, yqB], [yqB], out=yq[:, i, :], in0=xq[:, i, s:s + 512], scalar=cw[:, i, l * 4 + s:l * 4 + s + 1],
                               in1=yq[:, i, :], op0=ALU.mult, op1=ALU.add)
                    op("gpsimd", "tensor_copy", [xqB], [sB], out=ctail[l], in_=xq[:, :, 512:515])
                    op("scalar", "activation", [yqB, sB], [xqB], out=eq, in_=yq, func=AF.Exp, scale=-1.0)
                    op("scalar", "activation", [xqB], [xqB], out=eq, in_=eq, func=AF.Ln, bias=1.0)
                    op("scalar", "activation", [xqB], [xqB], out=eq, in_=eq, func=AF.Exp, scale=-1.0)
                    op("vector", "tensor_tensor", [yqB, xqB], [qkB], out=qkb[:, 0:2, :], in0=yq[:, 0:2, :], in1=eq[:, 0:2, :], op=ALU.mult)
                    op("vector", "scalar_tensor_tensor", [yqB, xqB], [qkB], out=qkb[:, 2:4, :], in0=yq[:, 2:4, :], scalar=0.125, in1=eq[:, 2:4, :], op0=ALU.mult, op1=ALU.mult)

                    ck(1)
                    for ti in range(4):
                        tt = blk * 4 + ti
                        gt = part * NT + tt
                        c0 = ti * 128

                        def amm(c_lo, n):
                            pb, pB = bank()
                            for k in range(KC):
                                op("tensor", "matmul", [winB[k // 2], uTB], [pB], pb[:, 0:n], lhsT=uTb[:, k, c0:c0 + 128], rhs=winb[:, k, c_lo:c_lo + n],
                                   start=(k == 0), stop=(k == KC - 1))
                            return pb, pB
                        pb, pB = amm(C_MV, 512)
                        op("scalar", "activation", [pB], [vB], out=v_ext[:, :, 0:128], in_=pb.rearrange("p (h e) -> p h e", h=4), func=AF.Copy)
                        pb, pB = amm(C_MIF, 8)
                        op("vector", "tensor_tensor", [pB, smallB], [mB], out=ifp, in0=pb[:, 0:8], in1=ifb[:, l * 8:(l + 1) * 8], op=ALU.add)
                        pb, pB = amm(C_MO, 512)
                        op("scalar", "activation", [pB], [t512B[0]], out=t512[0], in_=pb, func=AF.Exp, scale=-1.0)
                        op("scalar", "activation", [t512B[0]], [t512B[0]], out=t512[0], in_=t512[0], func=AF.Ln, bias=1.0)
                        op("scalar", "activation", [t512B[0]], [t512B[0]], out=t512[0], in_=t512[0], func=AF.Exp, scale=-1.0)
                        op("vector", "tensor_tensor", [t512B[0], hnB], [ogB], out=og, in0=t512[0], in1=hnbc[:, 0:512], op=ALU.mult)
                        pb, pB = amm(C_GV, 256)
                        op("scalar", "activation", [pB], [vgrB], out=vgr[:, 0:256], in_=pb[:, 0:256], func=AF.Copy)
                        pb, pB = amm(C_GG, 512)
                        op("scalar", "activation", [pB], [xgB], out=xg[:, 0:256], in_=pb[:, 0:256], func=AF.Copy)
                        op("scalar", "activation", [pB], [t512B[1]], out=t512[1][:, 0:256], in_=pb[:, 0:256], func=AF.Exp, scale=-1.0)
                        op("scalar", "activation", [pB], [XrB], out=Xr.rearrange("p a b -> p (a b)"), in_=pb[:, 256:512], func=AF.Copy)
                        pb, pB = amm(C_RV, 512)
                        op("scalar", "activation", [pB], [vgrB], out=vgr[:, 256:512], in_=pb[:, 0:256], func=AF.Copy)
                        op("scalar", "activation", [pB], [xgB], out=xg[:, 256:512], in_=pb[:, 256:512], func=AF.Copy)
                        op("scalar", "activation", [pB], [t512B[1]], out=t512[1][:, 256:512], in_=pb[:, 256:512], func=AF.Exp, scale=-1.0)
                        op("gpsimd", "tensor_tensor", [xgB, hnB], [xgB], out=xg, in0=xg, in1=hnbc[:, 512:1024], op=ALU.mult)
                        op("scalar", "activation", [t512B[1]], [t512B[1]], out=t512[1], in_=t512[1], func=AF.Ln, bias=1.0)
                        op("scalar", "activation", [t512B[1]], [t512B[1]], out=t512[1], in_=t512[1], func=AF.Exp, scale=-1.0)
                        op("vector", "tensor_tensor", [xgB, t512B[1]], [gateB], out=gate, in0=xg, in1=t512[1], op=ALU.mult)

                        cs = bc_mid(cosT[:, gt, :], 8)
                        sn = bc_mid(sinT[:, gt, :], 8)
                        X1 = Xr[:, :, 0:16]
                        X2 = Xr[:, :, 16:32]
                        op("gpsimd", "tensor_tensor", [XrB, rotB], [rtB], out=rt0, in0=X2, in1=sn, op=ALU.mult)
                        op("gpsimd", "tensor_tensor", [XrB, rotB], [XoB], out=Xo[:, :, 0:16], in0=X1, in1=cs, op=ALU.mult)
                        op("gpsimd", "tensor_tensor", [rtB, XoB], [XoB], out=Xo[:, :, 0:16], in0=Xo[:, :, 0:16], in1=rt0, op=ALU.subtract)
                        op("gpsimd", "tensor_tensor", [XrB, rotB], [rtB], out=rt0, in0=X1, in1=sn, op=ALU.mult)
                        op("gpsimd", "tensor_tensor", [XrB, rotB, XoB], [XoB], out=Xo[:, :, 16:32], in0=X2, in1=cs, op=ALU.mult)
                        op("gpsimd", "tensor_tensor", [rtB, XoB], [XoB], out=Xo[:, :, 16:32], in0=Xo[:, :, 16:32], in1=rt0, op=ALU.add)
                        op("gpsimd", "tensor_tensor", [XoB, cfB], [qkrB], out=qkr, in0=Xo.rearrange("p a b -> p (a b)"), in1=retE, op=ALU.mult)
                        ck(2)
                        op("scalar", "activation", [mB], [mB], out=t4, in_=ifp[:, 4:8], func=AF.Exp, scale=-1.0)
                        op("scalar", "activation", [mB], [mB], out=t4, in_=t4, func=AF.Ln, bias=1.0)
                        op("vector", "tensor_tensor", [mB, cfB], [RB], out=R, in0=bc_mid(ntri, 4), in1=bc_last(t4, 128), op=ALU.mult)
                        Bb, BbB = bank()
                        op("tensor", "matmul", [RB, cfB], [BbB], Bb, lhsT=ones, rhs=R.rearrange("p h i -> p (h i)"), start=True, stop=True)
                        bj, bjB = bank()
                        op("tensor", "matmul", [mB, cfB], [bjB], bj[:, 0:4], lhsT=ntri, rhs=t4, start=True, stop=True)
                        op("vector", "scalar_tensor_tensor", [bjB, mB], [mB], out=aj, in0=bj[:, 0:4], scalar=-1.0, in1=ifp[:, 0:4], op0=ALU.mult, op1=ALU.add)
                        ck(22)
                        Bb3 = Bb.rearrange("p (h i) -> p h i", h=4)
                        for hh in range(4):
                            op("scalar", "activation", [BbB, mB], [DTB], out=DT[:, hh, :], in_=Bb3[:, hh, :], func=AF.Exp, bias=aj[:, hh:hh + 1])
                        op("scalar", "activation", [BbB], [EB], out=E.rearrange("p h i -> p (h i)"), in_=Bb, func=AF.Exp)
                        op("vector", "tensor_tensor", [BbB, mB], [mB], out=wj, in0=Bb3[:, :, 127], in1=aj, op=ALU.add)
                        op("scalar", "activation", [mB], [mB], out=wj, in_=wj, func=AF.Exp)
                        ck(23)
                        Sc, ScB = bank()
                        for mt in range(2):
                            op("gpsimd", "tensor_tensor", [qkB, cfB], [QbdB], out=Qbd[:, 2 * mt:2 * mt + 2, :], in0=bc_mid(qkb[:, mt, c0:c0 + 128], 2),
                               in1=bc_last(hm2, 128), op=ALU.mult)
                        for mt in range(2):
                            op("tensor", "matmul", [qkB, QbdB], [ScB], Sc[:, mt * 256:(mt + 1) * 256], lhsT=qkb[:, 2 + mt, c0:c0 + 128],
                               rhs=Qbd[:, 2 * mt:2 * mt + 2, :].rearrange("p h i -> p (h i)"), start=True, stop=True)
                        tS = t512[2]
                        op("vector", "tensor_tensor", [ScB, cfB], [t512B[2]], out=tS.rearrange("p (h i) -> p h i", h=4), in0=Sc.rearrange("p (h i) -> p h i", h=4),
                           in1=bc_mid(tri, 4), op=ALU.mult)
                        op("vector", "tensor_tensor", [t512B[2], DTB], [PTB], out=PT.rearrange("p h i -> p (h i)"), in0=tS, in1=DT.rearrange("p h i -> p (h i)"), op=ALU.mult)
                        ck(231)
                        for mt in range(2):
                            for hh in range(2):
                                r0 = hh * 64
                                op("gpsimd", "tensor_tensor", [qkB, EB], [qpB], out=qp[r0:r0 + 64, mt, :], in0=qkb[r0:r0 + 64, mt, c0:c0 + 128], in1=E[r0:r0 + 64, 2 * mt + hh, :], op=ALU.mult)
                        ck(24)
                        O = [bank(), bank()]
                        for hh in range(4):
                            ob, oB = O[hh // 2]
                            r0 = (hh % 2) * 64
                            osl = ob[:, (hh % 2) * 129:(hh % 2) * 129 + 129]
                            op("tensor", "matmul", [PTB, vB], [oB], osl, lhsT=PT[:, hh, :], rhs=v_ext[:, hh, :], start=(hh % 2 == 0), stop=False, skip_group_check=True)
                            op("tensor", "matmul", [qpB, sB], [oB], osl, lhsT=qp[r0:r0 + 64, hh // 2, :], rhs=C_bf[r0:r0 + 64, hh // 2, :], start=False, stop=True, skip_group_check=True)
                        ck(25)
                        for b2 in range(2):
                            ob, oB = O[b2]
                            op("vector", "tensor_copy", [oB], [mB], out=den[:, 2 * b2:2 * b2 + 2], in_=ob[:, 0:258].rearrange("p (h c) -> p h c", h=2)[:, :, 128])
                        op("vector", "tensor_scalar", [mB], [mB], out=t4, in0=den, scalar1=-1.0, scalar2=None, op0=ALU.mult)
                        op("vector", "tensor_tensor", [mB], [mB], out=den, in0=den, in1=t4, op=ALU.max)
                        op("vector", "tensor_scalar", [mB], [mB], out=den, in0=den, scalar1=1.0, scalar2=None, op0=ALU.max)
                        op("vector", "reciprocal", [mB], [mB], out=den, in_=den)
                        op("gpsimd", "memset", [], [mB], ssm, 0.0)
                        for hh in range(4):
                            ob, oB = O[hh // 2]
                            op("scalar", "activation", [oB, mB], [t512B[0], mB], out=t512[0].bitcast(BF16)[:, 0:128], in_=ob[:, (hh % 2) * 129:(hh % 2) * 129 + 128], func=AF.Square,
                               accum_out=ssm[:, hh:hh + 1])
                        op("vector", "tensor_tensor", [mB], [mB], out=fac, in0=den, in1=den, op=ALU.mult)
                        op("vector", "tensor_tensor", [mB], [mB], out=fac, in0=fac, in1=ssm, op=ALU.mult)
                        rstd_from(fac, 128, fac, mB)
                        op("vector", "tensor_tensor", [mB], [mB], out=fac, in0=fac, in1=den, op=ALU.mult)
                        for hh in range(4):
                            ob, oB = O[hh // 2]
                            op("vector", "scalar_tensor_tensor", [oB, mB, ogB], [yB], out=ytile[:, hh * 128:(hh + 1) * 128], in0=ob[:, (hh % 2) * 129:(hh % 2) * 129 + 128],
                               scalar=fac[:, hh:hh + 1], in1=og[:, hh * 128:(hh + 1) * 128], op0=ALU.mult, op1=ALU.mult)
                        ck(26)
                        Kp, KpB = bank()
                        Kpb = Kp.bitcast(BF16)
                        for mt in range(2):
                            op("tensor", "transpose", [qkB, cfB], [KpB], out=Kpb[:, mt * 128:(mt + 1) * 128], in_=qkb[:, 2 + mt, c0:c0 + 128], identity=identb)
                        op("vector", "tensor_tensor", [KpB, mB], [kpB], out=kp.rearrange("p (h d) -> p h d", h=4), in0=Kpb[:, 0:256].rearrange("p (h d) -> p h d", h=4),
                           in1=bc_last(wj, 64), op=ALU.mult)
                        U = [bank(), bank()]
                        for mt in range(2):
                            ub, uB = U[mt]
                            for hh in range(2):
                                op("tensor", "matmul", [kpB, vB], [uB], ub[:, hh * 129:hh * 129 + 129], lhsT=kp[:, mt * 128:(mt + 1) * 128], rhs=v_ext[:, 2 * mt + hh, :], start=True, stop=True)
                            for hh in range(2):
                                r0 = hh * 64
                                cs_ = Cst[l][r0:r0 + 64, mt, :]
                                op("vector", "scalar_tensor_tensor", [uB, EB, sB], [sB], out=cs_, in0=cs_, scalar=E[r0:r0 + 64, 2 * mt + hh, 127:128],
                                   in1=ub[r0:r0 + 64, hh * 129:hh * 129 + 129], op0=ALU.mult, op1=ALU.add)

                        ck(3)
                        op("gpsimd", "tensor_copy", [sB], [sB], out=C_bf, in_=Cst[l])
                        Lp, LpB = bank()
                        op("tensor", "matmul", [gaB, smallB], [LpB], Lp[:, 0:128], lhsT=gaT[:, c0:c0 + 128], rhs=wup[:, l, :], start=True, stop=True)
                        op("vector", "tensor_tensor", [LpB, gbbB], [laB], out=la, in0=Lp[:, 0:128], in1=gbb, op=ALU.add)
                        op("scalar", "activation", [laB], [laB], out=la, in_=la, func=AF.Exp, scale=-1.0)
                        op("scalar", "activation", [laB], [laB], out=la, in_=la, func=AF.Ln, bias=1.0)
                        BT, BTB = bank()
                        op("tensor", "matmul", [laB, cfB], [BTB], BT[:, 0:128], lhsT=la, rhs=tri16, start=True, stop=True)
                        op("scalar", "activation", [BTB], [EpB], out=Ep, in_=BT[:, 0:128], func=AF.Exp)
                        op("scalar", "activation", [BTB], [EpB], out=En, in_=BT[:, 0:128], func=AF.Exp, scale=-1.0)
                        op("gpsimd", "tensor_tensor", [gqkB, EpB], [qtB], out=qt, in0=gqk[:, 0, c0:c0 + 128], in1=Ep, op=ALU.mult)
                        op("gpsimd", "tensor_tensor", [gqkB, EpB], [ktB], out=kt, in0=gqk[:, 1, c0:c0 + 128], in1=En, op=ALU.mult)
                        Tp, TpB = bank()
                        Tpb = Tp.bitcast(BF16)
                        op("tensor", "transpose", [ktB, cfB], [TpB], out=Tpb[:, 0:128], in_=kt, identity=identb)
                        op("scalar", "activation", [TpB], [ktokB], out=ktok, in_=Tpb[:, 0:128], func=AF.Copy)
                        core(l, c0, vgr[:, 0:256], ktok, ktokB, Sg[l], Sg_bf, Ep[:, 127:128], EpB, gate[:, 0:256], 512)

                        ck(4)
                        Tp2, Tp2B = bank()
                        Tp2b = Tp2.bitcast(BF16)
                        op("tensor", "transpose", [qkrB, cfB], [Tp2B], out=Tp2b[:, 0:128], in_=qkr[:, 0:128], identity=identb)
                        op("tensor", "transpose", [qkrB, cfB], [Tp2B], out=Tp2b[:, 128:256], in_=qkr[:, 128:256], identity=identb)
                        op("scalar", "activation", [Tp2B], [qtB], out=qt, in_=Tp2b[:, 0:128], func=AF.Copy)
                        op("scalar", "activation", [Tp2B], [ktB], out=kt, in_=Tp2b[:, 128:256], func=AF.Copy)
                        core(l, c0, vgr[:, 256:512], qkr[:, 128:256], qkrB, Sr[l], Sr_bf, elast_r, cfB, gate[:, 256:512], 768)

                        ck(5)
                        pb, pB = bank()
                        pbb = pb.bitcast(BF16)
                        for k in range(KC):
                            op("tensor", "transpose", [yB, cfB], [pB], out=pbb[:, k * 128:(k + 1) * 128], in_=ytile[:, k * 128:(k + 1) * 128], identity=identb)
                        op("scalar", "activation", [pB], [yTB], out=yT, in_=pbb.rearrange("p (k t) -> p k t", k=KC), func=AF.Copy)
                        W = [bank(), bank()]
                        for dh in range(2):
                            wb, wB = W[dh]
                            for k in range(KC):
                                op("tensor", "matmul", [yTB, woutB], [wB], wb, lhsT=yT[:, k, :], rhs=woutb[:, k, dh * 512:(dh + 1) * 512], start=(k == 0), stop=(k == KC - 1))
                        post_norm_residual([W[0][0], W[1][0]], [W[0][1], W[1][1]], t512[1], t512B[1], tt)

                ck(6)
                fence()
                load_gain(l * 4 + 2)
                for tt in range(NT):
                    norm_to_T(h[:, tt, :], hB[tt], utokf, utokfB, uTh[:, :, tt * 128:(tt + 1) * 128], uThB)
                load_gain(l * 4 + 3)
                ck(7)
                nxt = None
                if l + 1 < L:
                    nxt = l + 1
                elif part + 1 < run_parts:
                    nxt = 0
                for g in range(NG):
                    for tb in range(TH // 512):
                        for fc in range(4):
                            pb, pB = bank()
                            for k in range(KC):
                                op("tensor", "matmul", [ringB[0], uThB], [pB], pb, lhsT=w1v[:, k, fc * 128:(fc + 1) * 128], rhs=uTh[:, k, tb * 512:(tb + 1) * 512],
                                   start=(k == 0), stop=(k == KC - 1))
                            op("scalar", "activation", [pB], [rlB], out=rl, in_=pb, func=AF.Relu)
                            op("gpsimd", "tensor_tensor", [rlB], [hdnB], out=hdn[:, fc, tb * 512:(tb + 1) * 512], in0=rl, in1=rl, op=ALU.mult)
                    if g + 1 < NG:
                        load_w1(l, g + 1)
                    elif nxt is not None:
                        load_w1(nxt, 0)
                    for tt in range(NT):
                        for dh in range(2):
                            pb, pB = bank()
                            for fc in range(4):
                                op("tensor", "matmul", [hdnB, ringB[1]], [pB], pb, lhsT=hdn[:, fc, tt * 128:(tt + 1) * 128], rhs=w2v[:, fc, dh * 512:(dh + 1) * 512],
                                   start=(fc == 0), stop=(fc == 3))
                            a_ = acc[:, tt, dh * 512:(dh + 1) * 512]
                            if g == 0:
                                op("vector", "tensor_copy", [pB], [accB[tt]], out=a_, in_=pb)
                            else:
                                op("vector", "tensor_tensor", [pB, accB[tt]], [accB[tt]], out=a_, in0=pb, in1=a_, op=ALU.add)
                    if g + 1 < NG:
                        load_w2(l, g + 1)
                    elif nxt is not None:
                        load_w2(nxt, 0)
                    if g == 3 and nxt is not None:
                        load_mixer_weights(nxt)
                for tt in range(NT):
                    post_norm_residual([acc[:, tt, 0:512], acc[:, tt, 512:1024]], [accB[tt], accB[tt]], tf, tfB, tt)
                fence()
              except StopBuild:
                break

            osrc = out_d[t0:t0 + TH, :].rearrange("(t p) d -> p t d", p=128)
            outB = Buf("out")
            for q in range(NT):
                dma("sync", osrc[:, q, :], h[:, q, :], [hB[q]], [outB])
            S.add("sync", lambda e: None, [outB], [])

        S.emit(nc, st)
    return nc, S


def make_consts():
    cf = np.zeros((128, K_END), np.float32)
    p = np.arange(128)
    cf[:, K_ID:K_ID + 128] = np.eye(128, dtype=np.float32)
    tri = (p[:, None] <= p[None, :]).astype(np.float32)
    cf[:, K_TRI:K_TRI + 128] = tri
    cf[:, K_NTRI:K_NTRI + 128] = -tri
    cf[:, K_T16:K_T16 + 128] = -tri / 16.0
    cf[:, K_ONES:K_ONES + 128] = 1.0
    cf[:, K_HM:K_HM + 4] = (p[:, None] // 32 == np.arange(4)[None, :]).astype(np.float32)
    cf[:, K_BM:K_BM + 256] = (p[:, None] // 32 == (np.arange(256)[None, :] // 64)).astype(np.float32)
    lg = np.log1p(-np.exp2(-5.0 - np.arange(4, dtype=np.float64)))
    hd = np.arange(128) // 32
    tok = np.arange(128, dtype=np.float64)
    epos = np.exp((tok[:, None] + 1.0) * lg[hd][None, :])
    eneg = np.exp(-(tok[:, None] + 1.0) * lg[hd][None, :]) * (32.0 ** -0.5)
    cf[:, K_RE:K_RE + 128] = epos
    cf[:, K_RE + 128:K_RE + 256] = eneg
    cf[:, K_EL] = np.exp(128.0 * lg[hd])
    invf = (np.float32(10000.0) ** (-np.arange(0, 32, 2, dtype=np.float32) / np.float32(32))).astype(np.float32)
    cf[:, K_IF:K_IF + 16] = invf[None, :]
    cf[:, K_IF + 16:K_IF + 18] = (p[:, None] // 64 == np.arange(2)[None, :]).astype(np.float32)
    return cf


def prepare_inputs(inputs, n_layers=DEPTH):
    L = n_layers
    f32 = np.float32
    g = lambda k: np.asarray(inputs[k])
    gains = np.stack([g("norm_pre_mix")[:L], g("norm_post_mix")[:L], g("norm_pre_ffn")[:L], g("norm_post_ffn")[:L]], axis=1).reshape(L * 4, D).astype(f32)
    convT = np.ascontiguousarray(np.transpose(g("mlstm_conv_w")[:L], (2, 0, 1)).reshape(512, L * 4)).astype(f32)
    ifb = np.concatenate([g("mlstm_i_bias")[:L], g("mlstm_f_bias")[:L]], axis=1).reshape(1, L * 8).astype(f32)
    hn = np.concatenate([g("mlstm_norm")[:L], g("gla_norm")[:L], g("ret_norm")[:L]], axis=1).astype(f32)
    shared = {
        "gains": np.ascontiguousarray(gains),
        "w_in": np.ascontiguousarray(g("w_in")[:L], dtype=f32),
        "w_out": np.ascontiguousarray(g("w_out")[:L], dtype=f32),
        "w_ff1": np.ascontiguousarray(g("w_ff1")[:L], dtype=f32),
        "w_ff2": np.ascontiguousarray(g("w_ff2")[:L], dtype=f32),
        "convT": convT,
        "ifb": np.ascontiguousarray(ifb),
        "hn": np.ascontiguousarray(hn),
        "wup": np.ascontiguousarray(g("gla_w_up")[:L], dtype=f32),
        "gb": np.ascontiguousarray(g("gla_gate_bias")[:L].reshape(1, L * 128), dtype=f32),
        "cf": make_consts(),
    }
    x = g("x")
    pos = g("positions")
    maps = []
    for b in range(x.shape[0]):
        m = dict(shared)
        m["x"] = np.ascontiguousarray(x[b], dtype=f32)
        m["pos"] = np.ascontiguousarray(pos[b].reshape(T // 128, 128).T).astype(np.int32)
        maps.append(m)
    return maps


_CACHE = {}


def kernel(**inputs):
    if "nc" not in _CACHE:
        _CACHE["nc"] = build_program()[0]
    nc = _CACHE["nc"]
    maps = prepare_inputs(inputs)
    res = run_bass_kernel_spmd(nc, maps, core_ids=list(range(len(maps))))
    out = np.stack([np.asarray(r["out"]) for r in res.results], axis=0)
    return out.astype(np.float32)
```

```python
import numpy as np
from contextlib import ExitStack
import concourse.bass as bass
import concourse.mybir as mybir
from concourse.bass_utils import run_bass_kernel_spmd

F32 = mybir.dt.float32
BF16 = mybir.dt.bfloat16
I32 = mybir.dt.int32
AF = mybir.ActivationFunctionType
ALU = mybir.AluOpType
AX = mybir.AxisListType

ENGS = ["tensor", "vector", "scalar", "gpsimd", "sync"]
N_DMA_SEMS = 8

D = 1024
T = 2048
DEPTH = 4
NPART = 2
TH = T // NPART
NT = TH // 128
NBLK = TH // 512
KC = D // 128
INC = 3096
DFF = 4096
NG = 8
EPS = 1e-6
C_MQ, C_MK, C_MV, C_MIF, C_MO = 0, 256, 512, 1024, 1032
C_GQ, C_GK, C_GV, C_GA, C_GG = 1544, 1672, 1800, 2056, 2072
C_RQ, C_RK, C_RV, C_RG = 2328, 2456, 2584, 2840
K_ID, K_TRI, K_ONES, K_HM, K_BM, K_RE, K_EL, K_IF, K_NTRI, K_T16, K_END = 0, 128, 256, 384, 388, 644, 900, 901, 920, 1048, 1176


class Buf:
    __slots__ = ("name", "last_w", "readers", "excl")

    def __init__(self, name="", excl=False):
        self.name = name
        self.last_w = None
        self.readers = []
        self.excl = excl


class Op:
    __slots__ = ("eng", "fn", "deps", "signal", "ordinal", "dma", "dma_sem", "dma_val", "dma_prev")

    def __init__(self, eng, fn, dma):
        self.eng = eng
        self.fn = fn
        self.deps = set()
        self.signal = False
        self.ordinal = None
        self.dma = dma
        self.dma_sem = None
        self.dma_val = None
        self.dma_prev = 0


class Sched:
    def __init__(self, same_engine_sync=True):
        self.ops = {e: [] for e in ENGS}
        self.same_engine_sync = same_engine_sync
        self.n_dma = {e: 0 for e in ENGS}

    def add(self, eng, fn, reads=(), writes=(), dma=False):
        op = Op(eng, fn, dma)
        deps = set()
        for b in reads:
            if b.last_w is not None:
                deps.add(b.last_w)
            if b.excl:
                for r in b.readers:
                    if r.eng != eng:
                        deps.add(r)
        for b in writes:
            if b.last_w is not None:
                deps.add(b.last_w)
            deps.update(b.readers)
        for b in reads:
            b.readers.append(op)
        for b in writes:
            b.last_w = op
            b.readers = []
        deps.discard(op)
        pruned = set()
        for d in deps:
            if d.eng == eng and not d.dma:
                if eng == "tensor" or not self.same_engine_sync:
                    continue
            pruned.add(d)
        op.deps = pruned
        for d in pruned:
            d.signal = True
        if dma:
            n = self.n_dma[eng]
            self.n_dma[eng] = n + 1
            op.dma_sem = n % N_DMA_SEMS
            op.dma_val = 16 * (n // N_DMA_SEMS + 1)
            op.dma_prev = 16 * (n // N_DMA_SEMS)
        self.ops[eng].append(op)
        return op

    def emit(self, nc, stack):
        for e in ENGS:
            c = 0
            for op in self.ops[e]:
                if not op.dma and op.signal:
                    c += 1
                    op.ordinal = c
        esem = {e: stack.enter_context(nc.semaphore("es_" + e)) for e in ENGS}
        dsem = {e: [stack.enter_context(nc.semaphore("ds_%s_%d" % (e, i))) for i in range(N_DMA_SEMS)]
                for e in ENGS if self.n_dma[e] > 0}
        block = stack.enter_context(nc.Block())
        nwaits = [0]

        def run(ename, eng):
            known = {}
            for op in self.ops[ename]:
                waits = {}
                for d in op.deps:
                    if d.dma:
                        s = dsem[d.eng][d.dma_sem]
                        v = d.dma_val
                    else:
                        s = esem[d.eng]
                        v = d.ordinal
                    key = id(s)
                    if key not in waits or waits[key][1] < v:
                        waits[key] = (s, v)
                if op.dma and op.dma_prev > 0:
                    s = dsem[ename][op.dma_sem]
                    key = id(s)
                    if key not in waits or waits[key][1] < op.dma_prev:
                        waits[key] = (s, op.dma_prev)
                for key, (s, v) in waits.items():
                    if known.get(key, 0) >= v:
                        continue
                    known[key] = v
                    eng.wait_ge(s, v)
                    nwaits[0] += 1
                ins = op.fn(eng)
                if ins is None:
                    continue
                if op.dma:
                    ins.then_inc(dsem[ename][op.dma_sem], 16)
                elif op.signal:
                    ins.then_inc(esem[ename], 1)

        @block.sync
        def _(e):
            run("sync", e)

        @block.tensor
        def _(e):
            run("tensor", e)

        @block.vector
        def _(e):
            run("vector", e)

        @block.scalar
        def _(e):
            run("scalar", e)

        @block.gpsimd
        def _(e):
            run("gpsimd", e)
        self.nwaits = nwaits[0]


def _dsize(dt):
    return 2 if dt == BF16 else 4


class Arena:
    def __init__(self, big, lo, hi):
        self.big = big
        self.lo = lo
        self.cur = lo
        self.hi = hi

    def alloc(self, free_shape, dtype=F32, parts=128):
        n = 1
        for s in free_shape:
            n *= s
        nbytes = n * _dsize(dtype)
        nbytes_r = (nbytes + 31) // 32 * 32
        off = self.cur
        self.cur += nbytes_r
        assert self.cur <= self.hi, "arena overflow %d > %d" % (self.cur, self.hi)
        assert nbytes % 4 == 0
        v = self.big[0:parts, off // 4: off // 4 + nbytes // 4]
        if dtype != F32:
            v = v.bitcast(dtype)
        if len(free_shape) == 2:
            v = v.rearrange("p (a b) -> p a b", a=free_shape[0])
        elif len(free_shape) == 3:
            v = v.rearrange("p (a b c) -> p a b c", a=free_shape[0], b=free_shape[1])
        return v


def bc_mid(ap2, n):
    P, F = ap2.shape
    return ap2.rearrange("p (o f) -> p o f", o=1).broadcast_to([P, n, F])


def bc_last(ap2, n):
    P, H = ap2.shape
    return ap2.rearrange("p (h o) -> p h o", o=1).broadcast_to([P, H, n])


def build_program(n_layers=DEPTH, run_parts=NPART, stage=None):
    nc = bass.Bass("TRN2", target_bir_lowering=False)
    L = n_layers

    def din(name, shape, dt=F32):
        return nc.dram_tensor(name, shape, dt, kind="ExternalInput").ap()

    x_d = din("x", [T, D])
    pos_d = din("pos", [128, T // 128], I32)
    gains_d = din("gains", [L * 4, D])
    w_in_d = din("w_in", [L, D, INC])
    w_out_d = din("w_out", [L, D, D])
    w_ff1_d = din("w_ff1", [L, D, DFF])
    w_ff2_d = din("w_ff2", [L, DFF, D])
    convT_d = din("convT", [512, L * 4])
    ifb_d = din("ifb", [1, L * 8])
    hn_d = din("hn", [L, D])
    wup_d = din("wup", [L, 16, 128])
    gb_d = din("gb", [1, L * 128])
    cf_d = din("cf", [128, K_END])
    out_d = nc.dram_tensor("out", [T, D], F32, kind="ExternalOutput").ap()

    S = Sched()

    class StopBuild(Exception):
        pass

    def ck(n):
        if stage is not None and stage == n:
            raise StopBuild()

    defer = [None]

    def op(eng, method, reads, writes, *args, **kw):
        if defer[0] is not None:
            defer[0].append((eng, method, reads, writes, args, kw))
            return None
        return S.add(eng, lambda e: getattr(e, method)(*args, **kw), reads, writes)

    def run_interleaved(chains, pools):
        lists = []
        for c, pool in zip(chains, pools):
            defer[0] = []
            bank_pool[0] = pool
            c()
            lists.append(defer[0])
        defer[0] = None
        bank_pool[0] = None
        idx = [0] * len(lists)
        while True:
            best, bf = None, 2.0
            for i, lst in enumerate(lists):
                if idx[i] < len(lst):
                    f = idx[i] / float(len(lst))
                    if f < bf:
                        best, bf = i, f
            if best is None:
                break
            eng, method, reads, writes, args, kw = lists[best][idx[best]]
            idx[best] += 1
            op(eng, method, reads, writes, *args, **kw)

    def dma(eng, out, in_, reads, writes):
        return S.add(eng, lambda e: e.dma_start(out=out, in_=in_), reads, writes, dma=True)

    st = ExitStack()
    with st:
        total = nc.sbuf_bytes_remaining
        total = (total // 128) * 128 - 128
        big_h = nc.alloc_sbuf_tensor("big", [128, total // 4], F32)
        A = Arena(big_h, 0, total)

        psum = [nc.alloc_psum_tensor("ps%d" % i, [128, 512], F32) for i in range(8)]
        psB = [Buf("ps%d" % i, excl=True) for i in range(8)]
        bank_ctr = [0]

        bank_pool = [None]
        pool_ctr = {}

        def bank():
            if bank_pool[0] is not None:
                lst = bank_pool[0]
                k = pool_ctr.get(lst, 0)
                pool_ctr[lst] = k + 1
                i = lst[k % len(lst)]
            else:
                i = bank_ctr[0] % 8
                bank_ctr[0] += 1
            return psum[i][:], psB[i]

        h = A.alloc([NT, D])
        hB = [Buf("h%d" % i) for i in range(NT)]
        winb = A.alloc([KC, INC], BF16)
        winB = [Buf("win%d" % i) for i in range(4)]
        woutb = A.alloc([KC, D], BF16)
        woutB = Buf("wout")
        ring = [A.alloc([8 * 512], BF16) for _ in range(2)]
        ringB = [Buf("ringA"), Buf("ringB")]
        w1v = ring[0].rearrange("p (c f) -> p c f", c=8)
        w2v = ring[1].rearrange("p (c d) -> p c d", c=4)
        cf = A.alloc([K_END])
        cfB = Buf("cf")
        identb = A.alloc([128], BF16)
        cosT = A.alloc([T // 128, 16])
        sinT = A.alloc([T // 128, 16])
        rotB = Buf("rot")
        cw = A.alloc([4, L * 4])
        ifb = A.alloc([L * 8])
        gbb = A.alloc([128]); gbbB = Buf("gbb")
        wup = A.alloc([L, 128], parts=16)
        smallB = Buf("small")
        Cst = [A.alloc([2, 129]) for _ in range(L)]
        Sg = [A.alloc([256]) for _ in range(L)]
        Sr = [A.alloc([256]) for _ in range(L)]
        ctail = [A.alloc([4, 3]) for _ in range(L)]
        stB = [Buf("state%d" % l) for l in range(L)]
        C_bf = A.alloc([2, 129], BF16)
        Sg_bf = A.alloc([256], BF16)
        Sr_bf = A.alloc([256], BF16)
        v_ext = A.alloc([4, 129], BF16)
        vB = Buf("v_ext")
        gbc = A.alloc([D])
        gbcB = Buf("gbc")
        gbc2 = A.alloc([D])
        gbc2B = Buf("gbc2")
        sm = A.alloc([64])
        smB = Buf("sm")
        ss = sm[:, 0:1]
        ss2 = sm[:, 1:3]
        rstd = sm[:, 3:4]
        ph_lo = A.cur
        ph_hi = total

        M = Arena(big_h, ph_lo, ph_hi)
        hnbc = M.alloc([D]); hnB = Buf("hn")
        uTb = M.alloc([KC, 512], BF16); uTB = Buf("uTb")
        ytile = M.alloc([D], BF16); yB = Buf("ytile")
        xq = M.alloc([4, 515]); xqB = Buf("xq")
        eq = xq[:, :, 0:512]
        yq = M.alloc([4, 512]); yqB = Buf("yq")
        qkb = M.alloc([4, 512], BF16); qkB = Buf("qkb")
        gqk = M.alloc([2, 512], BF16); gqkB = Buf("gqk")
        gaT = M.alloc([512], parts=16); gaB = Buf("gaT")
        ifp = M.alloc([8]); lf = M.alloc([4]); t4 = M.alloc([4]); aj = M.alloc([4]); wj = M.alloc([4])
        den = M.alloc([4]); ssm = M.alloc([4]); fac = M.alloc([4]); mB = Buf("msmall")
        og = M.alloc([512]); ogB = Buf("og")
        t512 = [M.alloc([512]) for _ in range(3)]; t512B = [Buf("t512_%d" % i) for i in range(3)]
        gate = M.alloc([512]); gateB = Buf("gate")
        xg = t512[2]; xgB = t512B[2]
        R = t512[2].rearrange("p (h i) -> p h i", h=4); RB = t512B[2]
        vgr = M.alloc([512], BF16); vgrB = Buf("vgr")
        Xr = M.alloc([8, 32]); XrB = Buf("Xr")
        Xo = M.alloc([8, 32]); XoB = Buf("Xo")
        rt0 = M.alloc([8, 16]); rtB = Buf("rt")
        qkr = M.alloc([256], BF16); qkrB = Buf("qkr")
        DT = M.alloc([4, 128]); DTB = Buf("DT")
        E = M.alloc([4, 128]); EB = Buf("E")
        PT = M.alloc([4, 128], BF16); PTB = Buf("PT")
        qp = M.alloc([2, 128], BF16); qpB = Buf("qp")
        kp = M.alloc([256], BF16); kpB = Buf("kp")
        yT = M.alloc([KC, 128], BF16); yTB = Buf("yT")
        qt = M.alloc([128], BF16); qtB = Buf("qt")
        kt = M.alloc([128], BF16); ktB = Buf("kt")
        ktok = M.alloc([128], BF16); ktokB = Buf("ktok")
        Qbd = M.alloc([4, 128], BF16); QbdB = Buf("Qbd")
        rs4 = M.alloc([4]); rs4B = Buf("rs4")
        yqf = yq.rearrange("p a b -> p (a b)")
        xqf = xq.rearrange("p a b -> p (a b)")

        def bfv(ap):
            return ap.bitcast(BF16)
        PT_g = bfv(yqf[:, 0:256]).rearrange("p (h i) -> p h i", h=4); PTgB = Buf("PT_g")
        PT_r = bfv(yqf[:, 256:512]).rearrange("p (h i) -> p h i", h=4); PTrB = Buf("PT_r")
        Qbd_g = bfv(yqf[:, 512:768]).rearrange("p (h i) -> p h i", h=4); QbdgB = Buf("Qbd_g")
        Qbd_r = bfv(yqf[:, 768:1024]).rearrange("p (h i) -> p h i", h=4); QbdrB = Buf("Qbd_r")
        tmp_g = yqf[:, 1024:1280]; tmpgB = Buf("tmp_g")
        tmp_r = yqf[:, 1280:1536]; tmprB = Buf("tmp_r")
        tU_g = yqf[:, 1536:1792]; tUgB = Buf("tU_g")
        tU_r = yqf[:, 1792:2048]; tUrB = Buf("tU_r")
        la = xqf[:, 0:128]; laB = Buf("laE")
        Ep = xqf[:, 128:256]; En = xqf[:, 256:384]; EpB = laB
        qt_r = bfv(xqf[:, 384:448]); qtrB = Buf("qt_r")
        kt_r = bfv(xqf[:, 448:512]); ktrB = Buf("kt_r")
        junkm = bfv(xqf[:, 512:576]); junkmB = Buf("junkm")
        rs4_r = xqf[:, 576:580]; rs4rB = Buf("rs4_r")
        Zbufs = [PTgB, PTrB, QbdgB, QbdrB, tmpgB, tmprB, tUgB, tUrB, laB, qtrB, ktrB, junkmB, rs4rB]
        CB_G = dict(PT=PT_g, PTB=PTgB, Qbd=Qbd_g, QbdB=QbdgB, tmp=tmp_g, tmpB=tmpgB, tU=tU_g, tUB=tUgB, rs4=rs4, rs4B=rs4B, qt=qt, qtB=qtB, kt=kt, ktB=ktB)
        CB_R = dict(PT=PT_r, PTB=PTrB, Qbd=Qbd_r, QbdB=QbdrB, tmp=tmp_r, tmpB=tmprB, tU=tU_r, tUB=tUrB, rs4=rs4_r, rs4B=rs4rB, qt=qt_r, qtB=qtrB, kt=kt_r, ktB=ktrB)
        mixer_bufs = [hnB, uTB, yB, xqB, yqB, qkB, gqkB, gaB, mB, ogB, gateB, vgrB, XrB, XoB, rtB, qkrB, DTB, EB, PTB,
                      qpB, kpB, yTB, PTgB, PTrB, QbdgB, QbdrB, tmpgB, tmprB, tUgB, tUrB, laB, qtrB, ktrB, junkmB, rs4rB, qtB, ktB, ktokB, QbdB, rs4B] + t512B

        FA = Arena(big_h, ph_lo, ph_hi)
        acc = FA.alloc([NT, D]); accB = [Buf("acc%d" % i) for i in range(NT)]
        uTh = FA.alloc([KC, TH], BF16); uThB = Buf("uTh")
        hdn = FA.alloc([4, TH], BF16); hdnB = Buf("hdn")
        rl = FA.alloc([512]); rlB = Buf("rl")
        utokf = FA.alloc([D], BF16); utokfB = Buf("utok_f")
        tf = FA.alloc([512]); tfB = Buf("tf")
        ffn_bufs = accB + [uThB, hdnB, rlB, utokfB, tfB]
        print("SBUF: persistent %d, mixer %d, ffn %d, phase avail %d" % (ph_lo, M.cur - ph_lo, FA.cur - ph_lo, ph_hi - ph_lo))

        def fenceZ():
            op("gpsimd", "memset", [], [xqB, yqB] + Zbufs, sm[:, 9:10], 0.0)

        def fence():
            op("gpsimd", "memset", [], mixer_bufs + ffn_bufs, sm[:, 8:9], 0.0)

        dma("sync", cf, cf_d, [], [cfB])
        ident = cf[:, K_ID:K_ID + 128]
        tri = cf[:, K_TRI:K_TRI + 128]
        ones = cf[:, K_ONES:K_ONES + 128]
        hmask = cf[:, K_HM:K_HM + 4]
        bmask = cf[:, K_BM:K_BM + 256]
        retE = cf[:, K_RE:K_RE + 256]
        elast_r = cf[:, K_EL:K_EL + 1]
        invf = cf[:, K_IF:K_IF + 16]
        hm2 = cf[:, K_IF + 16:K_IF + 18]
        ntri = cf[:, K_NTRI:K_NTRI + 128]
        tri16 = cf[:, K_T16:K_T16 + 128]
        op("vector", "tensor_copy", [cfB], [cfB], out=identb, in_=ident)
        dma("sync", cw, convT_d.rearrange("(m p) k -> p m k", p=128), [], [smallB])
        dma("sync", ifb, ifb_d.partition_broadcast(128), [], [smallB])
        dma("sync", wup, wup_d.rearrange("l r c -> r l c"), [], [smallB])

        RA = Arena(big_h, ph_lo, ph_hi)
        NTT = T // 128
        posi = RA.alloc([NTT], I32)
        posf = RA.alloc([NTT])
        ang = RA.alloc([NTT, 16])
        tq = RA.alloc([NTT, 16])
        tqi = RA.alloc([NTT, 16], I32)
        tB = Buf("rot_tmp")
        dma("sync", posi, pos_d, [], [tB])
        op("vector", "tensor_copy", [tB], [tB], out=posf, in_=posi)
        op("vector", "tensor_tensor", [tB, cfB], [tB], out=ang, in0=bc_last(posf, 16), in1=bc_mid(invf, NTT), op=ALU.mult)
        TWO_PI = float(2 * np.pi)
        PI = float(np.pi)

        def make_trig(dst, shift):
            op("vector", "tensor_scalar", [tB], [tB], out=tq, in0=ang, scalar1=shift, scalar2=1.0 / TWO_PI, op0=ALU.add, op1=ALU.mult)
            op("vector", "tensor_copy", [tB], [tB], out=tqi, in_=tq)
            op("vector", "tensor_copy", [tB], [tB], out=tq, in_=tqi)
            op("vector", "scalar_tensor_tensor", [tB], [tB], out=tq, in0=tq, scalar=-TWO_PI, in1=ang, op0=ALU.mult, op1=ALU.add)
            op("vector", "tensor_scalar", [tB], [tB], out=tq, in0=tq, scalar1=shift, scalar2=None, op0=ALU.add)
            op("vector", "tensor_scalar", [tB], [rotB], out=dst, in0=tq, scalar1=PI, scalar2=-TWO_PI, op0=ALU.is_gt, op1=ALU.mult)
            op("vector", "tensor_tensor", [tB, rotB], [tB], out=tq, in0=tq, in1=dst, op=ALU.add)
            op("vector", "tensor_scalar", [tB], [rotB], out=dst, in0=tq, scalar1=-PI, scalar2=TWO_PI, op0=ALU.is_lt, op1=ALU.mult)
            op("vector", "tensor_tensor", [tB, rotB], [tB], out=tq, in0=tq, in1=dst, op=ALU.add)
            op("vector", "tensor_scalar", [tB], [tB], out=tq, in0=tq, scalar1=PI, scalar2=-PI, op0=ALU.min, op1=ALU.max)
            op("scalar", "activation", [tB], [rotB], out=dst, in_=tq, func=AF.Sin)

        make_trig(sinT, 0.0)
        make_trig(cosT, float(np.pi / 2))
        op("gpsimd", "memset", [], [tB] + mixer_bufs + ffn_bufs, sm[:, 8:9], 0.0)

        for l in range(L):
            op("gpsimd", "memset", [], [stB[l]], Cst[l], 0.0)
            op("gpsimd", "memset", [], [stB[l]], Sg[l], 0.0)
            op("gpsimd", "memset", [], [stB[l]], Sr[l], 0.0)
            op("gpsimd", "memset", [], [stB[l]], ctail[l], 0.0)
        op("gpsimd", "memset", [], [vB], v_ext, 1.0)

        def load_mixer_weights(l):
            src = w_in_d[l].rearrange("(c p) n -> p c n", p=128)
            for q in range(4):
                dma("gpsimd", winb[:, 2 * q:2 * q + 2, :], src[:, 2 * q:2 * q + 2, :], [], [winB[q]])
            dma("gpsimd", woutb, w_out_d[l].rearrange("(c p) n -> p c n", p=128), [], [woutB])

        def load_w1(l, g):
            dma("gpsimd", w1v, w_ff1_d[l][:, g * 512:(g + 1) * 512].rearrange("(c p) f -> p c f", p=128), [], [ringB[0]])

        def load_w2(l, g):
            dma("gpsimd", w2v, w_ff2_d[l][g * 512:(g + 1) * 512, :].rearrange("(c p) d -> p c d", p=128), [], [ringB[1]])

        def load_gain(row):
            if row % 2 == 0:
                dma("sync", gbc, gains_d[row:row + 1, :].partition_broadcast(128), [], [gbcB])
            else:
                dma("sync", gbc2, gains_d[row:row + 1, :].partition_broadcast(128), [], [gbc2B])

        def rstd_from(ss_ap, n, out_ap, buf):
            op("scalar", "activation", [buf], [buf], out=out_ap, in_=ss_ap, func=AF.Ln, scale=1.0 / n, bias=EPS)
            op("scalar", "activation", [buf], [buf], out=out_ap, in_=out_ap, func=AF.Exp, scale=-0.5)

        def norm_to_T(src_ap, srcB, utok, utokB, dstT, dstB):
            op("gpsimd", "memset", [], [smB], ss, 0.0)
            op("scalar", "activation", [srcB, smB], [utokB, smB], out=utok, in_=src_ap, func=AF.Square, accum_out=ss)
            rstd_from(ss, D, rstd, smB)
            op("vector", "scalar_tensor_tensor", [srcB, smB, gbcB], [utokB], out=utok, in0=src_ap, scalar=rstd, in1=gbc, op0=ALU.mult, op1=ALU.mult)
            pb, pB = bank()
            pbb = pb.bitcast(BF16)
            for k in range(KC):
                op("tensor", "transpose", [utokB, cfB], [pB], out=pbb[:, k * 128:(k + 1) * 128], in_=utok[:, k * 128:(k + 1) * 128], identity=identb)
            op("scalar", "activation", [pB], [dstB], out=dstT, in_=pbb.rearrange("p (k t) -> p k t", k=KC), func=AF.Copy)

        def post_norm_residual(srcs, srcBs, tmp, tmpB, tt):
            op("gpsimd", "memset", [], [smB], ss2, 0.0)
            for dh in range(2):
                op("scalar", "activation", [srcBs[dh], smB], [tmpB, smB], out=tmp.bitcast(BF16)[:, 0:512], in_=srcs[dh], func=AF.Square, accum_out=ss2[:, dh:dh + 1])
            op("vector", "tensor_tensor", [smB], [smB], out=ss, in0=ss2[:, 0:1], in1=ss2[:, 1:2], op=ALU.add)
            rstd_from(ss, D, rstd, smB)
            for dh in range(2):
                op("vector", "scalar_tensor_tensor", [srcBs[dh], smB, gbc2B], [tmpB], out=tmp, in0=srcs[dh], scalar=rstd, in1=gbc2[:, dh * 512:(dh + 1) * 512], op0=ALU.mult, op1=ALU.mult)
                hs = h[:, tt, dh * 512:(dh + 1) * 512]
                op("vector", "tensor_tensor", [tmpB, hB[tt]], [hB[tt]], out=hs, in0=hs, in1=tmp, op=ALU.add)

        def core(l, c0, v_ap, ktok_ap, ktok_B, Sst, Sbf, elast_ap, elB, gate_ap, ycols, cb):
            sB = stB[l]
            PT_, PTB_, Qbd_, QbdB_ = cb["PT"], cb["PTB"], cb["Qbd"], cb["QbdB"]
            tmp, tmpB, tU, tUB, rs_, rsB_ = cb["tmp"], cb["tmpB"], cb["tU"], cb["tUB"], cb["rs4"], cb["rs4B"]
            qt_, qtB_, kt_, ktB_ = cb["qt"], cb["qtB"], cb["kt"], cb["ktB"]
            op("gpsimd", "tensor_tensor", [qtB_, cfB], [QbdB_], out=Qbd_, in0=bc_mid(qt_, 4), in1=bc_last(hmask, 128), op=ALU.mult)
            Sc, ScB = bank()
            op("tensor", "matmul", [ktB_, QbdB_], [ScB], Sc, lhsT=kt_, rhs=Qbd_.rearrange("p h i -> p (h i)"), start=True, stop=True)
            op("vector", "tensor_tensor", [ScB, cfB], [PTB_], out=PT_, in0=Sc.rearrange("p (h i) -> p h i", h=4), in1=bc_mid(tri, 4), op=ALU.mult)
            ob, oB = bank()
            for hh in range(4):
                op("tensor", "matmul", [PTB_, vgrB], [oB], ob[:, hh * 64:(hh + 1) * 64], lhsT=PT_[:, hh, :], rhs=v_ap[:, hh * 64:(hh + 1) * 64], start=(hh == 0), stop=False, skip_group_check=True)
            op("tensor", "matmul", [qtB_, sB], [oB], ob[:, 0:256], lhsT=qt_, rhs=Sbf, start=False, stop=True, skip_group_check=True)
            o3 = ob[:, 0:256].rearrange("p (h e) -> p h e", h=4)
            tmp3 = tmp.rearrange("p (h e) -> p h e", h=4)
            op("scalar", "activation", [oB], [tmpB], out=tmp, in_=ob[:, 0:256], func=AF.Square)
            op("vector", "tensor_reduce", [tmpB], [rsB_], out=rs_, in_=tmp3, axis=AX.X, op=ALU.add)
            rstd_from(rs_, 64, rs_, rsB_)
            op("vector", "tensor_tensor", [oB, rsB_], [tmpB], out=tmp3, in0=o3, in1=bc_last(rs_, 64), op=ALU.mult)
            op("gpsimd", "tensor_tensor", [tmpB, gateB], [yB], out=ytile[:, ycols:ycols + 256], in0=tmp, in1=gate_ap, op=ALU.mult)
            ub, uB = bank()
            op("tensor", "matmul", [ktok_B, vgrB], [uB], ub[:, 0:256], lhsT=ktok_ap, rhs=v_ap, start=True, stop=True)
            op("vector", "scalar_tensor_tensor", [uB, elB, cfB], [tUB], out=tU, in0=ub[:, 0:256], scalar=elast_ap, in1=bmask, op0=ALU.mult, op1=ALU.mult)
            op("vector", "scalar_tensor_tensor", [sB, elB, tUB], [sB], out=Sst, in0=Sst, scalar=elast_ap, in1=tU, op0=ALU.mult, op1=ALU.add)
            op("gpsimd", "tensor_copy", [sB], [sB], out=Sbf, in_=Sst)

        load_mixer_weights(0)
        load_w1(0, 0)
        load_w2(0, 0)
        for part in range(run_parts):
            t0 = part * TH
            xsrc = x_d[t0:t0 + TH, :].rearrange("(t p) d -> p t d", p=128)
            for q in range(NT):
                dma("sync", h[:, q, :], xsrc[:, q, :], [], [hB[q]])

            for l in range(L if stage != 0 else 0):
              try:
                sB = stB[l]
                load_gain(l * 4 + 0)
                load_gain(l * 4 + 1)
                op("gpsimd", "tensor_copy", [sB], [sB], out=C_bf, in_=Cst[l])
                op("gpsimd", "tensor_copy", [sB], [sB], out=Sg_bf, in_=Sg[l])
                op("gpsimd", "tensor_copy", [sB], [sB], out=Sr_bf, in_=Sr[l])
                dma("sync", gbb, gb_d[:, l * 128:(l + 1) * 128].partition_broadcast(128), [], [gbbB])
                dma("sync", hnbc, hn_d[l:l + 1, :].partition_broadcast(128), [], [hnB])
                for blk in range(NBLK):
                    for ti in range(4):
                        tt = blk * 4 + ti
                        norm_to_T(h[:, tt, :], hB[tt], ytile, yB, uTb[:, :, ti * 128:(ti + 1) * 128], uTB)
                    fenceZ()
                    op("gpsimd", "tensor_copy", [sB], [xqB], out=xq[:, :, 0:3], in_=ctail[l])
                    bcols = [C_MQ, C_MQ + 128, C_MK, C_MK + 128, C_GQ, C_GK, C_GA]
                    bM = [128, 128, 128, 128, 128, 128, 16]
                    for i in range(7):
                        pb, pB = bank()
                        for k in range(KC):
                            op("tensor", "matmul", [winB[k // 2], uTB], [pB], pb[0:bM[i], :], lhsT=winb[:, k, bcols[i]:bcols[i] + bM[i]], rhs=uTb[:, k, :],
                               start=(k == 0), stop=(k == KC - 1))
                        if i < 4:
                            op("scalar", "activation", [pB], [xqB], out=xq[:, i, 3:515], in_=pb, func=AF.Copy)
                        elif i == 4:
                            op("scalar", "activation", [pB], [gqkB], out=gqk[:, 0, :], in_=pb, func=AF.Copy, scale=float(32 ** -0.5))
                        elif i == 5:
                            op("scalar", "activation", [pB], [gqkB], out=gqk[:, 1, :], in_=pb, func=AF.Copy)
                        else:
                            op("scalar", "activation", [pB], [gaB], out=gaT, in_=pb[0:16, :], func=AF.Copy)
                    for i in range(4):
                        op("scalar", "activation", [xqB, smallB], [yqB], out=yq[:, i, :], in_=xq[:, i, 3:515], func=AF.Copy, scale=cw[:, i, l * 4 + 3:l * 4 + 4])
                        for s in (2, 1, 0):
                            op("vector", "scalar_tensor_tensor", [xqB, smallB, yqB], [yqB], out=yq[:, i, :], in0=xq[:, i, s:s + 512], scalar=cw[:, i, l * 4 + s:l * 4 + s + 1],
                               in1=yq[:, i, :], op0=ALU.mult, op1=ALU.add)
                    op("gpsimd", "tensor_copy", [xqB], [sB], out=ctail[l], in_=xq[:, :, 512:515])
                    op("scalar", "activation", [yqB, sB], [xqB], out=eq, in_=yq, func=AF.Exp, scale=-1.0)
                    op("scalar", "activation", [xqB], [xqB], out=eq, in_=eq, func=AF.Ln, bias=1.0)
                    op("scalar", "activation", [xqB], [xqB], out=eq, in_=eq, func=AF.Exp, scale=-1.0)
                    op("vector", "tensor_tensor", [yqB, xqB], [qkB], out=qkb[:, 0:2, :], in0=yq[:, 0:2, :], in1=eq[:, 0:2, :], op=ALU.mult)
                    op("vector", "scalar_tensor_tensor", [yqB, xqB], [qkB], out=qkb[:, 2:4, :], in0=yq[:, 2:4, :], scalar=0.125, in1=eq[:, 2:4, :], op0=ALU.mult, op1=ALU.mult)

                    fenceZ()
                    ck(1)
                    for ti in range(4):
                        tt = blk * 4 + ti
                        gt = part * NT + tt
                        c0 = ti * 128

                        def amm(c_lo, n):
                            pb, pB = bank()
                            for k in range(KC):
                                op("tensor", "matmul", [winB[k // 2], uTB], [pB], pb[:, 0:n], lhsT=uTb[:, k, c0:c0 + 128], rhs=winb[:, k, c_lo:c_lo + n],
                                   start=(k == 0), stop=(k == KC - 1))
                            return pb, pB
                        pb, pB = amm(C_MV, 512)
                        op("scalar", "activation", [pB], [vB], out=v_ext[:, :, 0:128], in_=pb.rearrange("p (h e) -> p h e", h=4), func=AF.Copy)
                        pb, pB = amm(C_MIF, 8)
                        op("vector", "tensor_tensor", [pB, smallB], [mB], out=ifp, in0=pb[:, 0:8], in1=ifb[:, l * 8:(l + 1) * 8], op=ALU.add)
                        pb, pB = amm(C_MO, 512)
                        op("scalar", "activation", [pB], [t512B[0]], out=t512[0], in_=pb, func=AF.Exp, scale=-1.0)
                        op("scalar", "activation", [t512B[0]], [t512B[0]], out=t512[0], in_=t512[0], func=AF.Ln, bias=1.0)
                        op("scalar", "activation", [t512B[0]], [t512B[0]], out=t512[0], in_=t512[0], func=AF.Exp, scale=-1.0)
                        op("vector", "tensor_tensor", [t512B[0], hnB], [ogB], out=og, in0=t512[0], in1=hnbc[:, 0:512], op=ALU.mult)
                        pb, pB = amm(C_GV, 256)
                        op("scalar", "activation", [pB], [vgrB], out=vgr[:, 0:256], in_=pb[:, 0:256], func=AF.Copy)
                        pb, pB = amm(C_GG, 512)
                        op("scalar", "activation", [pB], [xgB], out=xg[:, 0:256], in_=pb[:, 0:256], func=AF.Copy)
                        op("scalar", "activation", [pB], [t512B[1]], out=t512[1][:, 0:256], in_=pb[:, 0:256], func=AF.Exp, scale=-1.0)
                        op("scalar", "activation", [pB], [XrB], out=Xr.rearrange("p a b -> p (a b)"), in_=pb[:, 256:512], func=AF.Copy)
                        pb, pB = amm(C_RV, 512)
                        op("scalar", "activation", [pB], [vgrB], out=vgr[:, 256:512], in_=pb[:, 0:256], func=AF.Copy)
                        op("scalar", "activation", [pB], [xgB], out=xg[:, 256:512], in_=pb[:, 256:512], func=AF.Copy)
                        op("scalar", "activation", [pB], [t512B[1]], out=t512[1][:, 256:512], in_=pb[:, 256:512], func=AF.Exp, scale=-1.0)
                        op("gpsimd", "tensor_tensor", [xgB, hnB], [xgB], out=xg, in0=xg, in1=hnbc[:, 512:1024], op=ALU.mult)
                        op("scalar", "activation", [t512B[1]], [t512B[1]], out=t512[1], in_=t512[1], func=AF.Ln, bias=1.0)
                        op("scalar", "activation", [t512B[1]], [t512B[1]], out=t512[1], in_=t512[1], func=AF.Exp, scale=-1.0)
                        op("vector", "tensor_tensor", [xgB, t512B[1]], [gateB], out=gate, in0=xg, in1=t512[1], op=ALU.mult)

                        cs = bc_mid(cosT[:, gt, :], 8)
                        sn = bc_mid(sinT[:, gt, :], 8)
                        X1 = Xr[:, :, 0:16]
                        X2 = Xr[:, :, 16:32]
                        op("gpsimd", "tensor_tensor", [XrB, rotB], [rtB], out=rt0, in0=X2, in1=sn, op=ALU.mult)
                        op("gpsimd", "tensor_tensor", [XrB, rotB], [XoB], out=Xo[:, :, 0:16], in0=X1, in1=cs, op=ALU.mult)
                        op("gpsimd", "tensor_tensor", [rtB, XoB], [XoB], out=Xo[:, :, 0:16], in0=Xo[:, :, 0:16], in1=rt0, op=ALU.subtract)
                        op("gpsimd", "tensor_tensor", [XrB, rotB], [rtB], out=rt0, in0=X1, in1=sn, op=ALU.mult)
                        op("gpsimd", "tensor_tensor", [XrB, rotB, XoB], [XoB], out=Xo[:, :, 16:32], in0=X2, in1=cs, op=ALU.mult)
                        op("gpsimd", "tensor_tensor", [rtB, XoB], [XoB], out=Xo[:, :, 16:32], in0=Xo[:, :, 16:32], in1=rt0, op=ALU.add)
                        op("gpsimd", "tensor_tensor", [XoB, cfB], [qkrB], out=qkr, in0=Xo.rearrange("p a b -> p (a b)"), in1=retE, op=ALU.mult)
                        ck(2)
                        def chain_m():
                            op("scalar", "activation", [mB], [mB], out=t4, in_=ifp[:, 4:8], func=AF.Exp, scale=-1.0)
                            op("scalar", "activation", [mB], [mB], out=t4, in_=t4, func=AF.Ln, bias=1.0)
                            op("vector", "tensor_tensor", [mB, cfB], [RB], out=R, in0=bc_mid(ntri, 4), in1=bc_last(t4, 128), op=ALU.mult)
                            Bb, BbB = bank()
                            op("tensor", "matmul", [RB, cfB], [BbB], Bb, lhsT=ones, rhs=R.rearrange("p h i -> p (h i)"), start=True, stop=True)
                            bj, bjB = bank()
                            op("tensor", "matmul", [mB, cfB], [bjB], bj[:, 0:4], lhsT=ntri, rhs=t4, start=True, stop=True)
                            op("vector", "scalar_tensor_tensor", [bjB, mB], [mB], out=aj, in0=bj[:, 0:4], scalar=-1.0, in1=ifp[:, 0:4], op0=ALU.mult, op1=ALU.add)
                            Bb3 = Bb.rearrange("p (h i) -> p h i", h=4)
                            for hh in range(4):
                                op("scalar", "activation", [BbB, mB], [DTB], out=DT[:, hh, :], in_=Bb3[:, hh, :], func=AF.Exp, bias=aj[:, hh:hh + 1])
                            op("scalar", "activation", [BbB], [EB], out=E.rearrange("p h i -> p (h i)"), in_=Bb, func=AF.Exp)
                            op("vector", "tensor_tensor", [BbB, mB], [mB], out=wj, in0=Bb3[:, :, 127], in1=aj, op=ALU.add)
                            op("scalar", "activation", [mB], [mB], out=wj, in_=wj, func=AF.Exp)
                            Sc, ScB = bank()
                            for mt in range(2):
                                op("gpsimd", "tensor_tensor", [qkB, cfB], [QbdB], out=Qbd[:, 2 * mt:2 * mt + 2, :], in0=bc_mid(qkb[:, mt, c0:c0 + 128], 2),
                                   in1=bc_last(hm2, 128), op=ALU.mult)
                            for mt in range(2):
                                op("tensor", "matmul", [qkB, QbdB], [ScB], Sc[:, mt * 256:(mt + 1) * 256], lhsT=qkb[:, 2 + mt, c0:c0 + 128],
                                   rhs=Qbd[:, 2 * mt:2 * mt + 2, :].rearrange("p h i -> p (h i)"), start=True, stop=True)
                            tS = t512[2]
                            op("vector", "tensor_tensor", [ScB, cfB], [t512B[2]], out=tS.rearrange("p (h i) -> p h i", h=4), in0=Sc.rearrange("p (h i) -> p h i", h=4),
                               in1=bc_mid(tri, 4), op=ALU.mult)
                            op("vector", "tensor_tensor", [t512B[2], DTB], [PTB], out=PT.rearrange("p h i -> p (h i)"), in0=tS, in1=DT.rearrange("p h i -> p (h i)"), op=ALU.mult)
                            for mt in range(2):
                                for hh in range(2):
                                    r0 = hh * 64
                                    op("gpsimd", "tensor_tensor", [qkB, EB], [qpB], out=qp[r0:r0 + 64, mt, :], in0=qkb[r0:r0 + 64, mt, c0:c0 + 128], in1=E[r0:r0 + 64, 2 * mt + hh, :], op=ALU.mult)
                            O = [bank(), bank()]
                            for hh in range(4):
                                ob, oB = O[hh // 2]
                                r0 = (hh % 2) * 64
                                osl = ob[:, (hh % 2) * 129:(hh % 2) * 129 + 129]
                                op("tensor", "matmul", [PTB, vB], [oB], osl, lhsT=PT[:, hh, :], rhs=v_ext[:, hh, :], start=(hh % 2 == 0), stop=False, skip_group_check=True)
                                op("tensor", "matmul", [qpB, sB], [oB], osl, lhsT=qp[r0:r0 + 64, hh // 2, :], rhs=C_bf[r0:r0 + 64, hh // 2, :], start=False, stop=True, skip_group_check=True)
                            for b2 in range(2):
                                ob, oB = O[b2]
                                op("vector", "tensor_copy", [oB], [mB], out=den[:, 2 * b2:2 * b2 + 2], in_=ob[:, 0:258].rearrange("p (h c) -> p h c", h=2)[:, :, 128])
                            op("vector", "tensor_scalar", [mB], [mB], out=t4, in0=den, scalar1=-1.0, scalar2=None, op0=ALU.mult)
                            op("vector", "tensor_tensor", [mB], [mB], out=den, in0=den, in1=t4, op=ALU.max)
                            op("vector", "tensor_scalar", [mB], [mB], out=den, in0=den, scalar1=1.0, scalar2=None, op0=ALU.max)
                            op("vector", "reciprocal", [mB], [mB], out=den, in_=den)
                            op("gpsimd", "memset", [], [mB], ssm, 0.0)
                            for hh in range(4):
                                ob, oB = O[hh // 2]
                                op("scalar", "activation", [oB, mB], [junkmB, mB], out=junkm, in_=ob[:, (hh % 2) * 129:(hh % 2) * 129 + 128], func=AF.Square,
                                   accum_out=ssm[:, hh:hh + 1])
                            op("vector", "tensor_tensor", [mB], [mB], out=fac, in0=den, in1=den, op=ALU.mult)
                            op("vector", "tensor_tensor", [mB], [mB], out=fac, in0=fac, in1=ssm, op=ALU.mult)
                            rstd_from(fac, 128, fac, mB)
                            op("vector", "tensor_tensor", [mB], [mB], out=fac, in0=fac, in1=den, op=ALU.mult)
                            for hh in range(4):
                                ob, oB = O[hh // 2]
                                op("vector", "scalar_tensor_tensor", [oB, mB, ogB], [yB], out=ytile[:, hh * 128:(hh + 1) * 128], in0=ob[:, (hh % 2) * 129:(hh % 2) * 129 + 128],
                                   scalar=fac[:, hh:hh + 1], in1=og[:, hh * 128:(hh + 1) * 128], op0=ALU.mult, op1=ALU.mult)
                            Kp, KpB = bank()
                            Kpb = Kp.bitcast(BF16)
                            for mt in range(2):
                                op("tensor", "transpose", [qkB, cfB], [KpB], out=Kpb[:, mt * 128:(mt + 1) * 128], in_=qkb[:, 2 + mt, c0:c0 + 128], identity=identb)
                            op("vector", "tensor_tensor", [KpB, mB], [kpB], out=kp.rearrange("p (h d) -> p h d", h=4), in0=Kpb[:, 0:256].rearrange("p (h d) -> p h d", h=4),
                               in1=bc_last(wj, 64), op=ALU.mult)
                            U = [bank(), bank()]
                            for mt in range(2):
                                ub, uB = U[mt]
                                for hh in range(2):
                                    op("tensor", "matmul", [kpB, vB], [uB], ub[:, hh * 129:hh * 129 + 129], lhsT=kp[:, mt * 128:(mt + 1) * 128], rhs=v_ext[:, 2 * mt + hh, :], start=True, stop=True)
                                for hh in range(2):
                                    r0 = hh * 64
                                    cs_ = Cst[l][r0:r0 + 64, mt, :]
                                    op("vector", "scalar_tensor_tensor", [uB, EB, sB], [sB], out=cs_, in0=cs_, scalar=E[r0:r0 + 64, 2 * mt + hh, 127:128],
                                       in1=ub[r0:r0 + 64, hh * 129:hh * 129 + 129], op0=ALU.mult, op1=ALU.add)

                            op("gpsimd", "tensor_copy", [sB], [sB], out=C_bf, in_=Cst[l])

                        def chain_g():
                            Lp, LpB = bank()
                            op("tensor", "matmul", [gaB, smallB], [LpB], Lp[:, 0:128], lhsT=gaT[:, c0:c0 + 128], rhs=wup[:, l, :], start=True, stop=True)
                            op("vector", "tensor_tensor", [LpB, gbbB], [laB], out=la, in0=Lp[:, 0:128], in1=gbb, op=ALU.add)
                            op("scalar", "activation", [laB], [laB], out=la, in_=la, func=AF.Exp, scale=-1.0)
                            op("scalar", "activation", [laB], [laB], out=la, in_=la, func=AF.Ln, bias=1.0)
                            BT, BTB = bank()
                            op("tensor", "matmul", [laB, cfB], [BTB], BT[:, 0:128], lhsT=la, rhs=tri16, start=True, stop=True)
                            op("scalar", "activation", [BTB], [EpB], out=Ep, in_=BT[:, 0:128], func=AF.Exp)
                            op("scalar", "activation", [BTB], [EpB], out=En, in_=BT[:, 0:128], func=AF.Exp, scale=-1.0)
                            op("gpsimd", "tensor_tensor", [gqkB, EpB], [qtB], out=qt, in0=gqk[:, 0, c0:c0 + 128], in1=Ep, op=ALU.mult)
                            op("gpsimd", "tensor_tensor", [gqkB, EpB], [ktB], out=kt, in0=gqk[:, 1, c0:c0 + 128], in1=En, op=ALU.mult)
                            Tp, TpB = bank()
                            Tpb = Tp.bitcast(BF16)
                            op("tensor", "transpose", [ktB, cfB], [TpB], out=Tpb[:, 0:128], in_=kt, identity=identb)
                            op("scalar", "activation", [TpB], [ktokB], out=ktok, in_=Tpb[:, 0:128], func=AF.Copy)
                            core(l, c0, vgr[:, 0:256], ktok, ktokB, Sg[l], Sg_bf, Ep[:, 127:128], EpB, gate[:, 0:256], 512, CB_G)


                        def chain_r():
                            Tp2, Tp2B = bank()
                            Tp2b = Tp2.bitcast(BF16)
                            op("tensor", "transpose", [qkrB, cfB], [Tp2B], out=Tp2b[:, 0:128], in_=qkr[:, 0:128], identity=identb)
                            op("tensor", "transpose", [qkrB, cfB], [Tp2B], out=Tp2b[:, 128:256], in_=qkr[:, 128:256], identity=identb)
                            op("scalar", "activation", [Tp2B], [qtrB], out=qt_r, in_=Tp2b[:, 0:128], func=AF.Copy)
                            op("scalar", "activation", [Tp2B], [ktrB], out=kt_r, in_=Tp2b[:, 128:256], func=AF.Copy)
                            core(l, c0, vgr[:, 256:512], qkr[:, 128:256], qkrB, Sr[l], Sr_bf, elast_r, cfB, gate[:, 256:512], 768, CB_R)


                        run_interleaved([chain_m, chain_g, chain_r], [(0, 1, 2, 3), (4, 5), (6, 7)])
                        ck(5)
                        pb, pB = bank()
                        pbb = pb.bitcast(BF16)
                        for k in range(KC):
                            op("tensor", "transpose", [yB, cfB], [pB], out=pbb[:, k * 128:(k + 1) * 128], in_=ytile[:, k * 128:(k + 1) * 128], identity=identb)
                        op("scalar", "activation", [pB], [yTB], out=yT, in_=pbb.rearrange("p (k t) -> p k t", k=KC), func=AF.Copy)
                        W = [bank(), bank()]
                        for dh in range(2):
                            wb, wB = W[dh]
                            for k in range(KC):
                                op("tensor", "matmul", [yTB, woutB], [wB], wb, lhsT=yT[:, k, :], rhs=woutb[:, k, dh * 512:(dh + 1) * 512], start=(k == 0), stop=(k == KC - 1))
                        post_norm_residual([W[0][0], W[1][0]], [W[0][1], W[1][1]], t512[1], t512B[1], tt)

                ck(6)
                fence()
                load_gain(l * 4 + 2)
                for tt in range(NT):
                    norm_to_T(h[:, tt, :], hB[tt], utokf, utokfB, uTh[:, :, tt * 128:(tt + 1) * 128], uThB)
                load_gain(l * 4 + 3)
                ck(7)
                nxt = None
                if l + 1 < L:
                    nxt = l + 1
                elif part + 1 < run_parts:
                    nxt = 0
                for g in range(NG):
                    for tb in range(TH // 512):
                        for fc in range(4):
                            pb, pB = bank()
                            for k in range(KC):
                                op("tensor", "matmul", [ringB[0], uThB], [pB], pb, lhsT=w1v[:, k, fc * 128:(fc + 1) * 128], rhs=uTh[:, k, tb * 512:(tb + 1) * 512],
                                   start=(k == 0), stop=(k == KC - 1))
                            op("scalar", "activation", [pB], [rlB], out=rl, in_=pb, func=AF.Relu)
                            op("gpsimd", "tensor_tensor", [rlB], [hdnB], out=hdn[:, fc, tb * 512:(tb + 1) * 512], in0=rl, in1=rl, op=ALU.mult)
                    if g + 1 < NG:
                        load_w1(l, g + 1)
                    elif nxt is not None:
                        load_w1(nxt, 0)
                    for tt in range(NT):
                        for dh in range(2):
                            pb, pB = bank()
                            for fc in range(4):
                                op("tensor", "matmul", [hdnB, ringB[1]], [pB], pb, lhsT=hdn[:, fc, tt * 128:(tt + 1) * 128], rhs=w2v[:, fc, dh * 512:(dh + 1) * 512],
                                   start=(fc == 0), stop=(fc == 3))
                            a_ = acc[:, tt, dh * 512:(dh + 1) * 512]
                            if g == 0:
                                op("vector", "tensor_copy", [pB], [accB[tt]], out=a_, in_=pb)
                            else:
                                op("vector", "tensor_tensor", [pB, accB[tt]], [accB[tt]], out=a_, in0=pb, in1=a_, op=ALU.add)
                    if g + 1 < NG:
                        load_w2(l, g + 1)
                    elif nxt is not None:
                        load_w2(nxt, 0)
                    if g == 3 and nxt is not None:
                        load_mixer_weights(nxt)
                for tt in range(NT):
                    post_norm_residual([acc[:, tt, 0:512], acc[:, tt, 512:1024]], [accB[tt], accB[tt]], tf, tfB, tt)
                fence()
              except StopBuild:
                break

            osrc = out_d[t0:t0 + TH, :].rearrange("(t p) d -> p t d", p=128)
            outB = Buf("out")
            for q in range(NT):
                dma("sync", osrc[:, q, :], h[:, q, :], [hB[q]], [outB])
            S.add("sync", lambda e: None, [outB], [])

        S.emit(nc, st)
    return nc, S


def make_consts():
    cf = np.zeros((128, K_END), np.float32)
    p = np.arange(128)
    cf[:, K_ID:K_ID + 128] = np.eye(128, dtype=np.float32)
    tri = (p[:, None] <= p[None, :]).astype(np.float32)
    cf[:, K_TRI:K_TRI + 128] = tri
    cf[:, K_NTRI:K_NTRI + 128] = -tri
    cf[:, K_T16:K_T16 + 128] = -tri / 16.0
    cf[:, K_ONES:K_ONES + 128] = 1.0
    cf[:, K_HM:K_HM + 4] = (p[:, None] // 32 == np.arange(4)[None, :]).astype(np.float32)
    cf[:, K_BM:K_BM + 256] = (p[:, None] // 32 == (np.arange(256)[None, :] // 64)).astype(np.float32)
    lg = np.log1p(-np.exp2(-5.0 - np.arange(4, dtype=np.float64)))
    hd = np.arange(128) // 32
    tok = np.arange(128, dtype=np.float64)
    epos = np.exp((tok[:, None] + 1.0) * lg[hd][None, :])
    eneg = np.exp(-(tok[:, None] + 1.0) * lg[hd][None, :]) * (32.0 ** -0.5)
    cf[:, K_RE:K_RE + 128] = epos
    cf[:, K_RE + 128:K_RE + 256] = eneg
    cf[:, K_EL] = np.exp(128.0 * lg[hd])
    invf = (np.float32(10000.0) ** (-np.arange(0, 32, 2, dtype=np.float32) / np.float32(32))).astype(np.float32)
    cf[:, K_IF:K_IF + 16] = invf[None, :]
    cf[:, K_IF + 16:K_IF + 18] = (p[:, None] // 64 == np.arange(2)[None, :]).astype(np.float32)
    return cf


def prepare_inputs(inputs, n_layers=DEPTH):
    L = n_layers
    f32 = np.float32
    g = lambda k: np.asarray(inputs[k])
    gains = np.stack([g("norm_pre_mix")[:L], g("norm_post_mix")[:L], g("norm_pre_ffn")[:L], g("norm_post_ffn")[:L]], axis=1).reshape(L * 4, D).astype(f32)
    convT = np.ascontiguousarray(np.transpose(g("mlstm_conv_w")[:L], (2, 0, 1)).reshape(512, L * 4)).astype(f32)
    ifb = np.concatenate([g("mlstm_i_bias")[:L], g("mlstm_f_bias")[:L]], axis=1).reshape(1, L * 8).astype(f32)
    hn = np.concatenate([g("mlstm_norm")[:L], g("gla_norm")[:L], g("ret_norm")[:L]], axis=1).astype(f32)
    shared = {
        "gains": np.ascontiguousarray(gains),
        "w_in": np.ascontiguousarray(g("w_in")[:L], dtype=f32),
        "w_out": np.ascontiguousarray(g("w_out")[:L], dtype=f32),
        "w_ff1": np.ascontiguousarray(g("w_ff1")[:L], dtype=f32),
        "w_ff2": np.ascontiguousarray(g("w_ff2")[:L], dtype=f32),
        "convT": convT,
        "ifb": np.ascontiguousarray(ifb),
        "hn": np.ascontiguousarray(hn),
        "wup": np.ascontiguousarray(g("gla_w_up")[:L], dtype=f32),
        "gb": np.ascontiguousarray(g("gla_gate_bias")[:L].reshape(1, L * 128), dtype=f32),
        "cf": make_consts(),
    }
    x = g("x")
    pos = g("positions")
    maps = []
    for b in range(x.shape[0]):
        m = dict(shared)
        m["x"] = np.ascontiguousarray(x[b], dtype=f32)
        m["pos"] = np.ascontiguousarray(pos[b].reshape(T // 128, 128).T).astype(np.int32)
        maps.append(m)
    return maps


_CACHE = {}


def kernel(**inputs):
    if "nc" not in _CACHE:
        _CACHE["nc"] = build_program()[0]
    nc = _CACHE["nc"]
    maps = prepare_inputs(inputs)
    res = run_bass_kernel_spmd(nc, maps, core_ids=list(range(len(maps))))
    out = np.stack([np.asarray(r["out"]) for r in res.results], axis=0)
    return out.astype(np.float32)
```

```python
import numpy as np
from contextlib import ExitStack
import concourse.bass as bass
import concourse.mybir as mybir
from concourse.bass_utils import run_bass_kernel_spmd

F32 = mybir.dt.float32
BF16 = mybir.dt.bfloat16
I32 = mybir.dt.int32
AF = mybir.ActivationFunctionType
ALU = mybir.AluOpType
AX = mybir.AxisListType

ENGS = ["tensor", "vector", "scalar", "gpsimd", "sync"]
N_DMA_SEMS = 8

D = 1024
T = 2048
DEPTH = 4
NPART = 2
TH = T // NPART
NT = TH // 128
NBLK = TH // 512
KC = D // 128
INC = 3096
DFF = 4096
NG = 8
import os as _os
PIPE = int(_os.environ.get("KPIPE", "1"))
EPS = 1e-6
C_MQ, C_MK, C_MV, C_MIF, C_MO = 0, 256, 512, 1024, 1032
C_GQ, C_GK, C_GV, C_GA, C_GG = 1544, 1672, 1800, 2056, 2072
C_RQ, C_RK, C_RV, C_RG = 2328, 2456, 2584, 2840
K_ID, K_TRI, K_ONES, K_HM, K_BM, K_RE, K_EL, K_IF, K_NTRI, K_T16, K_END = 0, 128, 256, 384, 388, 644, 900, 901, 920, 1048, 1176


class Buf:
    __slots__ = ("name", "last_w", "readers", "excl")

    def __init__(self, name="", excl=False):
        self.name = name
        self.last_w = None
        self.readers = []
        self.excl = excl


class Op:
    __slots__ = ("eng", "fn", "deps", "signal", "ordinal", "dma", "dma_sem", "dma_val", "dma_prev")

    def __init__(self, eng, fn, dma):
        self.eng = eng
        self.fn = fn
        self.deps = set()
        self.signal = False
        self.ordinal = None
        self.dma = dma
        self.dma_sem = None
        self.dma_val = None
        self.dma_prev = 0


class Sched:
    def __init__(self, same_engine_sync=True):
        self.ops = {e: [] for e in ENGS}
        self.same_engine_sync = same_engine_sync
        self.n_dma = {e: 0 for e in ENGS}

    def add(self, eng, fn, reads=(), writes=(), dma=False):
        op = Op(eng, fn, dma)
        deps = set()
        for b in reads:
            if b.last_w is not None:
                deps.add(b.last_w)
            if b.excl:
                for r in b.readers:
                    if r.eng != eng:
                        deps.add(r)
        for b in writes:
            if b.last_w is not None:
                deps.add(b.last_w)
            deps.update(b.readers)
        for b in reads:
            b.readers.append(op)
        for b in writes:
            b.last_w = op
            b.readers = []
        deps.discard(op)
        pruned = set()
        for d in deps:
            if d.eng == eng and not d.dma:
                if eng == "tensor" or not self.same_engine_sync:
                    continue
            pruned.add(d)
        op.deps = pruned
        for d in pruned:
            d.signal = True
        if dma:
            n = self.n_dma[eng]
            self.n_dma[eng] = n + 1
            op.dma_sem = n % N_DMA_SEMS
            op.dma_val = 16 * (n // N_DMA_SEMS + 1)
            op.dma_prev = 16 * (n // N_DMA_SEMS)
        self.ops[eng].append(op)
        return op

    def emit(self, nc, stack):
        for e in ENGS:
            c = 0
            for op in self.ops[e]:
                if not op.dma and op.signal:
                    c += 1
                    op.ordinal = c
        esem = {e: stack.enter_context(nc.semaphore("es_" + e)) for e in ENGS}
        dsem = {e: [stack.enter_context(nc.semaphore("ds_%s_%d" % (e, i))) for i in range(N_DMA_SEMS)]
                for e in ENGS if self.n_dma[e] > 0}
        block = stack.enter_context(nc.Block())
        nwaits = [0]

        def run(ename, eng):
            known = {}
            for op in self.ops[ename]:
                waits = {}
                for d in op.deps:
                    if d.dma:
                        s = dsem[d.eng][d.dma_sem]
                        v = d.dma_val
                    else:
                        s = esem[d.eng]
                        v = d.ordinal
                    key = id(s)
                    if key not in waits or waits[key][1] < v:
                        waits[key] = (s, v)
                if op.dma and op.dma_prev > 0:
                    s = dsem[ename][op.dma_sem]
                    key = id(s)
                    if key not in waits or waits[key][1] < op.dma_prev:
                        waits[key] = (s, op.dma_prev)
                for key, (s, v) in waits.items():
                    if known.get(key, 0) >= v:
                        continue
                    known[key] = v
                    eng.wait_ge(s, v)
                    nwaits[0] += 1
                ins = op.fn(eng)
                if ins is None:
                    continue
                if op.dma:
                    ins.then_inc(dsem[ename][op.dma_sem], 16)
                elif op.signal:
                    ins.then_inc(esem[ename], 1)

        @block.sync
        def _(e):
            run("sync", e)

        @block.tensor
        def _(e):
            run("tensor", e)

        @block.vector
        def _(e):
            run("vector", e)

        @block.scalar
        def _(e):
            run("scalar", e)

        @block.gpsimd
        def _(e):
            run("gpsimd", e)
        self.nwaits = nwaits[0]


def _dsize(dt):
    return 2 if dt == BF16 else 4


class Arena:
    def __init__(self, big, lo, hi):
        self.big = big
        self.lo = lo
        self.cur = lo
        self.hi = hi

    def alloc(self, free_shape, dtype=F32, parts=128):
        n = 1
        for s in free_shape:
            n *= s
        nbytes = n * _dsize(dtype)
        nbytes_r = (nbytes + 31) // 32 * 32
        off = self.cur
        self.cur += nbytes_r
        assert self.cur <= self.hi, "arena overflow %d > %d" % (self.cur, self.hi)
        assert nbytes % 4 == 0
        v = self.big[0:parts, off // 4: off // 4 + nbytes // 4]
        if dtype != F32:
            v = v.bitcast(dtype)
        if len(free_shape) == 2:
            v = v.rearrange("p (a b) -> p a b", a=free_shape[0])
        elif len(free_shape) == 3:
            v = v.rearrange("p (a b c) -> p a b c", a=free_shape[0], b=free_shape[1])
        return v


def bc_mid(ap2, n):
    P, F = ap2.shape
    return ap2.rearrange("p (o f) -> p o f", o=1).broadcast_to([P, n, F])


def bc_last(ap2, n):
    P, H = ap2.shape
    return ap2.rearrange("p (h o) -> p h o", o=1).broadcast_to([P, H, n])


def build_program(n_layers=DEPTH, run_parts=NPART, stage=None):
    nc = bass.Bass("TRN2", target_bir_lowering=False)
    L = n_layers

    def din(name, shape, dt=F32):
        return nc.dram_tensor(name, shape, dt, kind="ExternalInput").ap()

    x_d = din("x", [T, D])
    pos_d = din("pos", [128, T // 128], I32)
    gains_d = din("gains", [L * 4, D])
    w_in_d = din("w_in", [L, D, INC])
    w_out_d = din("w_out", [L, D, D])
    w_ff1_d = din("w_ff1", [L, D, DFF])
    w_ff2_d = din("w_ff2", [L, DFF, D])
    convT_d = din("convT", [512, L * 4])
    ifb_d = din("ifb", [1, L * 8])
    hn_d = din("hn", [L, D])
    wup_d = din("wup", [L, 16, 128])
    gb_d = din("gb", [1, L * 128])
    cf_d = din("cf", [128, K_END])
    out_d = nc.dram_tensor("out", [T, D], F32, kind="ExternalOutput").ap()

    S = Sched()

    class StopBuild(Exception):
        pass

    def ck(n):
        if stage is not None and stage == n:
            raise StopBuild()

    defer = [None]

    def op(eng, method, reads, writes, *args, **kw):
        if defer[0] is not None:
            defer[0].append((eng, method, reads, writes, args, kw))
            return None
        return S.add(eng, lambda e: getattr(e, method)(*args, **kw), reads, writes)

    def run_interleaved(chains, pools):
        lists = []
        for c, pool in zip(chains, pools):
            defer[0] = []
            bank_pool[0] = pool
            c()
            lists.append(defer[0])
        defer[0] = None
        bank_pool[0] = None
        idx = [0] * len(lists)
        while True:
            best, bf = None, 2.0
            for i, lst in enumerate(lists):
                if idx[i] < len(lst):
                    f = idx[i] / float(len(lst))
                    if f < bf:
                        best, bf = i, f
            if best is None:
                break
            eng, method, reads, writes, args, kw = lists[best][idx[best]]
            idx[best] += 1
            op(eng, method, reads, writes, *args, **kw)

    def dma(eng, out, in_, reads, writes):
        return S.add(eng, lambda e: e.dma_start(out=out, in_=in_), reads, writes, dma=True)

    st = ExitStack()
    with st:
        total = nc.sbuf_bytes_remaining
        total = (total // 128) * 128 - 128
        big_h = nc.alloc_sbuf_tensor("big", [128, total // 4], F32)
        A = Arena(big_h, 0, total)

        psum = [nc.alloc_psum_tensor("ps%d" % i, [128, 512], F32) for i in range(8)]
        psB = [Buf("ps%d" % i, excl=True) for i in range(8)]
        bank_ctr = [0]

        bank_pool = [None]
        pool_ctr = {}

        def bank():
            if bank_pool[0] is not None:
                lst = bank_pool[0]
                k = pool_ctr.get(lst, 0)
                pool_ctr[lst] = k + 1
                i = lst[k % len(lst)]
            else:
                i = bank_ctr[0] % 8
                bank_ctr[0] += 1
            return psum[i][:], psB[i]

        h = A.alloc([NT, D])
        hB = [Buf("h%d" % i) for i in range(NT)]
        winb = A.alloc([KC, INC], BF16)
        winB = [Buf("win%d" % i) for i in range(4)]
        woutb = A.alloc([KC, D], BF16)
        woutB = Buf("wout")
        ring = [A.alloc([8 * 512], BF16) for _ in range(2)]
        ringB = [Buf("ringA"), Buf("ringB")]
        w1v = ring[0].rearrange("p (c f) -> p c f", c=8)
        w2v = ring[1].rearrange("p (c d) -> p c d", c=4)
        cf = A.alloc([K_END])
        cfB = Buf("cf")
        identb = A.alloc([128], BF16)
        cosT = A.alloc([T // 128, 16])
        sinT = A.alloc([T // 128, 16])
        rotB = Buf("rot")
        cw = A.alloc([4, L * 4])
        ifb = A.alloc([L * 8])
        gbb = A.alloc([128]); gbbB = Buf("gbb")
        wup = A.alloc([128], parts=16); wupB = Buf("wup")
        smallB = Buf("small")
        Cst = [A.alloc([2, 129]) for _ in range(L)]
        Sg = [A.alloc([256]) for _ in range(L)]
        Sr = [A.alloc([256]) for _ in range(L)]
        ctail = [A.alloc([4, 3]) for _ in range(L)]
        stC = [Buf("stC%d" % l) for l in range(L)]
        stG = [Buf("stG%d" % l) for l in range(L)]
        stR = [Buf("stR%d" % l) for l in range(L)]
        stT = [Buf("stT%d" % l) for l in range(L)]
        CbfB = Buf("C_bf"); SgbfB = Buf("Sg_bf"); SrbfB = Buf("Sr_bf")
        C_bf = A.alloc([2, 129], BF16)
        Sg_bf = A.alloc([256], BF16)
        Sr_bf = A.alloc([256], BF16)
        v_ext = A.alloc([4, 129], BF16)
        vB = Buf("v_ext")
        gbc = A.alloc([D])
        gbcB = Buf("gbc")
        gbc2 = A.alloc([D])
        gbc2B = Buf("gbc2")
        sm = A.alloc([64])
        smB = Buf("sm")
        ss = sm[:, 0:1]
        ss2 = sm[:, 1:3]
        rstd = sm[:, 3:4]
        ph_lo = A.cur
        ph_hi = total

        M = Arena(big_h, ph_lo, ph_hi)
        hnbc = M.alloc([D]); hnB = Buf("hn")
        uTb = M.alloc([KC, 512], BF16); uTB = Buf("uTb")
        ytile = M.alloc([D], BF16); yB = Buf("ytile")
        xq = M.alloc([4, 515]); xqB = Buf("xq")
        eq = xq[:, :, 0:512]
        yq = M.alloc([4, 512]); yqB = Buf("yq")
        qkb = M.alloc([4, 512], BF16); qkB = Buf("qkb")
        gqk = M.alloc([2, 512], BF16); gqkB = Buf("gqk")
        gaT = M.alloc([512], parts=16); gaB = Buf("gaT")
        ifp = M.alloc([8]); lf = M.alloc([4]); t4 = M.alloc([4]); aj = M.alloc([4]); wj = M.alloc([4])
        den = M.alloc([4]); ssm = M.alloc([4]); fac = M.alloc([4]); mB = Buf("msmall")
        og = M.alloc([512], BF16); ogB = Buf("og")
        t512 = [M.alloc([512]) for _ in range(3)]; t512B = [Buf("t512_%d" % i) for i in range(3)]
        gate = M.alloc([512], BF16); gateB = Buf("gate")
        xg = t512[0]; xgB = t512B[0]
        ABUF = [dict(v_ext=v_ext, vB=vB, ifp=ifp, ifpB=Buf("ifp0"), og=og, ogB=ogB, gate=gate, gateB=gateB),
                dict(v_ext=M.alloc([4, 129], BF16), vB=Buf("v_ext1"), ifp=M.alloc([8]), ifpB=Buf("ifp1"), og=M.alloc([512], BF16), ogB=Buf("og1"),
                     gate=M.alloc([512], BF16), gateB=Buf("gate1"))]
        R = t512[2].rearrange("p (h i) -> p h i", h=4); RB = t512B[2]
        vgr = M.alloc([512], BF16); vgrB = Buf("vgr")
        ABUF[0].update(vgr=vgr, vgrB=vgrB)
        ABUF[1].update(vgr=M.alloc([512], BF16), vgrB=Buf("vgr1"))
        Xr = M.alloc([8, 32]); XrB = Buf("Xr")
        Xo = t512[1][:, 0:256].rearrange("p (a b) -> p a b", a=8); XoB = t512B[1]
        rt0 = t512[1][:, 256:384].rearrange("p (a b) -> p a b", a=8); rtB = t512B[1]
        qkr = M.alloc([256], BF16); qkrB = Buf("qkr")
        ABUF[0].update(qkr=qkr, qkrB=qkrB)
        ABUF[1].update(qkr=M.alloc([256], BF16), qkrB=Buf("qkr1"))
        DT = M.alloc([4, 128]); DTB = Buf("DT")
        E = M.alloc([4, 128]); EB = Buf("E")
        PT = M.alloc([4, 128], BF16); PTB = Buf("PT")
        qp = M.alloc([2, 128], BF16); qpB = Buf("qp")
        kp = M.alloc([256], BF16); kpB = Buf("kp")
        yT = M.alloc([KC, 128], BF16); yTB = Buf("yT")
        qt = M.alloc([128], BF16); qtB = Buf("qt")
        kt = M.alloc([128], BF16); ktB = Buf("kt")
        ktok = M.alloc([128], BF16); ktokB = Buf("ktok")
        Qbd = M.alloc([4, 128], BF16); QbdB = Buf("Qbd")
        rs4 = M.alloc([4]); rs4B = Buf("rs4")
        yqf = yq.rearrange("p a b -> p (a b)")
        xqf = xq.rearrange("p a b -> p (a b)")

        def bfv(ap):
            return ap.bitcast(BF16)
        PT_g = bfv(yqf[:, 0:256]).rearrange("p (h i) -> p h i", h=4); PTgB = Buf("PT_g")
        PT_r = bfv(yqf[:, 256:512]).rearrange("p (h i) -> p h i", h=4); PTrB = Buf("PT_r")
        Qbd_g = bfv(yqf[:, 512:768]).rearrange("p (h i) -> p h i", h=4); QbdgB = Buf("Qbd_g")
        Qbd_r = bfv(yqf[:, 768:1024]).rearrange("p (h i) -> p h i", h=4); QbdrB = Buf("Qbd_r")
        tmp_g = yqf[:, 1024:1280]; tmpgB = Buf("tmp_g")
        tmp_r = yqf[:, 1280:1536]; tmprB = Buf("tmp_r")
        tU_g = yqf[:, 1536:1792]; tUgB = Buf("tU_g")
        tU_r = yqf[:, 1792:2048]; tUrB = Buf("tU_r")
        la = xqf[:, 0:128]; laB = Buf("laE")
        Ep = xqf[:, 128:256]; En = xqf[:, 256:384]; EpB = laB
        qt_r = bfv(xqf[:, 384:448]); qtrB = Buf("qt_r")
        kt_r = bfv(xqf[:, 448:512]); ktrB = Buf("kt_r")
        junkm = bfv(xqf[:, 512:576]); junkmB = Buf("junkm")
        rs4_r = xqf[:, 576:580]; rs4rB = Buf("rs4_r")
        Zbufs = [PTgB, PTrB, QbdgB, QbdrB, tmpgB, tmprB, tUgB, tUrB, laB, qtrB, ktrB, junkmB, rs4rB]
        CB_G = dict(PT=PT_g, PTB=PTgB, Qbd=Qbd_g, QbdB=QbdgB, tmp=tmp_g, tmpB=tmpgB, tU=tU_g, tUB=tUgB, rs4=rs4, rs4B=rs4B, qt=qt, qtB=qtB, kt=kt, ktB=ktB)
        CB_R = dict(PT=PT_r, PTB=PTrB, Qbd=Qbd_r, QbdB=QbdrB, tmp=tmp_r, tmpB=tmprB, tU=tU_r, tUB=tUrB, rs4=rs4_r, rs4B=rs4rB, qt=qt_r, qtB=qtrB, kt=kt_r, ktB=ktrB)
        mixer_bufs = [ABUF[i][k] for i in range(2) for k in ("vB", "ifpB", "ogB", "gateB", "vgrB", "qkrB")] + [hnB, uTB, yB, xqB, yqB, qkB, gqkB, gaB, mB, ogB, gateB, vgrB, XrB, qkrB, DTB, EB, PTB,
                      qpB, kpB, yTB, PTgB, PTrB, QbdgB, QbdrB, tmpgB, tmprB, tUgB, tUrB, laB, qtrB, ktrB, junkmB, rs4rB, qtB, ktB, ktokB, QbdB, rs4B] + t512B

        FA = Arena(big_h, ph_lo, ph_hi)
        acc = FA.alloc([NT, D]); accB = [Buf("acc%d" % i) for i in range(NT)]
        uTh = FA.alloc([KC, TH], BF16); uThB = Buf("uTh")
        hdn = FA.alloc([4, TH], BF16); hdnB = Buf("hdn")
        rl = FA.alloc([512]); rlB = Buf("rl")
        utokf = FA.alloc([D], BF16); utokfB = Buf("utok_f")
        tf = FA.alloc([512]); tfB = Buf("tf")
        ffn_bufs = accB + [uThB, hdnB, rlB, utokfB, tfB]
        print("SBUF: persistent %d, mixer %d, ffn %d, phase avail %d" % (ph_lo, M.cur - ph_lo, FA.cur - ph_lo, ph_hi - ph_lo))

        def fenceZ():
            op("gpsimd", "memset", [], [xqB, yqB] + Zbufs, sm[:, 9:10], 0.0)

        def fence():
            op("gpsimd", "memset", [], mixer_bufs + ffn_bufs, sm[:, 8:9], 0.0)

        dma("sync", cf, cf_d, [], [cfB])
        ident = cf[:, K_ID:K_ID + 128]
        tri = cf[:, K_TRI:K_TRI + 128]
        ones = cf[:, K_ONES:K_ONES + 128]
        hmask = cf[:, K_HM:K_HM + 4]
        bmask = cf[:, K_BM:K_BM + 256]
        retE = cf[:, K_RE:K_RE + 256]
        elast_r = cf[:, K_EL:K_EL + 1]
        invf = cf[:, K_IF:K_IF + 16]
        hm2 = cf[:, K_IF + 16:K_IF + 18]
        ntri = cf[:, K_NTRI:K_NTRI + 128]
        tri16 = cf[:, K_T16:K_T16 + 128]
        op("vector", "tensor_copy", [cfB], [cfB], out=identb, in_=ident)
        dma("sync", cw, convT_d.rearrange("(m p) k -> p m k", p=128), [], [smallB])
        dma("sync", ifb, ifb_d.partition_broadcast(128), [], [smallB])

        RA = Arena(big_h, ph_lo, ph_hi)
        NTT = T // 128
        posi = RA.alloc([NTT], I32)
        posf = RA.alloc([NTT])
        ang = RA.alloc([NTT, 16])
        tq = RA.alloc([NTT, 16])
        tqi = RA.alloc([NTT, 16], I32)
        tB = Buf("rot_tmp")
        dma("sync", posi, pos_d, [], [tB])
        op("vector", "tensor_copy", [tB], [tB], out=posf, in_=posi)
        op("vector", "tensor_tensor", [tB, cfB], [tB], out=ang, in0=bc_last(posf, 16), in1=bc_mid(invf, NTT), op=ALU.mult)
        TWO_PI = float(2 * np.pi)
        PI = float(np.pi)

        def make_trig(dst, shift):
            op("vector", "tensor_scalar", [tB], [tB], out=tq, in0=ang, scalar1=shift, scalar2=1.0 / TWO_PI, op0=ALU.add, op1=ALU.mult)
            op("vector", "tensor_copy", [tB], [tB], out=tqi, in_=tq)
            op("vector", "tensor_copy", [tB], [tB], out=tq, in_=tqi)
            op("vector", "scalar_tensor_tensor", [tB], [tB], out=tq, in0=tq, scalar=-TWO_PI, in1=ang, op0=ALU.mult, op1=ALU.add)
            op("vector", "tensor_scalar", [tB], [tB], out=tq, in0=tq, scalar1=shift, scalar2=None, op0=ALU.add)
            op("vector", "tensor_scalar", [tB], [rotB], out=dst, in0=tq, scalar1=PI, scalar2=-TWO_PI, op0=ALU.is_gt, op1=ALU.mult)
            op("vector", "tensor_tensor", [tB, rotB], [tB], out=tq, in0=tq, in1=dst, op=ALU.add)
            op("vector", "tensor_scalar", [tB], [rotB], out=dst, in0=tq, scalar1=-PI, scalar2=TWO_PI, op0=ALU.is_lt, op1=ALU.mult)
            op("vector", "tensor_tensor", [tB, rotB], [tB], out=tq, in0=tq, in1=dst, op=ALU.add)
            op("vector", "tensor_scalar", [tB], [tB], out=tq, in0=tq, scalar1=PI, scalar2=-PI, op0=ALU.min, op1=ALU.max)
            op("scalar", "activation", [tB], [rotB], out=dst, in_=tq, func=AF.Sin)

        make_trig(sinT, 0.0)
        make_trig(cosT, float(np.pi / 2))
        op("gpsimd", "memset", [], [tB] + mixer_bufs + ffn_bufs, sm[:, 8:9], 0.0)

        for l in range(L):
            op("gpsimd", "memset", [], [stC[l]], Cst[l], 0.0)
            op("gpsimd", "memset", [], [stG[l]], Sg[l], 0.0)
            op("gpsimd", "memset", [], [stR[l]], Sr[l], 0.0)
            op("gpsimd", "memset", [], [stT[l]], ctail[l], 0.0)
        op("gpsimd", "memset", [], [vB], v_ext, 1.0)
        op("gpsimd", "memset", [], [ABUF[1]["vB"]], ABUF[1]["v_ext"], 1.0)

        def load_mixer_weights(l):
            src = w_in_d[l].rearrange("(c p) n -> p c n", p=128)
            for q in range(4):
                dma("gpsimd", winb[:, 2 * q:2 * q + 2, :], src[:, 2 * q:2 * q + 2, :], [], [winB[q]])
            dma("gpsimd", woutb, w_out_d[l].rearrange("(c p) n -> p c n", p=128), [], [woutB])

        def load_w1(l, g):
            dma("gpsimd", w1v, w_ff1_d[l][:, g * 512:(g + 1) * 512].rearrange("(c p) f -> p c f", p=128), [], [ringB[0]])

        def load_w2(l, g):
            dma("gpsimd", w2v, w_ff2_d[l][g * 512:(g + 1) * 512, :].rearrange("(c p) d -> p c d", p=128), [], [ringB[1]])

        def load_gain(row):
            if row % 2 == 0:
                dma("sync", gbc, gains_d[row:row + 1, :].partition_broadcast(128), [], [gbcB])
            else:
                dma("sync", gbc2, gains_d[row:row + 1, :].partition_broadcast(128), [], [gbc2B])

        def rstd_from(ss_ap, n, out_ap, buf):
            op("scalar", "activation", [buf], [buf], out=out_ap, in_=ss_ap, func=AF.Ln, scale=1.0 / n, bias=EPS)
            op("scalar", "activation", [buf], [buf], out=out_ap, in_=out_ap, func=AF.Exp, scale=-0.5)

        def norm_to_T(src_ap, srcB, utok, utokB, dstT, dstB):
            op("gpsimd", "memset", [], [smB], ss, 0.0)
            op("scalar", "activation", [srcB, smB], [utokB, smB], out=utok, in_=src_ap, func=AF.Square, accum_out=ss)
            rstd_from(ss, D, rstd, smB)
            op("vector", "scalar_tensor_tensor", [srcB, smB, gbcB], [utokB], out=utok, in0=src_ap, scalar=rstd, in1=gbc, op0=ALU.mult, op1=ALU.mult)
            pb, pB = bank()
            pbb = pb.bitcast(BF16)
            for k in range(KC):
                op("tensor", "transpose", [utokB, cfB], [pB], out=pbb[:, k * 128:(k + 1) * 128], in_=utok[:, k * 128:(k + 1) * 128], identity=identb)
            op("scalar", "activation", [pB], [dstB], out=dstT, in_=pbb.rearrange("p (k t) -> p k t", k=KC), func=AF.Copy)

        def post_norm_residual(srcs, srcBs, tmp, tmpB, tt):
            op("gpsimd", "memset", [], [smB], ss2, 0.0)
            for dh in range(2):
                op("scalar", "activation", [srcBs[dh], smB], [tmpB, smB], out=tmp.bitcast(BF16)[:, 0:512], in_=srcs[dh], func=AF.Square, accum_out=ss2[:, dh:dh + 1])
            op("vector", "tensor_tensor", [smB], [smB], out=ss, in0=ss2[:, 0:1], in1=ss2[:, 1:2], op=ALU.add)
            rstd_from(ss, D, rstd, smB)
            for dh in range(2):
                op("vector", "scalar_tensor_tensor", [srcBs[dh], smB, gbc2B], [tmpB], out=tmp, in0=srcs[dh], scalar=rstd, in1=gbc2[:, dh * 512:(dh + 1) * 512], op0=ALU.mult, op1=ALU.mult)
                hs = h[:, tt, dh * 512:(dh + 1) * 512]
                op("vector", "tensor_tensor", [tmpB, hB[tt]], [hB[tt]], out=hs, in0=hs, in1=tmp, op=ALU.add)

        def core(l, c0, v_ap, ktok_ap, ktok_B, Sst, Sbf, elast_ap, elB, gate_ap, ycols, cb):
            sB = cb["stB"]
            SbfB_ = cb["SbfB"]
            PT_, PTB_, Qbd_, QbdB_ = cb["PT"], cb["PTB"], cb["Qbd"], cb["QbdB"]
            tmp, tmpB, tU, tUB, rs_, rsB_ = cb["tmp"], cb["tmpB"], cb["tU"], cb["tUB"], cb["rs4"], cb["rs4B"]
            qt_, qtB_, kt_, ktB_ = cb["qt"], cb["qtB"], cb["kt"], cb["ktB"]
            op("gpsimd", "tensor_tensor", [qtB_, cfB], [QbdB_], out=Qbd_, in0=bc_mid(qt_, 4), in1=bc_last(hmask, 128), op=ALU.mult)
            Sc, ScB = bank()
            op("tensor", "matmul", [ktB_, QbdB_], [ScB], Sc, lhsT=kt_, rhs=Qbd_.rearrange("p h i -> p (h i)"), start=True, stop=True)
            op("vector", "tensor_tensor", [ScB, cfB], [PTB_], out=PT_, in0=Sc.rearrange("p (h i) -> p h i", h=4), in1=bc_mid(tri, 4), op=ALU.mult)
            ob, oB = bank()
            for hh in range(4):
                op("tensor", "matmul", [PTB_, vgrB], [oB], ob[:, hh * 64:(hh + 1) * 64], lhsT=PT_[:, hh, :], rhs=v_ap[:, hh * 64:(hh + 1) * 64], start=(hh == 0), stop=False, skip_group_check=True)
            op("tensor", "matmul", [qtB_, SbfB_], [oB], ob[:, 0:256], lhsT=qt_, rhs=Sbf, start=False, stop=True, skip_group_check=True)
            o3 = ob[:, 0:256].rearrange("p (h e) -> p h e", h=4)
            tmp3 = tmp.rearrange("p (h e) -> p h e", h=4)
            op("scalar", "activation", [oB], [tmpB], out=tmp, in_=ob[:, 0:256], func=AF.Square)
            op("vector", "tensor_reduce", [tmpB], [rsB_], out=rs_, in_=tmp3, axis=AX.X, op=ALU.add)
            rstd_from(rs_, 64, rs_, rsB_)
            op("vector", "tensor_tensor", [oB, rsB_], [tmpB], out=tmp3, in0=o3, in1=bc_last(rs_, 64), op=ALU.mult)
            op("gpsimd", "tensor_tensor", [tmpB, gateB], [yB], out=ytile[:, ycols:ycols + 256], in0=tmp, in1=gate_ap, op=ALU.mult)
            ub, uB = bank()
            op("tensor", "matmul", [ktok_B, vgrB], [uB], ub[:, 0:256], lhsT=ktok_ap, rhs=v_ap, start=True, stop=True)
            op("vector", "scalar_tensor_tensor", [uB, elB, cfB], [tUB], out=tU, in0=ub[:, 0:256], scalar=elast_ap, in1=bmask, op0=ALU.mult, op1=ALU.mult)
            op("vector", "scalar_tensor_tensor", [sB, elB, tUB], [sB], out=Sst, in0=Sst, scalar=elast_ap, in1=tU, op0=ALU.mult, op1=ALU.add)
            op("gpsimd", "tensor_copy", [sB], [SbfB_], out=Sbf, in_=Sst)

        load_mixer_weights(0)
        load_w1(0, 0)
        load_w2(0, 0)
        for part in range(run_parts):
            t0 = part * TH
            xsrc = x_d[t0:t0 + TH, :].rearrange("(t p) d -> p t d", p=128)
            for q in range(NT):
                dma("sync", h[:, q, :], xsrc[:, q, :], [], [hB[q]])

            for l in range(L if stage != 0 else 0):
              try:
                load_gain(l * 4 + 0)
                load_gain(l * 4 + 1)
                op("gpsimd", "tensor_copy", [stC[l]], [CbfB], out=C_bf, in_=Cst[l])
                op("gpsimd", "tensor_copy", [stG[l]], [SgbfB], out=Sg_bf, in_=Sg[l])
                op("gpsimd", "tensor_copy", [stR[l]], [SrbfB], out=Sr_bf, in_=Sr[l])
                dma("sync", gbb, gb_d[:, l * 128:(l + 1) * 128].partition_broadcast(128), [], [gbbB])
                dma("sync", wup, wup_d[l], [], [wupB])
                dma("sync", hnbc, hn_d[l:l + 1, :].partition_broadcast(128), [], [hnB])
                for blk in range(NBLK):
                    for ti in range(4):
                        tt = blk * 4 + ti
                        norm_to_T(h[:, tt, :], hB[tt], ytile, yB, uTb[:, :, ti * 128:(ti + 1) * 128], uTB)
                    fenceZ()
                    op("gpsimd", "tensor_copy", [stT[l]], [xqB], out=xq[:, :, 0:3], in_=ctail[l])
                    bcols = [C_MQ, C_MQ + 128, C_MK, C_MK + 128, C_GQ, C_GK, C_GA]
                    bM = [128, 128, 128, 128, 128, 128, 16]
                    for i in range(7):
                        pb, pB = bank()
                        for k in range(KC):
                            op("tensor", "matmul", [winB[k // 2], uTB], [pB], pb[0:bM[i], :], lhsT=winb[:, k, bcols[i]:bcols[i] + bM[i]], rhs=uTb[:, k, :],
                               start=(k == 0), stop=(k == KC - 1))
                        if i < 4:
                            op("scalar", "activation", [pB], [xqB], out=xq[:, i, 3:515], in_=pb, func=AF.Copy)
                        elif i == 4:
                            op("scalar", "activation", [pB], [gqkB], out=gqk[:, 0, :], in_=pb, func=AF.Copy, scale=float(32 ** -0.5))
                        elif i == 5:
                            op("scalar", "activation", [pB], [gqkB], out=gqk[:, 1, :], in_=pb, func=AF.Copy)
                        else:
                            op("scalar", "activation", [pB], [gaB], out=gaT, in_=pb[0:16, :], func=AF.Copy)
                    for i in range(4):
                        op("scalar", "activation", [xqB, smallB], [yqB], out=yq[:, i, :], in_=xq[:, i, 3:515], func=AF.Copy, scale=cw[:, i, l * 4 + 3:l * 4 + 4])
                        for s in (2, 1, 0):
                            op("vector", "scalar_tensor_tensor", [xqB, smallB, yqB], [yqB], out=yq[:, i, :], in0=xq[:, i, s:s + 512], scalar=cw[:, i, l * 4 + s:l * 4 + s + 1],
                               in1=yq[:, i, :], op0=ALU.mult, op1=ALU.add)
                    op("gpsimd", "tensor_copy", [xqB], [stT[l]], out=ctail[l], in_=xq[:, :, 512:515])
                    op("scalar", "activation", [yqB], [xqB], out=eq, in_=yq, func=AF.Exp, scale=-1.0)
                    op("scalar", "activation", [xqB], [xqB], out=eq, in_=eq, func=AF.Ln, bias=1.0)
                    op("scalar", "activation", [xqB], [xqB], out=eq, in_=eq, func=AF.Exp, scale=-1.0)
                    op("vector", "tensor_tensor", [yqB, xqB], [qkB], out=qkb[:, 0:2, :], in0=yq[:, 0:2, :], in1=eq[:, 0:2, :], op=ALU.mult)
                    op("vector", "scalar_tensor_tensor", [yqB, xqB], [qkB], out=qkb[:, 2:4, :], in0=yq[:, 2:4, :], scalar=0.125, in1=eq[:, 2:4, :], op0=ALU.mult, op1=ALU.mult)

                    fenceZ()
                    ck(1)
                    def A_phase(ti, b):
                        tt = blk * 4 + ti
                        gt = part * NT + tt
                        c0 = ti * 128

                        def amm(c_lo, n):
                            pb, pB = bank()
                            for k in range(KC):
                                op("tensor", "matmul", [winB[k // 2], uTB], [pB], pb[:, 0:n], lhsT=uTb[:, k, c0:c0 + 128], rhs=winb[:, k, c_lo:c_lo + n],
                                   start=(k == 0), stop=(k == KC - 1))
                            return pb, pB
                        pb, pB = amm(C_MV, 512)
                        op("scalar", "activation", [pB], [b["vB"]], out=b["v_ext"][:, :, 0:128], in_=pb.rearrange("p (h e) -> p h e", h=4), func=AF.Copy)
                        pb, pB = amm(C_MIF, 8)
                        op("vector", "tensor_tensor", [pB, smallB], [b["ifpB"]], out=b["ifp"], in0=pb[:, 0:8], in1=ifb[:, l * 8:(l + 1) * 8], op=ALU.add)
                        pb, pB = amm(C_MO, 512)
                        op("scalar", "activation", [pB], [t512B[0]], out=t512[0], in_=pb, func=AF.Exp, scale=-1.0)
                        op("scalar", "activation", [t512B[0]], [t512B[0]], out=t512[0], in_=t512[0], func=AF.Ln, bias=1.0)
                        op("scalar", "activation", [t512B[0]], [t512B[0]], out=t512[0], in_=t512[0], func=AF.Exp, scale=-1.0)
                        op("vector", "tensor_tensor", [t512B[0], hnB], [b["ogB"]], out=b["og"], in0=t512[0], in1=hnbc[:, 0:512], op=ALU.mult)
                        pb, pB = amm(C_GV, 256)
                        op("scalar", "activation", [pB], [b["vgrB"]], out=b["vgr"][:, 0:256], in_=pb[:, 0:256], func=AF.Copy)
                        pb, pB = amm(C_GG, 512)
                        op("scalar", "activation", [pB], [xgB], out=xg[:, 0:256], in_=pb[:, 0:256], func=AF.Copy)
                        op("scalar", "activation", [pB], [t512B[1]], out=t512[1][:, 0:256], in_=pb[:, 0:256], func=AF.Exp, scale=-1.0)
                        op("scalar", "activation", [pB], [XrB], out=Xr.rearrange("p a b -> p (a b)"), in_=pb[:, 256:512], func=AF.Copy)
                        pb, pB = amm(C_RV, 512)
                        op("scalar", "activation", [pB], [b["vgrB"]], out=b["vgr"][:, 256:512], in_=pb[:, 0:256], func=AF.Copy)
                        op("scalar", "activation", [pB], [xgB], out=xg[:, 256:512], in_=pb[:, 256:512], func=AF.Copy)
                        op("scalar", "activation", [pB], [t512B[1]], out=t512[1][:, 256:512], in_=pb[:, 256:512], func=AF.Exp, scale=-1.0)
                        op("gpsimd", "tensor_tensor", [xgB, hnB], [xgB], out=xg, in0=xg, in1=hnbc[:, 512:1024], op=ALU.mult)
                        op("scalar", "activation", [t512B[1]], [t512B[1]], out=t512[1], in_=t512[1], func=AF.Ln, bias=1.0)
                        op("scalar", "activation", [t512B[1]], [t512B[1]], out=t512[1], in_=t512[1], func=AF.Exp, scale=-1.0)
                        op("vector", "tensor_tensor", [xgB, t512B[1]], [b["gateB"]], out=b["gate"], in0=xg, in1=t512[1], op=ALU.mult)
                        cs = bc_mid(cosT[:, gt, :], 8)
                        sn = bc_mid(sinT[:, gt, :], 8)
                        X1 = Xr[:, :, 0:16]
                        X2 = Xr[:, :, 16:32]
                        op("gpsimd", "tensor_tensor", [XrB, rotB], [rtB], out=rt0, in0=X2, in1=sn, op=ALU.mult)
                        op("gpsimd", "tensor_tensor", [XrB, rotB], [XoB], out=Xo[:, :, 0:16], in0=X1, in1=cs, op=ALU.mult)
                        op("gpsimd", "tensor_tensor", [rtB, XoB], [XoB], out=Xo[:, :, 0:16], in0=Xo[:, :, 0:16], in1=rt0, op=ALU.subtract)
                        op("gpsimd", "tensor_tensor", [XrB, rotB], [rtB], out=rt0, in0=X1, in1=sn, op=ALU.mult)
                        op("gpsimd", "tensor_tensor", [XrB, rotB, XoB], [XoB], out=Xo[:, :, 16:32], in0=X2, in1=cs, op=ALU.mult)
                        op("gpsimd", "tensor_tensor", [rtB, XoB], [XoB], out=Xo[:, :, 16:32], in0=Xo[:, :, 16:32], in1=rt0, op=ALU.add)
                        op("gpsimd", "tensor_tensor", [XoB, cfB], [b["qkrB"]], out=b["qkr"], in0=Xo.rearrange("p a b -> p (a b)"), in1=retE, op=ALU.mult)

                    if PIPE:
                        A_phase(0, ABUF[0])
                    for ti in range(4):
                        if not PIPE:
                            A_phase(ti, ABUF[ti % 2])
                        tt = blk * 4 + ti
                        gt = part * NT + tt
                        c0 = ti * 128
                        cur = ABUF[ti % 2]
                        v_ext, vB, ifp, ifpB, og, ogB = cur["v_ext"], cur["vB"], cur["ifp"], cur["ifpB"], cur["og"], cur["ogB"]
                        gate, gateB, vgr, vgrB, qkr, qkrB = cur["gate"], cur["gateB"], cur["vgr"], cur["vgrB"], cur["qkr"], cur["qkrB"]
                        def chain_m():
                            op("scalar", "activation", [mB, ifpB], [mB], out=t4, in_=ifp[:, 4:8], func=AF.Exp, scale=-1.0)
                            op("scalar", "activation", [mB], [mB], out=t4, in_=t4, func=AF.Ln, bias=1.0)
                            op("vector", "tensor_tensor", [mB, cfB], [RB], out=R, in0=bc_mid(ntri, 4), in1=bc_last(t4, 128), op=ALU.mult)
                            Bb, BbB = bank()
                            op("tensor", "matmul", [RB, cfB], [BbB], Bb, lhsT=ones, rhs=R.rearrange("p h i -> p (h i)"), start=True, stop=True)
                            bj, bjB = bank()
                            op("tensor", "matmul", [mB, cfB], [bjB], bj[:, 0:4], lhsT=ntri, rhs=t4, start=True, stop=True)
                            op("vector", "scalar_tensor_tensor", [bjB, mB, ifpB], [mB], out=aj, in0=bj[:, 0:4], scalar=-1.0, in1=ifp[:, 0:4], op0=ALU.mult, op1=ALU.add)
                            Bb3 = Bb.rearrange("p (h i) -> p h i", h=4)
                            for hh in range(4):
                                op("scalar", "activation", [BbB, mB], [DTB], out=DT[:, hh, :], in_=Bb3[:, hh, :], func=AF.Exp, bias=aj[:, hh:hh + 1])
                            op("scalar", "activation", [BbB], [EB], out=E.rearrange("p h i -> p (h i)"), in_=Bb, func=AF.Exp)
                            op("vector", "tensor_tensor", [BbB, mB], [mB], out=wj, in0=Bb3[:, :, 127], in1=aj, op=ALU.add)
                            op("scalar", "activation", [mB], [mB], out=wj, in_=wj, func=AF.Exp)
                            Sc, ScB = bank()
                            for mt in range(2):
                                op("gpsimd", "tensor_tensor", [qkB, cfB], [QbdB], out=Qbd[:, 2 * mt:2 * mt + 2, :], in0=bc_mid(qkb[:, mt, c0:c0 + 128], 2),
                                   in1=bc_last(hm2, 128), op=ALU.mult)
                            for mt in range(2):
                                op("tensor", "matmul", [qkB, QbdB], [ScB], Sc[:, mt * 256:(mt + 1) * 256], lhsT=qkb[:, 2 + mt, c0:c0 + 128],
                                   rhs=Qbd[:, 2 * mt:2 * mt + 2, :].rearrange("p h i -> p (h i)"), start=True, stop=True)
                            tS = t512[2]
                            op("vector", "tensor_tensor", [ScB, cfB], [t512B[2]], out=tS.rearrange("p (h i) -> p h i", h=4), in0=Sc.rearrange("p (h i) -> p h i", h=4),
                               in1=bc_mid(tri, 4), op=ALU.mult)
                            op("vector", "tensor_tensor", [t512B[2], DTB], [PTB], out=PT.rearrange("p h i -> p (h i)"), in0=tS, in1=DT.rearrange("p h i -> p (h i)"), op=ALU.mult)
                            for mt in range(2):
                                for hh in range(2):
                                    r0 = hh * 64
                                    op("gpsimd", "tensor_tensor", [qkB, EB], [qpB], out=qp[r0:r0 + 64, mt, :], in0=qkb[r0:r0 + 64, mt, c0:c0 + 128], in1=E[r0:r0 + 64, 2 * mt + hh, :], op=ALU.mult)
                            O = [bank(), bank()]
                            for hh in range(4):
                                ob, oB = O[hh // 2]
                                r0 = (hh % 2) * 64
                                osl = ob[:, (hh % 2) * 129:(hh % 2) * 129 + 129]
                                op("tensor", "matmul", [PTB, vB], [oB], osl, lhsT=PT[:, hh, :], rhs=v_ext[:, hh, :], start=(hh % 2 == 0), stop=False, skip_group_check=True)
                                op("tensor", "matmul", [qpB, CbfB], [oB], osl, lhsT=qp[r0:r0 + 64, hh // 2, :], rhs=C_bf[r0:r0 + 64, hh // 2, :], start=False, stop=True, skip_group_check=True)
                            for b2 in range(2):
                                ob, oB = O[b2]
                                op("vector", "tensor_copy", [oB], [mB], out=den[:, 2 * b2:2 * b2 + 2], in_=ob[:, 0:258].rearrange("p (h c) -> p h c", h=2)[:, :, 128])
                            op("vector", "tensor_scalar", [mB], [mB], out=t4, in0=den, scalar1=-1.0, scalar2=None, op0=ALU.mult)
                            op("vector", "tensor_tensor", [mB], [mB], out=den, in0=den, in1=t4, op=ALU.max)
                            op("vector", "tensor_scalar", [mB], [mB], out=den, in0=den, scalar1=1.0, scalar2=None, op0=ALU.max)
                            op("vector", "reciprocal", [mB], [mB], out=den, in_=den)
                            op("gpsimd", "memset", [], [mB], ssm, 0.0)
                            for hh in range(4):
                                ob, oB = O[hh // 2]
                                op("scalar", "activation", [oB, mB], [junkmB, mB], out=junkm, in_=ob[:, (hh % 2) * 129:(hh % 2) * 129 + 128], func=AF.Square,
                                   accum_out=ssm[:, hh:hh + 1])
                            op("vector", "tensor_tensor", [mB], [mB], out=fac, in0=den, in1=den, op=ALU.mult)
                            op("vector", "tensor_tensor", [mB], [mB], out=fac, in0=fac, in1=ssm, op=ALU.mult)
                            rstd_from(fac, 128, fac, mB)
                            op("vector", "tensor_tensor", [mB], [mB], out=fac, in0=fac, in1=den, op=ALU.mult)
                            for hh in range(4):
                                ob, oB = O[hh // 2]
                                op("vector", "scalar_tensor_tensor", [oB, mB, ogB], [yB], out=ytile[:, hh * 128:(hh + 1) * 128], in0=ob[:, (hh % 2) * 129:(hh % 2) * 129 + 128],
                                   scalar=fac[:, hh:hh + 1], in1=og[:, hh * 128:(hh + 1) * 128], op0=ALU.mult, op1=ALU.mult)
                            Kp, KpB = bank()
                            Kpb = Kp.bitcast(BF16)
                            for mt in range(2):
                                op("tensor", "transpose", [qkB, cfB], [KpB], out=Kpb[:, mt * 128:(mt + 1) * 128], in_=qkb[:, 2 + mt, c0:c0 + 128], identity=identb)
                            op("vector", "tensor_tensor", [KpB, mB], [kpB], out=kp.rearrange("p (h d) -> p h d", h=4), in0=Kpb[:, 0:256].rearrange("p (h d) -> p h d", h=4),
                               in1=bc_last(wj, 64), op=ALU.mult)
                            U = [bank(), bank()]
                            for mt in range(2):
                                ub, uB = U[mt]
                                for hh in range(2):
                                    op("tensor", "matmul", [kpB, vB], [uB], ub[:, hh * 129:hh * 129 + 129], lhsT=kp[:, mt * 128:(mt + 1) * 128], rhs=v_ext[:, 2 * mt + hh, :], start=True, stop=True)
                                for hh in range(2):
                                    r0 = hh * 64
                                    cs_ = Cst[l][r0:r0 + 64, mt, :]
                                    op("vector", "scalar_tensor_tensor", [uB, EB, stC[l]], [stC[l]], out=cs_, in0=cs_, scalar=E[r0:r0 + 64, 2 * mt + hh, 127:128],
                                       in1=ub[r0:r0 + 64, hh * 129:hh * 129 + 129], op0=ALU.mult, op1=ALU.add)

                            op("gpsimd", "tensor_copy", [stC[l]], [CbfB], out=C_bf, in_=Cst[l])

                        def chain_g():
                            Lp, LpB = bank()
                            op("tensor", "matmul", [gaB, wupB], [LpB], Lp[:, 0:128], lhsT=gaT[:, c0:c0 + 128], rhs=wup, start=True, stop=True)
                            op("vector", "tensor_tensor", [LpB, gbbB], [laB], out=la, in0=Lp[:, 0:128], in1=gbb, op=ALU.add)
                            op("scalar", "activation", [laB], [laB], out=la, in_=la, func=AF.Exp, scale=-1.0)
                            op("scalar", "activation", [laB], [laB], out=la, in_=la, func=AF.Ln, bias=1.0)
                            BT, BTB = bank()
                            op("tensor", "matmul", [laB, cfB], [BTB], BT[:, 0:128], lhsT=la, rhs=tri16, start=True, stop=True)
                            op("scalar", "activation", [BTB], [EpB], out=Ep, in_=BT[:, 0:128], func=AF.Exp)
                            op("scalar", "activation", [BTB], [EpB], out=En, in_=BT[:, 0:128], func=AF.Exp, scale=-1.0)
                            op("gpsimd", "tensor_tensor", [gqkB, EpB], [qtB], out=qt, in0=gqk[:, 0, c0:c0 + 128], in1=Ep, op=ALU.mult)
                            op("gpsimd", "tensor_tensor", [gqkB, EpB], [ktB], out=kt, in0=gqk[:, 1, c0:c0 + 128], in1=En, op=ALU.mult)
                            Tp, TpB = bank()
                            Tpb = Tp.bitcast(BF16)
                            op("tensor", "transpose", [ktB, cfB], [TpB], out=Tpb[:, 0:128], in_=kt, identity=identb)
                            op("scalar", "activation", [TpB], [ktokB], out=ktok, in_=Tpb[:, 0:128], func=AF.Copy)
                            core(l, c0, vgr[:, 0:256], ktok, ktokB, Sg[l], Sg_bf, Ep[:, 127:128], EpB, gate[:, 0:256], 512, dict(CB_G, stB=stG[l], SbfB=SgbfB))


                        def chain_r():
                            Tp2, Tp2B = bank()
                            Tp2b = Tp2.bitcast(BF16)
                            op("tensor", "transpose", [qkrB, cfB], [Tp2B], out=Tp2b[:, 0:128], in_=qkr[:, 0:128], identity=identb)
                            op("tensor", "transpose", [qkrB, cfB], [Tp2B], out=Tp2b[:, 128:256], in_=qkr[:, 128:256], identity=identb)
                            op("scalar", "activation", [Tp2B], [qtrB], out=qt_r, in_=Tp2b[:, 0:128], func=AF.Copy)
                            op("scalar", "activation", [Tp2B], [ktrB], out=kt_r, in_=Tp2b[:, 128:256], func=AF.Copy)
                            core(l, c0, vgr[:, 256:512], qkr[:, 128:256], qkrB, Sr[l], Sr_bf, elast_r, cfB, gate[:, 256:512], 768, dict(CB_R, stB=stR[l], SbfB=SrbfB))


                        chains = [chain_m, chain_g, chain_r]
                        pools = [(0, 1), (2, 3), (4, 5)]
                        if ti < 3 and PIPE:
                            chains.append(lambda ti=ti: A_phase(ti + 1, ABUF[(ti + 1) % 2]))
                            pools.append((6, 7))
                        run_interleaved(chains, pools)
                        ck(5)
                        pb, pB = bank()
                        pbb = pb.bitcast(BF16)
                        for k in range(KC):
                            op("tensor", "transpose", [yB, cfB], [pB], out=pbb[:, k * 128:(k + 1) * 128], in_=ytile[:, k * 128:(k + 1) * 128], identity=identb)
                        op("scalar", "activation", [pB], [yTB], out=yT, in_=pbb.rearrange("p (k t) -> p k t", k=KC), func=AF.Copy)
                        W = [bank(), bank()]
                        for dh in range(2):
                            wb, wB = W[dh]
                            for k in range(KC):
                                op("tensor", "matmul", [yTB, woutB], [wB], wb, lhsT=yT[:, k, :], rhs=woutb[:, k, dh * 512:(dh + 1) * 512], start=(k == 0), stop=(k == KC - 1))
                        post_norm_residual([W[0][0], W[1][0]], [W[0][1], W[1][1]], t512[1], t512B[1], tt)

                ck(6)
                fence()
                load_gain(l * 4 + 2)
                for tt in range(NT):
                    norm_to_T(h[:, tt, :], hB[tt], utokf, utokfB, uTh[:, :, tt * 128:(tt + 1) * 128], uThB)
                load_gain(l * 4 + 3)
                ck(7)
                nxt = None
                if l + 1 < L:
                    nxt = l + 1
                elif part + 1 < run_parts:
                    nxt = 0
                for g in range(NG):
                    for tb in range(TH // 512):
                        for fc in range(4):
                            pb, pB = bank()
                            for k in range(KC):
                                op("tensor", "matmul", [ringB[0], uThB], [pB], pb, lhsT=w1v[:, k, fc * 128:(fc + 1) * 128], rhs=uTh[:, k, tb * 512:(tb + 1) * 512],
                                   start=(k == 0), stop=(k == KC - 1))
                            op("scalar", "activation", [pB], [rlB], out=rl, in_=pb, func=AF.Relu)
                            op("gpsimd", "tensor_tensor", [rlB], [hdnB], out=hdn[:, fc, tb * 512:(tb + 1) * 512], in0=rl, in1=rl, op=ALU.mult)
                    if g + 1 < NG:
                        load_w1(l, g + 1)
                    elif nxt is not None:
                        load_w1(nxt, 0)
                    for tt in range(NT):
                        for dh in range(2):
                            pb, pB = bank()
                            for fc in range(4):
                                op("tensor", "matmul", [hdnB, ringB[1]], [pB], pb, lhsT=hdn[:, fc, tt * 128:(tt + 1) * 128], rhs=w2v[:, fc, dh * 512:(dh + 1) * 512],
                                   start=(fc == 0), stop=(fc == 3))
                            a_ = acc[:, tt, dh * 512:(dh + 1) * 512]
                            if g == 0:
                                op("vector", "tensor_copy", [pB], [accB[tt]], out=a_, in_=pb)
                            else:
                                op("vector", "tensor_tensor", [pB, accB[tt]], [accB[tt]], out=a_, in0=pb, in1=a_, op=ALU.add)
                    if g + 1 < NG:
                        load_w2(l, g + 1)
                    elif nxt is not None:
                        load_w2(nxt, 0)
                    if g == 3 and nxt is not None:
                        load_mixer_weights(nxt)
                for tt in range(NT):
                    post_norm_residual([acc[:, tt, 0:512], acc[:, tt, 512:1024]], [accB[tt], accB[tt]], tf, tfB, tt)
                fence()
              except StopBuild:
                break

            osrc = out_d[t0:t0 + TH, :].rearrange("(t p) d -> p t d", p=128)
            outB = Buf("out")
            for q in range(NT):
                dma("sync", osrc[:, q, :], h[:, q, :], [hB[q]], [outB])
            S.add("sync", lambda e: None, [outB], [])

        S.emit(nc, st)
    return nc, S


def make_consts():
    cf = np.zeros((128, K_END), np.float32)
    p = np.arange(128)
    cf[:, K_ID:K_ID + 128] = np.eye(128, dtype=np.float32)
    tri = (p[:, None] <= p[None, :]).astype(np.float32)
    cf[:, K_TRI:K_TRI + 128] = tri
    cf[:, K_NTRI:K_NTRI + 128] = -tri
    cf[:, K_T16:K_T16 + 128] = -tri / 16.0
    cf[:, K_ONES:K_ONES + 128] = 1.0
    cf[:, K_HM:K_HM + 4] = (p[:, None] // 32 == np.arange(4)[None, :]).astype(np.float32)
    cf[:, K_BM:K_BM + 256] = (p[:, None] // 32 == (np.arange(256)[None, :] // 64)).astype(np.float32)
    lg = np.log1p(-np.exp2(-5.0 - np.arange(4, dtype=np.float64)))
    hd = np.arange(128) // 32
    tok = np.arange(128, dtype=np.float64)
    epos = np.exp((tok[:, None] + 1.0) * lg[hd][None, :])
    eneg = np.exp(-(tok[:, None] + 1.0) * lg[hd][None, :]) * (32.0 ** -0.5)
    cf[:, K_RE:K_RE + 128] = epos
    cf[:, K_RE + 128:K_RE + 256] = eneg
    cf[:, K_EL] = np.exp(128.0 * lg[hd])
    invf = (np.float32(10000.0) ** (-np.arange(0, 32, 2, dtype=np.float32) / np.float32(32))).astype(np.float32)
    cf[:, K_IF:K_IF + 16] = invf[None, :]
    cf[:, K_IF + 16:K_IF + 18] = (p[:, None] // 64 == np.arange(2)[None, :]).astype(np.float32)
    return cf


def prepare_inputs(inputs, n_layers=DEPTH):
    L = n_layers
    f32 = np.float32
    g = lambda k: np.asarray(inputs[k])
    gains = np.stack([g("norm_pre_mix")[:L], g("norm_post_mix")[:L], g("norm_pre_ffn")[:L], g("norm_post_ffn")[:L]], axis=1).reshape(L * 4, D).astype(f32)
    convT = np.ascontiguousarray(np.transpose(g("mlstm_conv_w")[:L], (2, 0, 1)).reshape(512, L * 4)).astype(f32)
    ifb = np.concatenate([g("mlstm_i_bias")[:L], g("mlstm_f_bias")[:L]], axis=1).reshape(1, L * 8).astype(f32)
    hn = np.concatenate([g("mlstm_norm")[:L], g("gla_norm")[:L], g("ret_norm")[:L]], axis=1).astype(f32)
    shared = {
        "gains": np.ascontiguousarray(gains),
        "w_in": np.ascontiguousarray(g("w_in")[:L], dtype=f32),
        "w_out": np.ascontiguousarray(g("w_out")[:L], dtype=f32),
        "w_ff1": np.ascontiguousarray(g("w_ff1")[:L], dtype=f32),
        "w_ff2": np.ascontiguousarray(g("w_ff2")[:L], dtype=f32),
        "convT": convT,
        "ifb": np.ascontiguousarray(ifb),
        "hn": np.ascontiguousarray(hn),
        "wup": np.ascontiguousarray(g("gla_w_up")[:L], dtype=f32),
        "gb": np.ascontiguousarray(g("gla_gate_bias")[:L].reshape(1, L * 128), dtype=f32),
        "cf": make_consts(),
    }
    x = g("x")
    pos = g("positions")
    maps = []
    for b in range(x.shape[0]):
        m = dict(shared)
        m["x"] = np.ascontiguousarray(x[b], dtype=f32)
        m["pos"] = np.ascontiguousarray(pos[b].reshape(T // 128, 128).T).astype(np.int32)
        maps.append(m)
    return maps


_CACHE = {}


def kernel(**inputs):
    if "nc" not in _CACHE:
        _CACHE["nc"] = build_program()[0]
    nc = _CACHE["nc"]
    maps = prepare_inputs(inputs)
    res = run_bass_kernel_spmd(nc, maps, core_ids=list(range(len(maps))))
    out = np.stack([np.asarray(r["out"]) for r in res.results], axis=0)
    return out.astype(np.float32)
```

```python
import numpy as np
from contextlib import ExitStack
import concourse.bass as bass
import concourse.mybir as mybir
from concourse.bass_utils import run_bass_kernel_spmd

F32 = mybir.dt.float32
BF16 = mybir.dt.bfloat16
I32 = mybir.dt.int32
AF = mybir.ActivationFunctionType
ALU = mybir.AluOpType
AX = mybir.AxisListType

ENGS = ["tensor", "vector", "scalar", "gpsimd", "sync"]
N_DMA_SEMS = 8

D = 1024
T = 2048
DEPTH = 4
NPART = 2
TH = T // NPART
NT = TH // 128
NBLK = TH // 512
KC = D // 128
INC = 3096
DFF = 4096
NG = 8
import os as _os
PIPE = int(_os.environ.get("KPIPE", "1"))
EPS = 1e-6
C_MQ, C_MK, C_MV, C_MIF, C_MO = 0, 256, 512, 1024, 1032
C_GQ, C_GK, C_GV, C_GA, C_GG = 1544, 1672, 1800, 2056, 2072
C_RQ, C_RK, C_RV, C_RG = 2328, 2456, 2584, 2840
K_ID, K_TRI, K_ONES, K_HM, K_BM, K_RE, K_EL, K_IF, K_NTRI, K_T16, K_END = 0, 128, 256, 384, 388, 644, 900, 901, 920, 1048, 1176


class Buf:
    __slots__ = ("name", "last_w", "readers", "excl")

    def __init__(self, name="", excl=False):
        self.name = name
        self.last_w = None
        self.readers = []
        self.excl = excl


class Op:
    __slots__ = ("eng", "fn", "deps", "signal", "ordinal", "dma", "dma_sem", "dma_val", "dma_prev")

    def __init__(self, eng, fn, dma):
        self.eng = eng
        self.fn = fn
        self.deps = set()
        self.signal = False
        self.ordinal = None
        self.dma = dma
        self.dma_sem = None
        self.dma_val = None
        self.dma_prev = 0


class Sched:
    def __init__(self, same_engine_sync=True):
        self.ops = {e: [] for e in ENGS}
        self.same_engine_sync = same_engine_sync
        self.n_dma = {e: 0 for e in ENGS}

    def add(self, eng, fn, reads=(), writes=(), dma=False):
        op = Op(eng, fn, dma)
        deps = set()
        for b in reads:
            if b.last_w is not None:
                deps.add(b.last_w)
            if b.excl:
                for r in b.readers:
                    if r.eng != eng:
                        deps.add(r)
        for b in writes:
            if b.last_w is not None:
                deps.add(b.last_w)
            deps.update(b.readers)
        for b in reads:
            b.readers.append(op)
        for b in writes:
            b.last_w = op
            b.readers = []
        deps.discard(op)
        pruned = set()
        for d in deps:
            if d.eng == eng and not d.dma:
                if eng == "tensor" or not self.same_engine_sync:
                    continue
            pruned.add(d)
        op.deps = pruned
        for d in pruned:
            d.signal = True
        if dma:
            n = self.n_dma[eng]
            self.n_dma[eng] = n + 1
            op.dma_sem = n % N_DMA_SEMS
            op.dma_val = 16 * (n // N_DMA_SEMS + 1)
            op.dma_prev = 16 * (n // N_DMA_SEMS)
        self.ops[eng].append(op)
        return op

    def emit(self, nc, stack):
        for e in ENGS:
            c = 0
            for op in self.ops[e]:
                if not op.dma and op.signal:
                    c += 1
                    op.ordinal = c
        esem = {e: stack.enter_context(nc.semaphore("es_" + e)) for e in ENGS}
        dsem = {e: [stack.enter_context(nc.semaphore("ds_%s_%d" % (e, i))) for i in range(N_DMA_SEMS)]
                for e in ENGS if self.n_dma[e] > 0}
        block = stack.enter_context(nc.Block())
        nwaits = [0]

        def run(ename, eng):
            known = {}
            for op in self.ops[ename]:
                waits = {}
                for d in op.deps:
                    if d.dma:
                        s = dsem[d.eng][d.dma_sem]
                        v = d.dma_val
                    else:
                        s = esem[d.eng]
                        v = d.ordinal
                    key = id(s)
                    if key not in waits or waits[key][1] < v:
                        waits[key] = (s, v)
                if op.dma and op.dma_prev > 0:
                    s = dsem[ename][op.dma_sem]
                    key = id(s)
                    if key not in waits or waits[key][1] < op.dma_prev:
                        waits[key] = (s, op.dma_prev)
                for key, (s, v) in waits.items():
                    if known.get(key, 0) >= v:
                        continue
                    known[key] = v
                    eng.wait_ge(s, v)
                    nwaits[0] += 1
                ins = op.fn(eng)
                if ins is None:
                    continue
                if op.dma:
                    ins.then_inc(dsem[ename][op.dma_sem], 16)
                elif op.signal:
                    ins.then_inc(esem[ename], 1)

        @block.sync
        def _(e):
            run("sync", e)

        @block.tensor
        def _(e):
            run("tensor", e)

        @block.vector
        def _(e):
            run("vector", e)

        @block.scalar
        def _(e):
            run("scalar", e)

        @block.gpsimd
        def _(e):
            run("gpsimd", e)
        self.nwaits = nwaits[0]


def _dsize(dt):
    return 2 if dt == BF16 else 4


class Arena:
    def __init__(self, big, lo, hi):
        self.big = big
        self.lo = lo
        self.cur = lo
        self.hi = hi

    def alloc(self, free_shape, dtype=F32, parts=128):
        n = 1
        for s in free_shape:
            n *= s
        nbytes = n * _dsize(dtype)
        nbytes_r = (nbytes + 31) // 32 * 32
        off = self.cur
        self.cur += nbytes_r
        assert self.cur <= self.hi, "arena overflow %d > %d" % (self.cur, self.hi)
        assert nbytes % 4 == 0
        v = self.big[0:parts, off // 4: off // 4 + nbytes // 4]
        if dtype != F32:
            v = v.bitcast(dtype)
        if len(free_shape) == 2:
            v = v.rearrange("p (a b) -> p a b", a=free_shape[0])
        elif len(free_shape) == 3:
            v = v.rearrange("p (a b c) -> p a b c", a=free_shape[0], b=free_shape[1])
        return v


def bc_mid(ap2, n):
    P, F = ap2.shape
    return ap2.rearrange("p (o f) -> p o f", o=1).broadcast_to([P, n, F])


def bc_last(ap2, n):
    P, H = ap2.shape
    return ap2.rearrange("p (h o) -> p h o", o=1).broadcast_to([P, H, n])


def build_program(n_layers=DEPTH, run_parts=NPART, stage=None):
    nc = bass.Bass("TRN2", target_bir_lowering=False)
    L = n_layers

    def din(name, shape, dt=F32):
        return nc.dram_tensor(name, shape, dt, kind="ExternalInput").ap()

    x_d = din("x", [T, D])
    pos_d = din("pos", [128, T // 128], I32)
    gains_d = din("gains", [L * 4, D])
    w_in_d = din("w_in", [L, D, INC])
    w_out_d = din("w_out", [L, D, D])
    w_ff1_d = din("w_ff1", [L, D, DFF])
    w_ff2_d = din("w_ff2", [L, DFF, D])
    convT_d = din("convT", [512, L * 4])
    ifb_d = din("ifb", [1, L * 8])
    hn_d = din("hn", [L, D])
    wup_d = din("wup", [L, 16, 128])
    gb_d = din("gb", [1, L * 128])
    cf_d = din("cf", [128, K_END])
    out_d = nc.dram_tensor("out", [T, D], F32, kind="ExternalOutput").ap()

    S = Sched()

    class StopBuild(Exception):
        pass

    def ck(n):
        if stage is not None and stage == n:
            raise StopBuild()

    defer = [None]

    def op(eng, method, reads, writes, *args, **kw):
        if defer[0] is not None:
            defer[0].append((eng, method, reads, writes, args, kw))
            return None
        return S.add(eng, lambda e: getattr(e, method)(*args, **kw), reads, writes)

    def run_interleaved(chains, pools):
        lists = []
        for c, pool in zip(chains, pools):
            defer[0] = []
            bank_pool[0] = pool
            c()
            lists.append(defer[0])
        defer[0] = None
        bank_pool[0] = None
        idx = [0] * len(lists)
        while True:
            best, bf = None, 2.0
            for i, lst in enumerate(lists):
                if idx[i] < len(lst):
                    f = idx[i] / float(len(lst))
                    if f < bf:
                        best, bf = i, f
            if best is None:
                break
            eng, method, reads, writes, args, kw = lists[best][idx[best]]
            idx[best] += 1
            op(eng, method, reads, writes, *args, **kw)

    def dma(eng, out, in_, reads, writes):
        return S.add(eng, lambda e: e.dma_start(out=out, in_=in_), reads, writes, dma=True)

    st = ExitStack()
    with st:
        total = nc.sbuf_bytes_remaining
        total = (total // 128) * 128 - 128
        big_h = nc.alloc_sbuf_tensor("big", [128, total // 4], F32)
        A = Arena(big_h, 0, total)

        psum = [nc.alloc_psum_tensor("ps%d" % i, [128, 512], F32) for i in range(8)]
        psB = [Buf("ps%d" % i, excl=True) for i in range(8)]
        bank_ctr = [0]

        bank_pool = [None]
        pool_ctr = {}

        def bank():
            if bank_pool[0] is not None:
                lst = bank_pool[0]
                k = pool_ctr.get(lst, 0)
                pool_ctr[lst] = k + 1
                i = lst[k % len(lst)]
            else:
                i = bank_ctr[0] % 8
                bank_ctr[0] += 1
            return psum[i][:], psB[i]

        h = A.alloc([NT, D])
        hB = [Buf("h%d" % i) for i in range(NT)]
        winb = A.alloc([KC, INC], BF16)
        winB = [Buf("win%d" % i) for i in range(4)]
        woutb = A.alloc([KC, D], BF16)
        woutB = Buf("wout")
        ring = [A.alloc([8 * 512], BF16) for _ in range(2)]
        ringB = [Buf("ringA"), Buf("ringB")]
        w1v = ring[0].rearrange("p (c f) -> p c f", c=8)
        w2v = ring[1].rearrange("p (c d) -> p c d", c=4)
        cf = A.alloc([K_END])
        cfB = Buf("cf")
        identb = A.alloc([128], BF16)
        cosT = A.alloc([T // 128, 16])
        sinT = A.alloc([T // 128, 16])
        rotB = Buf("rot")
        cw = A.alloc([4, L * 4])
        ifb = A.alloc([L * 8])
        gbb = A.alloc([128]); gbbB = Buf("gbb")
        wup = A.alloc([128], parts=16); wupB = Buf("wup")
        smallB = Buf("small")
        Cst = [A.alloc([2, 129]) for _ in range(L)]
        Sg = [A.alloc([256]) for _ in range(L)]
        Sr = [A.alloc([256]) for _ in range(L)]
        ctail = [A.alloc([4, 3]) for _ in range(L)]
        stC = [Buf("stC%d" % l) for l in range(L)]
        stG = [Buf("stG%d" % l) for l in range(L)]
        stR = [Buf("stR%d" % l) for l in range(L)]
        stT = [Buf("stT%d" % l) for l in range(L)]
        CbfB = Buf("C_bf"); SgbfB = Buf("Sg_bf"); SrbfB = Buf("Sr_bf")
        C_bf = A.alloc([2, 129], BF16)
        Sg_bf = A.alloc([256], BF16)
        Sr_bf = A.alloc([256], BF16)
        v_ext = A.alloc([4, 129], BF16)
        vB = Buf("v_ext")
        gbc = A.alloc([D])
        gbcB = Buf("gbc")
        gbc2 = A.alloc([D])
        gbc2B = Buf("gbc2")
        sm = A.alloc([64])
        smB = Buf("sm")
        ss = sm[:, 0:1]
        ss2 = sm[:, 1:3]
        rstd = sm[:, 3:4]
        ph_lo = A.cur
        ph_hi = total

        M = Arena(big_h, ph_lo, ph_hi)
        hnbc = M.alloc([D]); hnB = Buf("hn")
        uTb = M.alloc([KC, 512], BF16); uTB = Buf("uTb")
        ytile = M.alloc([D], BF16); yB = Buf("ytile")
        xq = M.alloc([4, 515]); xqB = Buf("xq")
        eq = xq[:, :, 0:512]
        yq = M.alloc([4, 512]); yqB = Buf("yq")
        qkb = M.alloc([4, 512], BF16); qkB = Buf("qkb")
        gqk = M.alloc([2, 512], BF16); gqkB = Buf("gqk")
        gaT = M.alloc([512], parts=16); gaB = Buf("gaT")
        ifp = M.alloc([8]); lf = M.alloc([4]); t4 = M.alloc([4]); aj = M.alloc([4]); wj = M.alloc([4])
        den = M.alloc([4]); ssm = M.alloc([4]); fac = M.alloc([4]); mB = Buf("msmall")
        og = M.alloc([512], BF16); ogB = Buf("og")
        t512 = [M.alloc([512]) for _ in range(3)]; t512B = [Buf("t512_%d" % i) for i in range(3)]
        gate = M.alloc([512], BF16); gateB = Buf("gate")
        xg = t512[0]; xgB = t512B[0]
        ABUF = [dict(v_ext=v_ext, vB=vB, ifp=ifp, ifpB=Buf("ifp0"), og=og, ogB=ogB, gate=gate, gateB=gateB),
                dict(v_ext=M.alloc([4, 129], BF16), vB=Buf("v_ext1"), ifp=M.alloc([8]), ifpB=Buf("ifp1"), og=M.alloc([512], BF16), ogB=Buf("og1"),
                     gate=M.alloc([512], BF16), gateB=Buf("gate1"))]
        R = t512[2].rearrange("p (h i) -> p h i", h=4); RB = t512B[2]
        vgr = M.alloc([512], BF16); vgrB = Buf("vgr")
        ABUF[0].update(vgr=vgr, vgrB=vgrB)
        ABUF[1].update(vgr=M.alloc([512], BF16), vgrB=Buf("vgr1"))
        Xr = M.alloc([8, 32]); XrB = Buf("Xr")
        Xo = t512[1][:, 0:256].rearrange("p (a b) -> p a b", a=8); XoB = t512B[1]
        rt0 = t512[1][:, 256:384].rearrange("p (a b) -> p a b", a=8); rtB = t512B[1]
        qkr = M.alloc([256], BF16); qkrB = Buf("qkr")
        ABUF[0].update(qkr=qkr, qkrB=qkrB)
        ABUF[1].update(qkr=M.alloc([256], BF16), qkrB=Buf("qkr1"))
        DT = M.alloc([4, 128]); DTB = Buf("DT")
        E = M.alloc([4, 128]); EB = Buf("E")
        PT = M.alloc([4, 128], BF16); PTB = Buf("PT")
        qp = M.alloc([2, 128], BF16); qpB = Buf("qp")
        kp = M.alloc([256], BF16); kpB = Buf("kp")
        yT = M.alloc([KC, 128], BF16); yTB = Buf("yT")
        qt = M.alloc([128], BF16); qtB = Buf("qt")
        kt = M.alloc([128], BF16); ktB = Buf("kt")
        ktok = M.alloc([128], BF16); ktokB = Buf("ktok")
        Qbd = M.alloc([4, 128], BF16); QbdB = Buf("Qbd")
        rs4 = M.alloc([4]); rs4B = Buf("rs4")
        yqf = yq.rearrange("p a b -> p (a b)")
        xqf = xq.rearrange("p a b -> p (a b)")

        def bfv(ap):
            return ap.bitcast(BF16)
        PT_g = bfv(yqf[:, 0:256]).rearrange("p (h i) -> p h i", h=4); PTgB = Buf("PT_g")
        PT_r = bfv(yqf[:, 256:512]).rearrange("p (h i) -> p h i", h=4); PTrB = Buf("PT_r")
        Qbd_g = bfv(yqf[:, 512:768]).rearrange("p (h i) -> p h i", h=4); QbdgB = Buf("Qbd_g")
        Qbd_r = bfv(yqf[:, 768:1024]).rearrange("p (h i) -> p h i", h=4); QbdrB = Buf("Qbd_r")
        tmp_g = yqf[:, 1024:1280]; tmpgB = Buf("tmp_g")
        tmp_r = yqf[:, 1280:1536]; tmprB = Buf("tmp_r")
        tU_g = yqf[:, 1536:1792]; tUgB = Buf("tU_g")
        tU_r = yqf[:, 1792:2048]; tUrB = Buf("tU_r")
        la = xqf[:, 0:128]; laB = Buf("laE")
        Ep = xqf[:, 128:256]; En = xqf[:, 256:384]; EpB = laB
        qt_r = bfv(xqf[:, 384:448]); qtrB = Buf("qt_r")
        kt_r = bfv(xqf[:, 448:512]); ktrB = Buf("kt_r")
        junkm = bfv(xqf[:, 512:576]); junkmB = Buf("junkm")
        rs4_r = xqf[:, 576:580]; rs4rB = Buf("rs4_r")
        Zbufs = [PTgB, PTrB, QbdgB, QbdrB, tmpgB, tmprB, tUgB, tUrB, laB, qtrB, ktrB, junkmB, rs4rB]
        CB_G = dict(PT=PT_g, PTB=PTgB, Qbd=Qbd_g, QbdB=QbdgB, tmp=tmp_g, tmpB=tmpgB, tU=tU_g, tUB=tUgB, rs4=rs4, rs4B=rs4B, qt=qt, qtB=qtB, kt=kt, ktB=ktB)
        CB_R = dict(PT=PT_r, PTB=PTrB, Qbd=Qbd_r, QbdB=QbdrB, tmp=tmp_r, tmpB=tmprB, tU=tU_r, tUB=tUrB, rs4=rs4_r, rs4B=rs4rB, qt=qt_r, qtB=qtrB, kt=kt_r, ktB=ktrB)
        mixer_bufs = [ABUF[i][k] for i in range(2) for k in ("vB", "ifpB", "ogB", "gateB", "vgrB", "qkrB")] + [hnB, uTB, yB, xqB, yqB, qkB, gqkB, gaB, mB, ogB, gateB, vgrB, XrB, qkrB, DTB, EB, PTB,
                      qpB, kpB, yTB, PTgB, PTrB, QbdgB, QbdrB, tmpgB, tmprB, tUgB, tUrB, laB, qtrB, ktrB, junkmB, rs4rB, qtB, ktB, ktokB, QbdB, rs4B] + t512B

        FA = Arena(big_h, ph_lo, ph_hi)
        acc = FA.alloc([NT, D]); accB = [Buf("acc%d" % i) for i in range(NT)]
        uTh = FA.alloc([KC, TH], BF16); uThB = Buf("uTh")
        hdn = FA.alloc([4, TH], BF16); hdnB = Buf("hdn")
        rl = FA.alloc([512]); rlB = Buf("rl")
        utokf = FA.alloc([D], BF16); utokfB = Buf("utok_f")
        tf = FA.alloc([512]); tfB = Buf("tf")
        ffn_bufs = accB + [uThB, hdnB, rlB, utokfB, tfB]
        print("SBUF: persistent %d, mixer %d, ffn %d, phase avail %d" % (ph_lo, M.cur - ph_lo, FA.cur - ph_lo, ph_hi - ph_lo))

        def fenceZ():
            op("gpsimd", "memset", [], [xqB, yqB] + Zbufs, sm[:, 9:10], 0.0)

        def fence():
            op("gpsimd", "memset", [], mixer_bufs + ffn_bufs, sm[:, 8:9], 0.0)

        dma("sync", cf, cf_d, [], [cfB])
        ident = cf[:, K_ID:K_ID + 128]
        tri = cf[:, K_TRI:K_TRI + 128]
        ones = cf[:, K_ONES:K_ONES + 128]
        hmask = cf[:, K_HM:K_HM + 4]
        bmask = cf[:, K_BM:K_BM + 256]
        retE = cf[:, K_RE:K_RE + 256]
        elast_r = cf[:, K_EL:K_EL + 1]
        invf = cf[:, K_IF:K_IF + 16]
        hm2 = cf[:, K_IF + 16:K_IF + 18]
        ntri = cf[:, K_NTRI:K_NTRI + 128]
        tri16 = cf[:, K_T16:K_T16 + 128]
        op("vector", "tensor_copy", [cfB], [cfB], out=identb, in_=ident)
        dma("sync", cw, convT_d.rearrange("(m p) k -> p m k", p=128), [], [smallB])
        dma("sync", ifb, ifb_d.partition_broadcast(128), [], [smallB])

        RA = Arena(big_h, ph_lo, ph_hi)
        NTT = T // 128
        posi = RA.alloc([NTT], I32)
        posf = RA.alloc([NTT])
        ang = RA.alloc([NTT, 16])
        tq = RA.alloc([NTT, 16])
        tqi = RA.alloc([NTT, 16], I32)
        tB = Buf("rot_tmp")
        dma("sync", posi, pos_d, [], [tB])
        op("vector", "tensor_copy", [tB], [tB], out=posf, in_=posi)
        op("vector", "tensor_tensor", [tB, cfB], [tB], out=ang, in0=bc_last(posf, 16), in1=bc_mid(invf, NTT), op=ALU.mult)
        TWO_PI = float(2 * np.pi)
        PI = float(np.pi)

        def make_trig(dst, shift):
            op("vector", "tensor_scalar", [tB], [tB], out=tq, in0=ang, scalar1=shift, scalar2=1.0 / TWO_PI, op0=ALU.add, op1=ALU.mult)
            op("vector", "tensor_copy", [tB], [tB], out=tqi, in_=tq)
            op("vector", "tensor_copy", [tB], [tB], out=tq, in_=tqi)
            op("vector", "scalar_tensor_tensor", [tB], [tB], out=tq, in0=tq, scalar=-TWO_PI, in1=ang, op0=ALU.mult, op1=ALU.add)
            op("vector", "tensor_scalar", [tB], [tB], out=tq, in0=tq, scalar1=shift, scalar2=None, op0=ALU.add)
            op("vector", "tensor_scalar", [tB], [rotB], out=dst, in0=tq, scalar1=PI, scalar2=-TWO_PI, op0=ALU.is_gt, op1=ALU.mult)
            op("vector", "tensor_tensor", [tB, rotB], [tB], out=tq, in0=tq, in1=dst, op=ALU.add)
            op("vector", "tensor_scalar", [tB], [rotB], out=dst, in0=tq, scalar1=-PI, scalar2=TWO_PI, op0=ALU.is_lt, op1=ALU.mult)
            op("vector", "tensor_tensor", [tB, rotB], [tB], out=tq, in0=tq, in1=dst, op=ALU.add)
            op("vector", "tensor_scalar", [tB], [tB], out=tq, in0=tq, scalar1=PI, scalar2=-PI, op0=ALU.min, op1=ALU.max)
            op("scalar", "activation", [tB], [rotB], out=dst, in_=tq, func=AF.Sin)

        make_trig(sinT, 0.0)
        make_trig(cosT, float(np.pi / 2))
        op("gpsimd", "memset", [], [tB] + mixer_bufs + ffn_bufs, sm[:, 8:9], 0.0)

        for l in range(L):
            op("gpsimd", "memset", [], [stC[l]], Cst[l], 0.0)
            op("gpsimd", "memset", [], [stG[l]], Sg[l], 0.0)
            op("gpsimd", "memset", [], [stR[l]], Sr[l], 0.0)
            op("gpsimd", "memset", [], [stT[l]], ctail[l], 0.0)
        op("gpsimd", "memset", [], [vB], v_ext, 1.0)
        op("gpsimd", "memset", [], [ABUF[1]["vB"]], ABUF[1]["v_ext"], 1.0)

        def load_mixer_weights(l):
            src = w_in_d[l].rearrange("(c p) n -> p c n", p=128)
            for q in range(4):
                dma("gpsimd", winb[:, 2 * q:2 * q + 2, :], src[:, 2 * q:2 * q + 2, :], [], [winB[q]])
            dma("gpsimd", woutb, w_out_d[l].rearrange("(c p) n -> p c n", p=128), [], [woutB])

        def load_w1(l, g):
            dma("gpsimd", w1v, w_ff1_d[l][:, g * 512:(g + 1) * 512].rearrange("(c p) f -> p c f", p=128), [], [ringB[0]])

        def load_w2(l, g):
            dma("gpsimd", w2v, w_ff2_d[l][g * 512:(g + 1) * 512, :].rearrange("(c p) d -> p c d", p=128), [], [ringB[1]])

        def load_gain(row):
            if row % 2 == 0:
                dma("sync", gbc, gains_d[row:row + 1, :].partition_broadcast(128), [], [gbcB])
            else:
                dma("sync", gbc2, gains_d[row:row + 1, :].partition_broadcast(128), [], [gbc2B])

        def rstd_from(ss_ap, n, out_ap, buf):
            op("scalar", "activation", [buf], [buf], out=out_ap, in_=ss_ap, func=AF.Ln, scale=1.0 / n, bias=EPS)
            op("scalar", "activation", [buf], [buf], out=out_ap, in_=out_ap, func=AF.Exp, scale=-0.5)

        def norm_to_T(src_ap, srcB, utok, utokB, dstT, dstB):
            op("gpsimd", "memset", [], [smB], ss, 0.0)
            op("scalar", "activation", [srcB, smB], [utokB, smB], out=utok, in_=src_ap, func=AF.Square, accum_out=ss)
            rstd_from(ss, D, rstd, smB)
            op("vector", "scalar_tensor_tensor", [srcB, smB, gbcB], [utokB], out=utok, in0=src_ap, scalar=rstd, in1=gbc, op0=ALU.mult, op1=ALU.mult)
            pb, pB = bank()
            pbb = pb.bitcast(BF16)
            for k in range(KC):
                op("tensor", "transpose", [utokB, cfB], [pB], out=pbb[:, k * 128:(k + 1) * 128], in_=utok[:, k * 128:(k + 1) * 128], identity=identb)
            op("scalar", "activation", [pB], [dstB], out=dstT, in_=pbb.rearrange("p (k t) -> p k t", k=KC), func=AF.Copy)

        def post_norm_residual(srcs, srcBs, tmp, tmpB, tt):
            op("gpsimd", "memset", [], [smB], ss2, 0.0)
            for dh in range(2):
                op("scalar", "activation", [srcBs[dh], smB], [tmpB, smB], out=tmp.bitcast(BF16)[:, 0:512], in_=srcs[dh], func=AF.Square, accum_out=ss2[:, dh:dh + 1])
            op("vector", "tensor_tensor", [smB], [smB], out=ss, in0=ss2[:, 0:1], in1=ss2[:, 1:2], op=ALU.add)
            rstd_from(ss, D, rstd, smB)
            for dh in range(2):
                op("vector", "scalar_tensor_tensor", [srcBs[dh], smB, gbc2B], [tmpB], out=tmp, in0=srcs[dh], scalar=rstd, in1=gbc2[:, dh * 512:(dh + 1) * 512], op0=ALU.mult, op1=ALU.mult)
                hs = h[:, tt, dh * 512:(dh + 1) * 512]
                op("vector", "tensor_tensor", [tmpB, hB[tt]], [hB[tt]], out=hs, in0=hs, in1=tmp, op=ALU.add)

        def core(l, c0, v_ap, ktok_ap, ktok_B, Sst, Sbf, elast_ap, elB, gate_ap, ycols, cb):
            sB = cb["stB"]
            SbfB_ = cb["SbfB"]
            PT_, PTB_, Qbd_, QbdB_ = cb["PT"], cb["PTB"], cb["Qbd"], cb["QbdB"]
            tmp, tmpB, tU, tUB, rs_, rsB_ = cb["tmp"], cb["tmpB"], cb["tU"], cb["tUB"], cb["rs4"], cb["rs4B"]
            qt_, qtB_, kt_, ktB_ = cb["qt"], cb["qtB"], cb["kt"], cb["ktB"]
            op("gpsimd", "tensor_tensor", [qtB_, cfB], [QbdB_], out=Qbd_, in0=bc_mid(qt_, 4), in1=bc_last(hmask, 128), op=ALU.mult)
            Sc, ScB = bank()
            op("tensor", "matmul", [ktB_, QbdB_], [ScB], Sc, lhsT=kt_, rhs=Qbd_.rearrange("p h i -> p (h i)"), start=True, stop=True)
            op("vector", "tensor_tensor", [ScB, cfB], [PTB_], out=PT_, in0=Sc.rearrange("p (h i) -> p h i", h=4), in1=bc_mid(tri, 4), op=ALU.mult)
            ob, oB = bank()
            for hh in range(4):
                op("tensor", "matmul", [PTB_, vgrB], [oB], ob[:, hh * 64:(hh + 1) * 64], lhsT=PT_[:, hh, :], rhs=v_ap[:, hh * 64:(hh + 1) * 64], start=(hh == 0), stop=False, skip_group_check=True)
            op("tensor", "matmul", [qtB_, SbfB_], [oB], ob[:, 0:256], lhsT=qt_, rhs=Sbf, start=False, stop=True, skip_group_check=True)
            o3 = ob[:, 0:256].rearrange("p (h e) -> p h e", h=4)
            tmp3 = tmp.rearrange("p (h e) -> p h e", h=4)
            op("scalar", "activation", [oB], [tmpB], out=tmp, in_=ob[:, 0:256], func=AF.Square)
            op("vector", "tensor_reduce", [tmpB], [rsB_], out=rs_, in_=tmp3, axis=AX.X, op=ALU.add)
            rstd_from(rs_, 64, rs_, rsB_)
            op("vector", "tensor_tensor", [oB, rsB_], [tmpB], out=tmp3, in0=o3, in1=bc_last(rs_, 64), op=ALU.mult)
            op("gpsimd", "tensor_tensor", [tmpB, gateB], [yB], out=ytile[:, ycols:ycols + 256], in0=tmp, in1=gate_ap, op=ALU.mult)
            ub, uB = bank()
            op("tensor", "matmul", [ktok_B, vgrB], [uB], ub[:, 0:256], lhsT=ktok_ap, rhs=v_ap, start=True, stop=True)
            op("vector", "scalar_tensor_tensor", [uB, elB, cfB], [tUB], out=tU, in0=ub[:, 0:256], scalar=elast_ap, in1=bmask, op0=ALU.mult, op1=ALU.mult)
            op("vector", "scalar_tensor_tensor", [sB, elB, tUB], [sB], out=Sst, in0=Sst, scalar=elast_ap, in1=tU, op0=ALU.mult, op1=ALU.add)
            op("gpsimd", "tensor_copy", [sB], [SbfB_], out=Sbf, in_=Sst)

        load_mixer_weights(0)
        load_w1(0, 0)
        load_w2(0, 0)
        for part in range(run_parts):
            t0 = part * TH
            xsrc = x_d[t0:t0 + TH, :].rearrange("(t p) d -> p t d", p=128)
            for q in range(NT):
                dma("sync", h[:, q, :], xsrc[:, q, :], [], [hB[q]])

            for l in range(L if stage != 0 else 0):
              try:
                load_gain(l * 4 + 0)
                load_gain(l * 4 + 1)
                op("gpsimd", "tensor_copy", [stC[l]], [CbfB], out=C_bf, in_=Cst[l])
                op("gpsimd", "tensor_copy", [stG[l]], [SgbfB], out=Sg_bf, in_=Sg[l])
                op("gpsimd", "tensor_copy", [stR[l]], [SrbfB], out=Sr_bf, in_=Sr[l])
                dma("sync", gbb, gb_d[:, l * 128:(l + 1) * 128].partition_broadcast(128), [], [gbbB])
                dma("sync", wup, wup_d[l], [], [wupB])
                dma("sync", hnbc, hn_d[l:l + 1, :].partition_broadcast(128), [], [hnB])
                for blk in range(NBLK):
                    for ti in range(4):
                        tt = blk * 4 + ti
                        norm_to_T(h[:, tt, :], hB[tt], ytile, yB, uTb[:, :, ti * 128:(ti + 1) * 128], uTB)
                    fenceZ()
                    op("gpsimd", "tensor_copy", [stT[l]], [xqB], out=xq[:, :, 0:3], in_=ctail[l])
                    bcols = [C_MQ, C_MQ + 128, C_MK, C_MK + 128, C_GQ, C_GK, C_GA]
                    bM = [128, 128, 128, 128, 128, 128, 16]
                    for i in range(7):
                        pb, pB = bank()
                        for k in range(KC):
                            op("tensor", "matmul", [winB[k // 2], uTB], [pB], pb[0:bM[i], :], lhsT=winb[:, k, bcols[i]:bcols[i] + bM[i]], rhs=uTb[:, k, :],
                               start=(k == 0), stop=(k == KC - 1))
                        if i < 4:
                            op("scalar", "activation", [pB], [xqB], out=xq[:, i, 3:515], in_=pb, func=AF.Copy)
                        elif i == 4:
                            op("scalar", "activation", [pB], [gqkB], out=gqk[:, 0, :], in_=pb, func=AF.Copy, scale=float(32 ** -0.5))
                        elif i == 5:
                            op("scalar", "activation", [pB], [gqkB], out=gqk[:, 1, :], in_=pb, func=AF.Copy)
                        else:
                            op("scalar", "activation", [pB], [gaB], out=gaT, in_=pb[0:16, :], func=AF.Copy)
                    for i in range(4):
                        op("scalar", "activation", [xqB, smallB], [yqB], out=yq[:, i, :], in_=xq[:, i, 3:515], func=AF.Copy, scale=cw[:, i, l * 4 + 3:l * 4 + 4])
                        for s in (2, 1, 0):
                            op("vector", "scalar_tensor_tensor", [xqB, smallB, yqB], [yqB], out=yq[:, i, :], in0=xq[:, i, s:s + 512], scalar=cw[:, i, l * 4 + s:l * 4 + s + 1],
                               in1=yq[:, i, :], op0=ALU.mult, op1=ALU.add)
                    op("gpsimd", "tensor_copy", [xqB], [stT[l]], out=ctail[l], in_=xq[:, :, 512:515])
                    op("scalar", "activation", [yqB], [xqB], out=eq, in_=yq, func=AF.Exp, scale=-1.0)
                    op("scalar", "activation", [xqB], [xqB], out=eq, in_=eq, func=AF.Ln, bias=1.0)
                    op("scalar", "activation", [xqB], [xqB], out=eq, in_=eq, func=AF.Exp, scale=-1.0)
                    op("vector", "tensor_tensor", [yqB, xqB], [qkB], out=qkb[:, 0:2, :], in0=yq[:, 0:2, :], in1=eq[:, 0:2, :], op=ALU.mult)
                    op("vector", "scalar_tensor_tensor", [yqB, xqB], [qkB], out=qkb[:, 2:4, :], in0=yq[:, 2:4, :], scalar=0.125, in1=eq[:, 2:4, :], op0=ALU.mult, op1=ALU.mult)

                    fenceZ()
                    ck(1)
                    def A_phase(ti, b):
                        tt = blk * 4 + ti
                        gt = part * NT + tt
                        c0 = ti * 128

                        def amm(c_lo, n):
                            pb, pB = bank()
                            for k in range(KC):
                                op("tensor", "matmul", [winB[k // 2], uTB], [pB], pb[:, 0:n], lhsT=uTb[:, k, c0:c0 + 128], rhs=winb[:, k, c_lo:c_lo + n],
                                   start=(k == 0), stop=(k == KC - 1))
                            return pb, pB
                        pb, pB = amm(C_MV, 512)
                        op("scalar", "activation", [pB], [b["vB"]], out=b["v_ext"][:, :, 0:128], in_=pb.rearrange("p (h e) -> p h e", h=4), func=AF.Copy)
                        pb, pB = amm(C_MIF, 8)
                        op("vector", "tensor_tensor", [pB, smallB], [b["ifpB"]], out=b["ifp"], in0=pb[:, 0:8], in1=ifb[:, l * 8:(l + 1) * 8], op=ALU.add)
                        pb, pB = amm(C_MO, 512)
                        op("scalar", "activation", [pB], [t512B[0]], out=t512[0], in_=pb, func=AF.Exp, scale=-1.0)
                        op("scalar", "activation", [t512B[0]], [t512B[0]], out=t512[0], in_=t512[0], func=AF.Ln, bias=1.0)
                        op("scalar", "activation", [t512B[0]], [t512B[0]], out=t512[0], in_=t512[0], func=AF.Exp, scale=-1.0)
                        op("vector", "tensor_tensor", [t512B[0], hnB], [b["ogB"]], out=b["og"], in0=t512[0], in1=hnbc[:, 0:512], op=ALU.mult)
                        pb, pB = amm(C_GV, 256)
                        op("scalar", "activation", [pB], [b["vgrB"]], out=b["vgr"][:, 0:256], in_=pb[:, 0:256], func=AF.Copy)
                        pb, pB = amm(C_GG, 512)
                        op("scalar", "activation", [pB], [xgB], out=xg[:, 0:256], in_=pb[:, 0:256], func=AF.Copy)
                        op("scalar", "activation", [pB], [t512B[1]], out=t512[1][:, 0:256], in_=pb[:, 0:256], func=AF.Exp, scale=-1.0)
                        op("scalar", "activation", [pB], [XrB], out=Xr.rearrange("p a b -> p (a b)"), in_=pb[:, 256:512], func=AF.Copy)
                        pb, pB = amm(C_RV, 512)
                        op("scalar", "activation", [pB], [b["vgrB"]], out=b["vgr"][:, 256:512], in_=pb[:, 0:256], func=AF.Copy)
                        op("scalar", "activation", [pB], [xgB], out=xg[:, 256:512], in_=pb[:, 256:512], func=AF.Copy)
                        op("scalar", "activation", [pB], [t512B[1]], out=t512[1][:, 256:512], in_=pb[:, 256:512], func=AF.Exp, scale=-1.0)
                        op("gpsimd", "tensor_tensor", [xgB, hnB], [xgB], out=xg, in0=xg, in1=hnbc[:, 512:1024], op=ALU.mult)
                        op("scalar", "activation", [t512B[1]], [t512B[1]], out=t512[1], in_=t512[1], func=AF.Ln, bias=1.0)
                        op("scalar", "activation", [t512B[1]], [t512B[1]], out=t512[1], in_=t512[1], func=AF.Exp, scale=-1.0)
                        op("vector", "tensor_tensor", [xgB, t512B[1]], [b["gateB"]], out=b["gate"], in0=xg, in1=t512[1], op=ALU.mult)
                        cs = bc_mid(cosT[:, gt, :], 8)
                        sn = bc_mid(sinT[:, gt, :], 8)
                        X1 = Xr[:, :, 0:16]
                        X2 = Xr[:, :, 16:32]
                        op("gpsimd", "tensor_tensor", [XrB, rotB], [rtB], out=rt0, in0=X2, in1=sn, op=ALU.mult)
                        op("gpsimd", "tensor_tensor", [XrB, rotB], [XoB], out=Xo[:, :, 0:16], in0=X1, in1=cs, op=ALU.mult)
                        op("gpsimd", "tensor_tensor", [rtB, XoB], [XoB], out=Xo[:, :, 0:16], in0=Xo[:, :, 0:16], in1=rt0, op=ALU.subtract)
                        op("gpsimd", "tensor_tensor", [XrB, rotB], [rtB], out=rt0, in0=X1, in1=sn, op=ALU.mult)
                        op("gpsimd", "tensor_tensor", [XrB, rotB, XoB], [XoB], out=Xo[:, :, 16:32], in0=X2, in1=cs, op=ALU.mult)
                        op("gpsimd", "tensor_tensor", [rtB, XoB], [XoB], out=Xo[:, :, 16:32], in0=Xo[:, :, 16:32], in1=rt0, op=ALU.add)
                        op("gpsimd", "tensor_tensor", [XoB, cfB], [b["qkrB"]], out=b["qkr"], in0=Xo.rearrange("p a b -> p (a b)"), in1=retE, op=ALU.mult)

                    if PIPE:
                        A_phase(0, ABUF[0])
                    for ti in range(4):
                        if not PIPE:
                            A_phase(ti, ABUF[ti % 2])
                        tt = blk * 4 + ti
                        gt = part * NT + tt
                        c0 = ti * 128
                        cur = ABUF[ti % 2]
                        v_ext, vB, ifp, ifpB, og, ogB = cur["v_ext"], cur["vB"], cur["ifp"], cur["ifpB"], cur["og"], cur["ogB"]
                        gate, gateB, vgr, vgrB, qkr, qkrB = cur["gate"], cur["gateB"], cur["vgr"], cur["vgrB"], cur["qkr"], cur["qkrB"]
                        def chain_m():
                            op("scalar", "activation", [mB, ifpB], [mB], out=t4, in_=ifp[:, 4:8], func=AF.Exp, scale=-1.0)
                            op("scalar", "activation", [mB], [mB], out=t4, in_=t4, func=AF.Ln, bias=1.0)
                            op("vector", "tensor_tensor", [mB, cfB], [RB], out=R, in0=bc_mid(ntri, 4), in1=bc_last(t4, 128), op=ALU.mult)
                            Bb, BbB = bank()
                            op("tensor", "matmul", [RB, cfB], [BbB], Bb, lhsT=ones, rhs=R.rearrange("p h i -> p (h i)"), start=True, stop=True)
                            bj, bjB = bank()
                            op("tensor", "matmul", [mB, cfB], [bjB], bj[:, 0:4], lhsT=ntri, rhs=t4, start=True, stop=True)
                            op("vector", "scalar_tensor_tensor", [bjB, mB, ifpB], [mB], out=aj, in0=bj[:, 0:4], scalar=-1.0, in1=ifp[:, 0:4], op0=ALU.mult, op1=ALU.add)
                            Bb3 = Bb.rearrange("p (h i) -> p h i", h=4)
                            for hh in range(4):
                                op("scalar", "activation", [BbB, mB], [DTB], out=DT[:, hh, :], in_=Bb3[:, hh, :], func=AF.Exp, bias=aj[:, hh:hh + 1])
                            op("scalar", "activation", [BbB], [EB], out=E.rearrange("p h i -> p (h i)"), in_=Bb, func=AF.Exp)
                            op("vector", "tensor_tensor", [BbB, mB], [mB], out=wj, in0=Bb3[:, :, 127], in1=aj, op=ALU.add)
                            op("scalar", "activation", [mB], [mB], out=wj, in_=wj, func=AF.Exp)
                            Sc, ScB = bank()
                            for mt in range(2):
                                op("gpsimd", "tensor_tensor", [qkB, cfB], [QbdB], out=Qbd[:, 2 * mt:2 * mt + 2, :], in0=bc_mid(qkb[:, mt, c0:c0 + 128], 2),
                                   in1=bc_last(hm2, 128), op=ALU.mult)
                            for mt in range(2):
                                op("tensor", "matmul", [qkB, QbdB], [ScB], Sc[:, mt * 256:(mt + 1) * 256], lhsT=qkb[:, 2 + mt, c0:c0 + 128],
                                   rhs=Qbd[:, 2 * mt:2 * mt + 2, :].rearrange("p h i -> p (h i)"), start=True, stop=True)
                            tS = t512[2]
                            op("vector", "tensor_tensor", [ScB, cfB], [t512B[2]], out=tS.rearrange("p (h i) -> p h i", h=4), in0=Sc.rearrange("p (h i) -> p h i", h=4),
                               in1=bc_mid(tri, 4), op=ALU.mult)
                            op("vector", "tensor_tensor", [t512B[2], DTB], [PTB], out=PT.rearrange("p h i -> p (h i)"), in0=tS, in1=DT.rearrange("p h i -> p (h i)"), op=ALU.mult)
                            for mt in range(2):
                                for hh in range(2):
                                    r0 = hh * 64
                                    op("gpsimd", "tensor_tensor", [qkB, EB], [qpB], out=qp[r0:r0 + 64, mt, :], in0=qkb[r0:r0 + 64, mt, c0:c0 + 128], in1=E[r0:r0 + 64, 2 * mt + hh, :], op=ALU.mult)
                            O = [bank(), bank()]
                            for hh in range(4):
                                ob, oB = O[hh // 2]
                                r0 = (hh % 2) * 64
                                osl = ob[:, (hh % 2) * 129:(hh % 2) * 129 + 129]
                                op("tensor", "matmul", [PTB, vB], [oB], osl, lhsT=PT[:, hh, :], rhs=v_ext[:, hh, :], start=(hh % 2 == 0), stop=False, skip_group_check=True)
                                op("tensor", "matmul", [qpB, CbfB], [oB], osl, lhsT=qp[r0:r0 + 64, hh // 2, :], rhs=C_bf[r0:r0 + 64, hh // 2, :], start=False, stop=True, skip_group_check=True)
                            for b2 in range(2):
                                ob, oB = O[b2]
                                op("vector", "tensor_copy", [oB], [mB], out=den[:, 2 * b2:2 * b2 + 2], in_=ob[:, 0:258].rearrange("p (h c) -> p h c", h=2)[:, :, 128])
                            op("vector", "tensor_scalar", [mB], [mB], out=t4, in0=den, scalar1=-1.0, scalar2=None, op0=ALU.mult)
                            op("vector", "tensor_tensor", [mB], [mB], out=den, in0=den, in1=t4, op=ALU.max)
                            op("vector", "tensor_scalar", [mB], [mB], out=den, in0=den, scalar1=1.0, scalar2=None, op0=ALU.max)
                            op("vector", "reciprocal", [mB], [mB], out=den, in_=den)
                            op("gpsimd", "memset", [], [mB], ssm, 0.0)
                            for hh in range(4):
                                ob, oB = O[hh // 2]
                                op("scalar", "activation", [oB, mB], [junkmB, mB], out=junkm, in_=ob[:, (hh % 2) * 129:(hh % 2) * 129 + 128], func=AF.Square,
                                   accum_out=ssm[:, hh:hh + 1])
                            op("vector", "tensor_tensor", [mB], [mB], out=fac, in0=den, in1=den, op=ALU.mult)
                            op("vector", "tensor_tensor", [mB], [mB], out=fac, in0=fac, in1=ssm, op=ALU.mult)
                            rstd_from(fac, 128, fac, mB)
                            op("vector", "tensor_tensor", [mB], [mB], out=fac, in0=fac, in1=den, op=ALU.mult)
                            for hh in range(4):
                                ob, oB = O[hh // 2]
                                op("vector", "scalar_tensor_tensor", [oB, mB, ogB], [yB], out=ytile[:, hh * 128:(hh + 1) * 128], in0=ob[:, (hh % 2) * 129:(hh % 2) * 129 + 128],
                                   scalar=fac[:, hh:hh + 1], in1=og[:, hh * 128:(hh + 1) * 128], op0=ALU.mult, op1=ALU.mult)
                            Kp, KpB = bank()
                            Kpb = Kp.bitcast(BF16)
                            for mt in range(2):
                                op("tensor", "transpose", [qkB, cfB], [KpB], out=Kpb[:, mt * 128:(mt + 1) * 128], in_=qkb[:, 2 + mt, c0:c0 + 128], identity=identb)
                            op("vector", "tensor_tensor", [KpB, mB], [kpB], out=kp.rearrange("p (h d) -> p h d", h=4), in0=Kpb[:, 0:256].rearrange("p (h d) -> p h d", h=4),
                               in1=bc_last(wj, 64), op=ALU.mult)
                            U = [bank(), bank()]
                            for mt in range(2):
                                ub, uB = U[mt]
                                for hh in range(2):
                                    op("tensor", "matmul", [kpB, vB], [uB], ub[:, hh * 129:hh * 129 + 129], lhsT=kp[:, mt * 128:(mt + 1) * 128], rhs=v_ext[:, 2 * mt + hh, :], start=True, stop=True)
                                for hh in range(2):
                                    r0 = hh * 64
                                    cs_ = Cst[l][r0:r0 + 64, mt, :]
                                    op("vector", "scalar_tensor_tensor", [uB, EB, stC[l]], [stC[l]], out=cs_, in0=cs_, scalar=E[r0:r0 + 64, 2 * mt + hh, 127:128],
                                       in1=ub[r0:r0 + 64, hh * 129:hh * 129 + 129], op0=ALU.mult, op1=ALU.add)

                            op("gpsimd", "tensor_copy", [stC[l]], [CbfB], out=C_bf, in_=Cst[l])

                        def chain_g():
                            Lp, LpB = bank()
                            op("tensor", "matmul", [gaB, wupB], [LpB], Lp[:, 0:128], lhsT=gaT[:, c0:c0 + 128], rhs=wup, start=True, stop=True)
                            op("vector", "tensor_tensor", [LpB, gbbB], [laB], out=la, in0=Lp[:, 0:128], in1=gbb, op=ALU.add)
                            op("scalar", "activation", [laB], [laB], out=la, in_=la, func=AF.Exp, scale=-1.0)
                            op("scalar", "activation", [laB], [laB], out=la, in_=la, func=AF.Ln, bias=1.0)
                            BT, BTB = bank()
                            op("tensor", "matmul", [laB, cfB], [BTB], BT[:, 0:128], lhsT=la, rhs=tri16, start=True, stop=True)
                            op("scalar", "activation", [BTB], [EpB], out=Ep, in_=BT[:, 0:128], func=AF.Exp)
                            op("scalar", "activation", [BTB], [EpB], out=En, in_=BT[:, 0:128], func=AF.Exp, scale=-1.0)
                            op("gpsimd", "tensor_tensor", [gqkB, EpB], [qtB], out=qt, in0=gqk[:, 0, c0:c0 + 128], in1=Ep, op=ALU.mult)
                            op("gpsimd", "tensor_tensor", [gqkB, EpB], [ktB], out=kt, in0=gqk[:, 1, c0:c0 + 128], in1=En, op=ALU.mult)
                            Tp, TpB = bank()
                            Tpb = Tp.bitcast(BF16)
                            op("tensor", "transpose", [ktB, cfB], [TpB], out=Tpb[:, 0:128], in_=kt, identity=identb)
                            op("scalar", "activation", [TpB], [ktokB], out=ktok, in_=Tpb[:, 0:128], func=AF.Copy)
                            core(l, c0, vgr[:, 0:256], ktok, ktokB, Sg[l], Sg_bf, Ep[:, 127:128], EpB, gate[:, 0:256], 512, dict(CB_G, stB=stG[l], SbfB=SgbfB))


                        def chain_r():
                            Tp2, Tp2B = bank()
                            Tp2b = Tp2.bitcast(BF16)
                            op("tensor", "transpose", [qkrB, cfB], [Tp2B], out=Tp2b[:, 0:128], in_=qkr[:, 0:128], identity=identb)
                            op("tensor", "transpose", [qkrB, cfB], [Tp2B], out=Tp2b[:, 128:256], in_=qkr[:, 128:256], identity=identb)
                            op("scalar", "activation", [Tp2B], [qtrB], out=qt_r, in_=Tp2b[:, 0:128], func=AF.Copy)
                            op("scalar", "activation", [Tp2B], [ktrB], out=kt_r, in_=Tp2b[:, 128:256], func=AF.Copy)
                            core(l, c0, vgr[:, 256:512], qkr[:, 128:256], qkrB, Sr[l], Sr_bf, elast_r, cfB, gate[:, 256:512], 768, dict(CB_R, stB=stR[l], SbfB=SrbfB))


                        chains = [chain_m, chain_g, chain_r]
                        pools = [(0, 1), (2, 3), (4, 5)]
                        if ti < 3 and PIPE:
                            chains.append(lambda ti=ti: A_phase(ti + 1, ABUF[(ti + 1) % 2]))
                            pools.append((6, 7))
                        run_interleaved(chains, pools)
                        ck(5)
                        pb, pB = bank()
                        pbb = pb.bitcast(BF16)
                        for k in range(KC):
                            op("tensor", "transpose", [yB, cfB], [pB], out=pbb[:, k * 128:(k + 1) * 128], in_=ytile[:, k * 128:(k + 1) * 128], identity=identb)
                        op("scalar", "activation", [pB], [yTB], out=yT, in_=pbb.rearrange("p (k t) -> p k t", k=KC), func=AF.Copy)
                        W = [bank(), bank()]
                        for dh in range(2):
                            wb, wB = W[dh]
                            for k in range(KC):
                                op("tensor", "matmul", [yTB, woutB], [wB], wb, lhsT=yT[:, k, :], rhs=woutb[:, k, dh * 512:(dh + 1) * 512], start=(k == 0), stop=(k == KC - 1))
                        post_norm_residual([W[0][0], W[1][0]], [W[0][1], W[1][1]], t512[1], t512B[1], tt)

                ck(6)
                fence()
                load_gain(l * 4 + 2)
                for tt in range(NT):
                    norm_to_T(h[:, tt, :], hB[tt], utokf, utokfB, uTh[:, :, tt * 128:(tt + 1) * 128], uThB)
                load_gain(l * 4 + 3)
                ck(7)
                nxt = None
                if l + 1 < L:
                    nxt = l + 1
                elif part + 1 < run_parts:
                    nxt = 0
                for g in range(NG):
                    for tb in range(TH // 512):
                        for fc in range(4):
                            pb, pB = bank()
                            for k in range(KC):
                                op("tensor", "matmul", [ringB[0], uThB], [pB], pb, lhsT=w1v[:, k, fc * 128:(fc + 1) * 128], rhs=uTh[:, k, tb * 512:(tb + 1) * 512],
                                   start=(k == 0), stop=(k == KC - 1))
                            op("scalar", "activation", [pB], [rlB], out=rl, in_=pb, func=AF.Relu)
                            op("vector", "tensor_tensor", [rlB], [hdnB], out=hdn[:, fc, tb * 512:(tb + 1) * 512], in0=rl, in1=rl, op=ALU.mult)
                    if g + 1 < NG:
                        load_w1(l, g + 1)
                    elif nxt is not None:
                        load_w1(nxt, 0)
                    for tt in range(NT):
                        for dh in range(2):
                            pb, pB = bank()
                            for fc in range(4):
                                op("tensor", "matmul", [hdnB, ringB[1]], [pB], pb, lhsT=hdn[:, fc, tt * 128:(tt + 1) * 128], rhs=w2v[:, fc, dh * 512:(dh + 1) * 512],
                                   start=(fc == 0), stop=(fc == 3))
                            a_ = acc[:, tt, dh * 512:(dh + 1) * 512]
                            if g == 0:
                                op("vector", "tensor_copy", [pB], [accB[tt]], out=a_, in_=pb)
                            else:
                                op("vector", "tensor_tensor", [pB, accB[tt]], [accB[tt]], out=a_, in0=pb, in1=a_, op=ALU.add)
                        if g == NG - 1:
                            post_norm_residual([acc[:, tt, 0:512], acc[:, tt, 512:1024]], [accB[tt], accB[tt]], tf, tfB, tt)
                    if g + 1 < NG:
                        load_w2(l, g + 1)
                    elif nxt is not None:
                        load_w2(nxt, 0)
                    if g == 3 and nxt is not None:
                        load_mixer_weights(nxt)
                fence()
              except StopBuild:
                break

            osrc = out_d[t0:t0 + TH, :].rearrange("(t p) d -> p t d", p=128)
            outB = Buf("out")
            for q in range(NT):
                dma("sync", osrc[:, q, :], h[:, q, :], [hB[q]], [outB])
            S.add("sync", lambda e: None, [outB], [])

        S.emit(nc, st)
    return nc, S


def make_consts():
    cf = np.zeros((128, K_END), np.float32)
    p = np.arange(128)
    cf[:, K_ID:K_ID + 128] = np.eye(128, dtype=np.float32)
    tri = (p[:, None] <= p[None, :]).astype(np.float32)
    cf[:, K_TRI:K_TRI + 128] = tri
    cf[:, K_NTRI:K_NTRI + 128] = -tri
    cf[:, K_T16:K_T16 + 128] = -tri / 16.0
    cf[:, K_ONES:K_ONES + 128] = 1.0
    cf[:, K_HM:K_HM + 4] = (p[:, None] // 32 == np.arange(4)[None, :]).astype(np.float32)
    cf[:, K_BM:K_BM + 256] = (p[:, None] // 32 == (np.arange(256)[None, :] // 64)).astype(np.float32)
    lg = np.log1p(-np.exp2(-5.0 - np.arange(4, dtype=np.float64)))
    hd = np.arange(128) // 32
    tok = np.arange(128, dtype=np.float64)
    epos = np.exp((tok[:, None] + 1.0) * lg[hd][None, :])
    eneg = np.exp(-(tok[:, None] + 1.0) * lg[hd][None, :]) * (32.0 ** -0.5)
    cf[:, K_RE:K_RE + 128] = epos
    cf[:, K_RE + 128:K_RE + 256] = eneg
    cf[:, K_EL] = np.exp(128.0 * lg[hd])
    invf = (np.float32(10000.0) ** (-np.arange(0, 32, 2, dtype=np.float32) / np.float32(32))).astype(np.float32)
    cf[:, K_IF:K_IF + 16] = invf[None, :]
    cf[:, K_IF + 16:K_IF + 18] = (p[:, None] // 64 == np.arange(2)[None, :]).astype(np.float32)
    return cf


def prepare_inputs(inputs, n_layers=DEPTH):
    L = n_layers
    f32 = np.float32
    g = lambda k: np.asarray(inputs[k])
    gains = np.stack([g("norm_pre_mix")[:L], g("norm_post_mix")[:L], g("norm_pre_ffn")[:L], g("norm_post_ffn")[:L]], axis=1).reshape(L * 4, D).astype(f32)
    convT = np.ascontiguousarray(np.transpose(g("mlstm_conv_w")[:L], (2, 0, 1)).reshape(512, L * 4)).astype(f32)
    ifb = np.concatenate([g("mlstm_i_bias")[:L], g("mlstm_f_bias")[:L]], axis=1).reshape(1, L * 8).astype(f32)
    hn = np.concatenate([g("mlstm_norm")[:L], g("gla_norm")[:L], g("ret_norm")[:L]], axis=1).astype(f32)
    shared = {
        "gains": np.ascontiguousarray(gains),
        "w_in": np.ascontiguousarray(g("w_in")[:L], dtype=f32),
        "w_out": np.ascontiguousarray(g("w_out")[:L], dtype=f32),
        "w_ff1": np.ascontiguousarray(g("w_ff1")[:L], dtype=f32),
        "w_ff2": np.ascontiguousarray(g("w_ff2")[:L], dtype=f32),
        "convT": convT,
        "ifb": np.ascontiguousarray(ifb),
        "hn": np.ascontiguousarray(hn),
        "wup": np.ascontiguousarray(g("gla_w_up")[:L], dtype=f32),
        "gb": np.ascontiguousarray(g("gla_gate_bias")[:L].reshape(1, L * 128), dtype=f32),
        "cf": make_consts(),
    }
    x = g("x")
    pos = g("positions")
    maps = []
    for b in range(x.shape[0]):
        m = dict(shared)
        m["x"] = np.ascontiguousarray(x[b], dtype=f32)
        m["pos"] = np.ascontiguousarray(pos[b].reshape(T // 128, 128).T).astype(np.int32)
        maps.append(m)
    return maps


_CACHE = {}


def kernel(**inputs):
    if "nc" not in _CACHE:
        _CACHE["nc"] = build_program()[0]
    nc = _CACHE["nc"]
    maps = prepare_inputs(inputs)
    res = run_bass_kernel_spmd(nc, maps, core_ids=list(range(len(maps))))
    out = np.stack([np.asarray(r["out"]) for r in res.results], axis=0)
    return out.astype(np.float32)
```
